# Optimizing a Trainium2 kernel written in Bass

```python
import math
import jax, jax.numpy as jnp
from jax import lax
import numpy as np

D_MODEL = 2048
BATCH = 4
SEQ = 4096
DEPTH = 1

HEAD_DIM = 128
A_Q_HEADS = 8
A_KV_HEADS = 2
B_Q_HEADS = 8
B_KV_HEADS = 2
D_MIX = (A_Q_HEADS + B_Q_HEADS) * HEAD_DIM
QKV_A = (A_Q_HEADS + 2 * A_KV_HEADS) * HEAD_DIM
QKV_B = (B_Q_HEADS + 2 * B_KV_HEADS) * HEAD_DIM
D_IN = QKV_A + QKV_B
BLOCK = 128
WINDOW = 128
GRID_W = 64
ROPE_THETA = 10000.0
ROPE_AXIS_DIM = HEAD_DIM // 2
N_BUCKETS = 32
MAX_DISTANCE = 128
N_EXPERTS = 16
CAPACITY_FACTOR = 2
D_EXPERT = 2048
EPS = 1e-6

kernel_name = "hybrid_axialgqa_swagqa_ecmoe_layer"


def rms_norm(x, g):
    x32 = x.astype(jnp.float32)
    y = x32 * lax.rsqrt(jnp.mean(x32 * x32, axis=-1, keepdims=True) + EPS)
    return (y * g.astype(jnp.float32)).astype(x.dtype)


def modulate(h, shift, scale):
    return h * (1.0 + scale[:, None, :]) + shift[:, None, :]


def axial_rope_tables(S):
    rows = S // GRID_W
    row = jnp.repeat(jnp.arange(rows, dtype=jnp.float32), GRID_W)
    col = jnp.tile(jnp.arange(GRID_W, dtype=jnp.float32), rows)
    inv = 1.0 / (ROPE_THETA ** (jnp.arange(0, ROPE_AXIS_DIM, 2, dtype=jnp.float32) / ROPE_AXIS_DIM))
    ang_r = row[:, None] * inv[None, :]
    ang_c = col[:, None] * inv[None, :]
    return jnp.cos(ang_r), jnp.sin(ang_r), jnp.cos(ang_c), jnp.sin(ang_c)


def rope_half(x, cos, sin):
    n = x.shape[-1] // 2
    x1, x2 = x[..., :n], x[..., n:]
    cos = cos[None, :, None, :]
    sin = sin[None, :, None, :]
    return jnp.concatenate([x1 * cos - x2 * sin, x2 * cos + x1 * sin], axis=-1)


def apply_axial_rope(x, tables):
    cr, sr, cc, sc = tables
    x32 = x.astype(jnp.float32)
    out = jnp.concatenate([rope_half(x32[..., :ROPE_AXIS_DIM], cr, sr),
                           rope_half(x32[..., ROPE_AXIS_DIM:], cc, sc)], axis=-1)
    return out.astype(x.dtype)


def t5_bucket(rel):
    nb = N_BUCKETS // 2
    ret = jnp.where(rel > 0, nb, 0)
    n = jnp.abs(rel)
    max_exact = nb // 2
    nf = jnp.maximum(n, 1).astype(jnp.float32)
    large = max_exact + (jnp.log(nf / max_exact) / math.log(MAX_DISTANCE / max_exact)
                         * (nb - max_exact)).astype(jnp.int32)
    large = jnp.minimum(large, nb - 1)
    return ret + jnp.where(n < max_exact, n, large)


def global_axial_gqa(q, k, v):
    B, S, Hq, d = q.shape
    Hkv = k.shape[2]
    G = Hq // Hkv
    nblk = S // BLOCK
    scale = 1.0 / math.sqrt(d)
    qb = q.reshape(B, nblk, BLOCK, Hkv, G, d).transpose(1, 0, 3, 4, 2, 5)
    kt = k.transpose(0, 2, 1, 3)
    vt = v.transpose(0, 2, 1, 3)

    def one_block(qi):
        s = jnp.einsum('bhgqd,bhkd->bhgqk', qi, kt).astype(jnp.float32) * scale
        p = jax.nn.softmax(s, axis=-1)
        return jnp.einsum('bhgqk,bhkd->bhgqd', p.astype(vt.dtype), vt)

    o = lax.map(one_block, qb)
    return o.transpose(1, 0, 4, 2, 3, 5).reshape(B, S, Hq * d)


def windowed_sink_gqa(q, k, v, rel_bias, sink):
    B, S, Hq, d = q.shape
    Hkv = k.shape[2]
    G = Hq // Hkv
    nblk = S // BLOCK
    scale = 1.0 / math.sqrt(d)
    qb = q.reshape(B, nblk, BLOCK, Hkv, G, d).transpose(0, 3, 4, 1, 2, 5)

    def band(t):
        tp = jnp.pad(t.transpose(0, 2, 1, 3), ((0, 0), (0, 0), (BLOCK, BLOCK), (0, 0)))
        tb = tp.reshape(B, Hkv, nblk + 2, BLOCK, d)
        return jnp.concatenate([tb[:, :, :-2], tb[:, :, 1:-1], tb[:, :, 2:]], axis=3)

    kw, vw = band(k), band(v)
    m = jnp.arange(3 * BLOCK)
    j = jnp.arange(BLOCK)
    rel = m[None, :] - BLOCK - j[:, None]
    kpos = jnp.arange(nblk)[:, None, None] * BLOCK - BLOCK + m[None, None, :]
    valid = (jnp.abs(rel) <= WINDOW)[None] & (kpos >= 0) & (kpos < S)
    bias = rel_bias.astype(jnp.float32)[t5_bucket(rel)]
    bias = bias.transpose(2, 0, 1).reshape(Hkv, G, BLOCK, 3 * BLOCK)

    s = jnp.einsum('bhgnqd,bhnkd->bhgnqk', qb, kw).astype(jnp.float32) * scale
    s = s + bias[None, :, :, None, :, :]
    s = jnp.where(valid[None, None, None], s, -1e30)
    sink_col = jnp.broadcast_to(sink.astype(jnp.float32).reshape(1, Hkv, G, 1, 1, 1),
                                s.shape[:-1] + (1,))
    p = jax.nn.softmax(jnp.concatenate([s, sink_col], axis=-1), axis=-1)[..., :-1]
    o = jnp.einsum('bhgnqk,bhnkd->bhgnqd', p.astype(vw.dtype), vw)
    return o.transpose(0, 3, 4, 1, 2, 5).reshape(B, S, Hq * d)


def expert_choice_moe(h, w_router, w_gate, w_up, w_down):
    B, S, D = h.shape
    C = CAPACITY_FACTOR * S // N_EXPERTS
    logits = jnp.einsum('bsd,de->bse', h, w_router).astype(jnp.float32)
    aff = jax.nn.softmax(logits, axis=-1)
    g, idx = lax.top_k(aff.transpose(0, 2, 1), C)
    xg = jax.vmap(lambda hb, ib: hb[ib])(h, idx)
    a = jnp.einsum('becd,edf->becf', xg, w_gate)
    u = jnp.einsum('becd,edf->becf', xg, w_up)
    y = jnp.einsum('becf,efd->becd', jax.nn.silu(a) * u, w_down)
    y = y * g[..., None].astype(y.dtype)
    return jax.vmap(lambda yb, ib: jnp.zeros((S, D), yb.dtype).at[ib.reshape(-1)].add(yb.reshape(-1, D)))(y, idx)


def setup_inputs(seed: int = 0) -> dict:
    key = jax.random.key(seed)
    ks = jax.random.split(key, 20)
    f32 = jnp.float32
    nrm = lambda k, shape, s: jax.random.normal(k, shape, f32) * s
    return {
        "x": nrm(ks[0], (BATCH, SEQ, D_MODEL), 1.0),
        "c": nrm(ks[1], (BATCH, D_MODEL), 1.0),
        "w_ada": nrm(ks[2], (D_MODEL, 6 * D_MODEL), 0.5 * D_MODEL ** -0.5),
        "b_ada": nrm(ks[3], (6 * D_MODEL,), 0.02),
        "g_norm1": 1.0 + nrm(ks[4], (D_MODEL,), 0.02),
        "w_in": nrm(ks[5], (D_MODEL, D_IN), D_MODEL ** -0.5),
        "qn_a": 1.0 + nrm(ks[6], (HEAD_DIM,), 0.02),
        "kn_a": 1.0 + nrm(ks[7], (HEAD_DIM,), 0.02),
        "qn_b": 1.0 + nrm(ks[8], (HEAD_DIM,), 0.02),
        "kn_b": 1.0 + nrm(ks[9], (HEAD_DIM,), 0.02),
        "sink_b": nrm(ks[10], (B_Q_HEADS,), 0.5),
        "rel_bias": nrm(ks[11], (N_BUCKETS, B_Q_HEADS), 0.1),
        "w_out": nrm(ks[12], (D_MIX, D_MODEL), D_MIX ** -0.5),
        "g_norm2": 1.0 + nrm(ks[13], (D_MODEL,), 0.02),
        "w_router": nrm(ks[14], (D_MODEL, N_EXPERTS), D_MODEL ** -0.5),
        "w_gate": nrm(ks[15], (N_EXPERTS, D_MODEL, D_EXPERT), D_MODEL ** -0.5),
        "w_up": nrm(ks[16], (N_EXPERTS, D_MODEL, D_EXPERT), D_MODEL ** -0.5),
        "w_down": nrm(ks[17], (N_EXPERTS, D_EXPERT, D_MODEL), D_EXPERT ** -0.5),
    }


def reference(x, c, w_ada, b_ada, g_norm1, w_in, qn_a, kn_a, qn_b, kn_b, sink_b,
              rel_bias, w_out, g_norm2, w_router, w_gate, w_up, w_down):
    B, S, D = x.shape
    tables = axial_rope_tables(S)
    oq = A_Q_HEADS * HEAD_DIM
    okv = A_KV_HEADS * HEAD_DIM
    bq = B_Q_HEADS * HEAD_DIM
    bkv = B_KV_HEADS * HEAD_DIM
    for _ in range(DEPTH):
        mod = jnp.einsum('bd,de->be', jax.nn.silu(c), w_ada) + b_ada
        sh1, sc1, gt1, sh2, sc2, gt2 = jnp.split(mod, 6, axis=-1)

        h = modulate(rms_norm(x, g_norm1), sh1, sc1)
        z = jnp.einsum('bsd,de->bse', h, w_in)
        qa = z[..., :oq].reshape(B, S, A_Q_HEADS, HEAD_DIM)
        ka = z[..., oq:oq + okv].reshape(B, S, A_KV_HEADS, HEAD_DIM)
        va = z[..., oq + okv:QKV_A].reshape(B, S, A_KV_HEADS, HEAD_DIM)
        zb = z[..., QKV_A:]
        qb = zb[..., :bq].reshape(B, S, B_Q_HEADS, HEAD_DIM)
        kb = zb[..., bq:bq + bkv].reshape(B, S, B_KV_HEADS, HEAD_DIM)
        vb = zb[..., bq + bkv:].reshape(B, S, B_KV_HEADS, HEAD_DIM)

        qa = apply_axial_rope(rms_norm(qa, qn_a), tables)
        ka = apply_axial_rope(rms_norm(ka, kn_a), tables)
        oa = global_axial_gqa(qa, ka, va)

        qb = rms_norm(qb, qn_b)
        kb = rms_norm(kb, kn_b)
        ob = windowed_sink_gqa(qb, kb, vb, rel_bias, sink_b)

        mix = jnp.einsum('bse,ed->bsd', jnp.concatenate([oa, ob], axis=-1), w_out)
        x = x + gt1[:, None, :] * mix

        h2 = modulate(rms_norm(x, g_norm2), sh2, sc2)
        x = x + gt2[:, None, :] * expert_choice_moe(h2, w_router, w_gate, w_up, w_down)
    return x
```

```python
import contextlib
import math

import numpy as np

import concourse.bass as bass
import concourse.mybir as mybir
from concourse.bass_utils import run_bass_kernel_spmd

F32 = mybir.dt.float32
BF16 = mybir.dt.bfloat16
I32 = mybir.dt.int32
AF = mybir.ActivationFunctionType
ALU = mybir.AluOpType
AX = mybir.AxisListType

S = 4096
D = 2048
NT = S // 128
NE = 16
CAP = 512
EPS = 1e-6
SCALE = 1.0 / math.sqrt(128.0)
N_CORES = 8


class Buf:
    __slots__ = ("name", "w", "r", "dsem")

    def __init__(self, name):
        self.name = name
        self.w = None
        self.r = {}
        self.dsem = None


class Sched:
    LIMIT = 30000

    def __init__(self, nc, es):
        self.nc = nc
        self.es = es
        self.eng = {"pe": nc.tensor, "act": nc.scalar, "dve": nc.vector,
                    "pool": nc.gpsimd, "sp": nc.sync}
        self.sem = {}
        self.cnt = {}
        self.nsem = 0
        self.waited = {e: {} for e in self.eng}
        self.dbufs = []
        self.retired = []
        for e in self.eng:
            self._newsem(e)

    def _mk(self, tag):
        self.nsem += 1
        return self.es.enter_context(self.nc.semaphore(f"s{self.nsem}_{tag}"))

    def _newsem(self, e):
        if e in self.sem and self.cnt[e] > 0:
            self.retired.append((self.sem[e][0], self.sem[e][1], self.cnt[e], e))
        self.sem[e] = (self._mk(e), self.nsem)
        self.cnt[e] = 0

    def _wait(self, e, tok):
        if tok is None:
            return
        s, key, v, src = tok
        if src == "pe" and e == "pe":
            return
        if self.waited[e].get(key, 0) >= v:
            return
        self.eng[e].wait_ge(s, v)
        self.waited[e][key] = v

    def _deps(self, e, reads, writes):
        for b in reads:
            self._wait(e, b.w)
        for b in writes:
            self._wait(e, b.w)
            for t in b.r.values():
                self._wait(e, t)

    def _mark(self, tok, reads, writes):
        for b in reads:
            b.r[tok[1]] = tok
        for b in writes:
            b.w = tok
            b.r = {}

    def op(self, e, fn, reads=(), writes=()):
        self._deps(e, reads, writes)
        if self.cnt[e] >= self.LIMIT:
            self._newsem(e)
        ins = fn(self.eng[e])
        self.cnt[e] += 1
        s, key = self.sem[e]
        ins.then_inc(s, 1)
        tok = (s, key, self.cnt[e], e)
        self._mark(tok, reads, writes)
        return tok

    def dma(self, e, chan, fn, reads=(), writes=()):
        self._deps(e, reads, writes)
        owner = writes[0] if len(writes) else reads[0]
        if owner.dsem is None:
            owner.dsem = [self._mk("d_" + owner.name), self.nsem, 0]
            self.dbufs.append(owner)
        c = owner.dsem
        ins = fn(self.eng[e])
        c[2] += 1
        ins.then_inc(c[0], 16)
        tok = (c[0], c[1], 16 * c[2], "dma")
        self._mark(tok, reads, writes)
        return tok

    def all_tokens(self):
        toks = list(self.retired)
        for e in self.eng:
            if self.cnt[e] > 0:
                s, key = self.sem[e]
                toks.append((s, key, self.cnt[e], e))
        for bf in self.dbufs:
            c = bf.dsem
            toks.append((c[0], c[1], 16 * c[2], "dma"))
        return toks

    def barrier(self):
        toks = self.all_tokens()
        for e in self.eng:
            for t in toks:
                if t[3] == e:
                    continue
                self._wait(e, t)


class Ctx:
    pass


def _pool(cx, es):
    nc = cx.nc
    n = [0]

    def sb(shape, dt, name=None):
        n[0] += 1
        cx.uid += 1
        return es.enter_context(nc.sbuf_tensor(f"{name or 't'}_{cx.uid}", list(shape), dt))

    def ps(shape, dt, name=None):
        cx.uid += 1
        return es.enter_context(nc.psum_tensor(f"{name or 'p'}_{cx.uid}", list(shape), dt))

    return sb, ps


def phase_consts(cx, es):
    nc, sc, dr = cx.nc, cx.sc, cx.dr
    sb, ps = _pool(cx, es)
    cx.ident_bf = sb([128, 128], BF16, "identbf")
    cx.ident_f = sb([128, 128], F32, "identf")
    cx.ones_bf = sb([128, 128], BF16, "onesbf")
    cx.b_const = Buf("consts")
    sc.dma("pool", "cst", lambda g: g.dma_start(out=cx.ident_bf[:], in_=dr["ident"]), writes=[cx.b_const])
    sc.dma("sp", "cst", lambda g: g.dma_start(out=cx.ident_f[:], in_=dr["ident"]), writes=[cx.b_const])
    sc.op("dve", lambda v: v.memset(cx.ones_bf[:], 1.0), writes=[cx.b_const])
    cx.A1col = sb([128, 16], F32, "A1col")
    cx.B1col = sb([128, 16], F32, "B1col")
    cx.b_ab1 = Buf("ab1")
    cx.epsc = sb([128, 1], F32, "epsc")
    sc.op("dve", lambda v: v.memset(cx.epsc[:], EPS), writes=[cx.b_const])


def phase_A(cx):
    nc, sc, dr = cx.nc, cx.sc, cx.dr
    with contextlib.ExitStack() as es:
        sb, ps = _pool(cx, es)
        cT = sb([128, 16], F32)
        sT = sb([128, 16], F32)
        g1T = sb([128, 16], F32)
        g2T = sb([128, 16], F32)
        wblk = [sb([128, 16, 512], BF16, "wada") for _ in range(3)]
        modT = sb([128, 96], F32)
        m2 = sb([128, 96, 2], F32)
        s2 = sb([128, 16, 2], BF16)
        sres = sb([128, 16], F32)
        bT96 = sb([96, 128], F32)
        v4 = sb([128, 96], F32)
        v4T = sb([96, 128], F32)
        modp = ps([128, 96, 2], F32)
        bTp = ps([128, 96], F32)
        v4p = ps([96, 128], F32)
        b_small = Buf("small")
        b_w = [Buf("wada0"), Buf("wada1"), Buf("wada2")]
        b_modp = Buf("modp")
        b_modT = Buf("modT")
        b_bTp = Buf("bTp")
        b_v4 = Buf("v4")
        b_v4p = Buf("v4p")
        b_v4T = Buf("v4T")
        sc.dma("sp", "ld", lambda g: g.dma_start(out=cT[:], in_=dr["cT"]), writes=[b_small])
        sc.dma("sp", "ld", lambda g: g.dma_start(out=g1T[:], in_=dr["g1T"]), writes=[b_small])
        sc.dma("sp", "ld", lambda g: g.dma_start(out=g2T[:], in_=dr["g2T"]), writes=[b_small])
        sc.dma("sp", "ld", lambda g: g.dma_start(out=bT96[:], in_=dr["b_ada"].rearrange("o (n p) -> (o n) p", p=128)), writes=[b_small])
        sc.op("act", lambda a: a.activation(out=sT[:], in_=cT[:], func=AF.Silu), reads=[b_small], writes=[b_small])
        sc.op("dve", lambda v: v.tensor_copy(out=s2[:, :, 0], in_=sT[:]), reads=[b_small], writes=[b_small])
        sc.op("dve", lambda v: v.tensor_tensor(out=sres[:], in0=sT[:], in1=s2[:, :, 0], op=ALU.subtract), reads=[b_small], writes=[b_small])
        sc.op("dve", lambda v: v.tensor_copy(out=s2[:, :, 1], in_=sres[:]), reads=[b_small], writes=[b_small])
        sc.op("pe", lambda t: t.transpose(bTp[:], bT96[:], cx.ident_f[0:96, 0:96]), reads=[b_small, cx.b_const], writes=[b_bTp])
        wv = dr["w_ada"].rearrange("(kc p) n -> p kc n", p=128)
        for nb in range(24):
            slot = nb % 3
            w = wblk[slot]
            sc.dma("pool", "wst", lambda g, w=w, nb=nb: g.dma_start(out=w[:], in_=wv[:, :, nb * 512:(nb + 1) * 512]),
                   writes=[b_w[slot]])
            for j in range(4):
                n = nb * 4 + j
                for kc in range(16):
                    sc.op("pe", lambda t, w=w, j=j, kc=kc, n=n: t.matmul(
                        modp[:, n, :], lhsT=w[:, kc, j * 128:(j + 1) * 128], rhs=s2[:, kc, :],
                        start=(kc == 0), stop=(kc == 15)), reads=[b_w[slot], b_small], writes=[b_modp])
        sc.op("dve", lambda v: v.tensor_copy(out=m2[:], in_=modp[:]), reads=[b_modp], writes=[b_modT])
        sc.op("dve", lambda v: v.tensor_tensor(out=modT[:], in0=m2[:, :, 0], in1=m2[:, :, 1], op=ALU.add), reads=[b_modT], writes=[b_modT])
        sc.op("dve", lambda v: v.tensor_tensor(out=modT[:], in0=modT[:], in1=bTp[:], op=ALU.add), reads=[b_modT, b_bTp], writes=[b_modT])
        sc.op("dve", lambda v: v.scalar_tensor_tensor(out=cx.A1col[:], in0=modT[:, 16:32], scalar=1.0, in1=g1T[:],
                                                       op0=ALU.add, op1=ALU.mult),
              reads=[b_modT, b_small], writes=[cx.b_ab1])
        sc.op("dve", lambda v: v.tensor_copy(out=cx.B1col[:], in_=modT[:, 0:16]), reads=[b_modT], writes=[cx.b_ab1])
        sc.op("dve", lambda v: v.scalar_tensor_tensor(out=v4[:, 0:16], in0=modT[:, 64:80], scalar=1.0, in1=g2T[:],
                                                       op0=ALU.add, op1=ALU.mult),
              reads=[b_modT, b_small], writes=[b_v4])
        sc.op("dve", lambda v: v.tensor_copy(out=v4[:, 16:32], in_=modT[:, 48:64]), reads=[b_modT], writes=[b_v4])
        sc.op("dve", lambda v: v.tensor_copy(out=v4[:, 32:48], in_=modT[:, 32:48]), reads=[b_modT], writes=[b_v4])
        sc.op("dve", lambda v: v.tensor_copy(out=v4[:, 48:64], in_=modT[:, 80:96]), reads=[b_modT], writes=[b_v4])
        sc.op("dve", lambda v: v.tensor_copy(out=v4[:, 64:80], in_=cx.A1col[:]), reads=[cx.b_ab1], writes=[b_v4])
        sc.op("dve", lambda v: v.tensor_copy(out=v4[:, 80:96], in_=cx.B1col[:]), reads=[cx.b_ab1], writes=[b_v4])
        sc.op("pe", lambda t: t.transpose(v4p[:], v4[:], cx.ident_f[:]), reads=[b_v4, cx.b_const], writes=[b_v4p])
        sc.op("dve", lambda v: v.tensor_copy(out=v4T[:], in_=v4p[:]), reads=[b_v4p], writes=[b_v4T])
        sc.dma("sp", "st", lambda g: g.dma_start(out=dr["vec_d"].rearrange("j (c p) -> (j c) p", p=128), in_=v4T[:]),
               reads=[b_v4T], writes=[cx.b_vec])
        if cx.dbg is not None and "dbg_mod" in cx.dbg:
            sc.dma("sp", "st", lambda g: g.dma_start(out=cx.dbg["dbg_mod"], in_=modT[:]), reads=[b_modT])
        sc.barrier()


def _head_cols():
    cols = []
    for h in range(8):
        cols.append(("qa", h, h * 128))
    for h in range(2):
        cols.append(("ka", h, 1024 + h * 128))
    for h in range(8):
        cols.append(("qb", h, 1536 + h * 128))
    for h in range(2):
        cols.append(("kb", h, 1536 + 1024 + h * 128))
    return cols


def phase_B(cx):
    nc, sc, dr = cx.nc, cx.sc, cx.dr
    with contextlib.ExitStack() as es:
        sb, ps = _pool(cx, es)
        w_in = sb([128, 16, 3072], BF16, "w_in")
        b_win = Buf("w_in")
        wv = dr["w_in"].rearrange("(kc p) n -> p kc n", p=128)
        for i in range(6):
            sc.dma("pool", "wst", lambda g, i=i: g.dma_start(out=w_in[:, :, i * 512:(i + 1) * 512],
                                                            in_=wv[:, :, i * 512:(i + 1) * 512]), writes=[b_win])
        ab1A = sb([128, 2048], F32, "ab1A")
        ab1B = sb([128, 2048], BF16, "ab1B")
        b_ab1bc = Buf("ab1bc")
        sc.dma("sp", "cst", lambda g: g.dma_start(out=ab1A[:], in_=dr["vec_d"][4:5, :].partition_broadcast(128)),
               reads=[cx.b_vec], writes=[b_ab1bc])
        sc.dma("pool", "cst", lambda g: g.dma_start(out=ab1B[:], in_=dr["vec_d"][5:6, :].partition_broadcast(128)),
               reads=[cx.b_vec], writes=[b_ab1bc])
        rotT = sb([128, 128], BF16, "rotT")
        gains = sb([128, 4], F32, "gains")
        b_c = Buf("bconst")
        sc.dma("pool", "cst", lambda g: g.dma_start(out=rotT[:], in_=dr["rotT"]), writes=[b_c])
        sc.dma("sp", "cst", lambda g: g.dma_start(out=gains[:], in_=dr["gains"]), writes=[b_c])
        xt = [sb([128, 2048], F32, "xt") for _ in range(2)]
        b_xt = [Buf("xt0"), Buf("xt1")]
        xs = [sb([128, 2048], BF16, "xs") for _ in range(2)]
        b_xs = [Buf("xs0"), Buf("xs1")]
        ss = [sb([128, 1], F32, "ss") for _ in range(2)]
        b_ss = [Buf("ss0"), Buf("ss1")]
        hT = [sb([128, 16, 512], BF16, "hT") for _ in range(2)]
        b_hT = [Buf("hT0"), Buf("hT1")]
        cs = [sb([128, 2, 512], F32, "cs") for _ in range(2)]
        b_cs = [Buf("cs0"), Buf("cs1")]
        NS = 3
        sq = [sb([128, 512], BF16, "sq") for _ in range(NS)]
        zg = [sb([128, 512], BF16, "zg") for _ in range(NS)]
        rs = [sb([128, 512], F32, "rs") for _ in range(NS)]
        t1 = [sb([128, 512], F32, "t1") for _ in range(NS)]
        t2 = [sb([128, 512], F32, "t2") for _ in range(NS)]
        qf = [sb([128, 512], BF16, "qf") for _ in range(NS)]
        vsb = [sb([128, 512], BF16, "vsb") for _ in range(2)]
        b_sq = [Buf(f"sq{i}") for i in range(NS)]
        b_zg = [Buf(f"zg{i}") for i in range(NS)]
        b_rs = [Buf(f"rs{i}") for i in range(NS)]
        b_t1 = [Buf(f"t1{i}") for i in range(NS)]
        b_t2 = [Buf(f"t2{i}") for i in range(NS)]
        b_qf = [Buf(f"qf{i}") for i in range(NS)]
        b_vsb = [Buf("v0"), Buf("v1")]
        ptr = [ps([128, 8, 128], BF16, "ptr") for _ in range(2)]
        b_ptr = [Buf("ptr0"), Buf("ptr1")]
        zq = [ps([128, 512], F32, "zq") for _ in range(2)]
        b_zq = [Buf("zq0"), Buf("zq1")]
        ssp = [ps([128, 512], F32, "ssp") for _ in range(2)]
        b_ssp = [Buf("ssp0"), Buf("ssp1")]
        rotp = [ps([128, 512], F32, "rotp") for _ in range(2)]
        b_rotp = [Buf("rotp0"), Buf("rotp1")]
        heads = _head_cols()
        vcols = [1280, 1408, 2816, 2944]

        def load_cs(ci):
            hs = ci % 2
            t0 = ci * 512
            sc.dma("sp", "ld", lambda g: g.dma_start(out=cs[hs][:, 0, :], in_=dr["cosT"][:, t0:t0 + 512]), writes=[b_cs[hs]])
            sc.dma("sp", "ld", lambda g: g.dma_start(out=cs[hs][:, 1, :], in_=dr["sinT"][:, t0:t0 + 512]), writes=[b_cs[hs]])

        def front_a(ti):
            ci, tt = ti // 4, ti % 4
            hs = ci % 2
            xsl = ti % 2
            if tt == 0:
                load_cs(ci)
            sc.dma("sp", "ld", lambda g: g.dma_start(out=xt[xsl][:], in_=dr["x"][ti * 128:(ti + 1) * 128, :]), writes=[b_xt[xsl]])
            sc.op("act", lambda a: a.activation(out=xs[xsl][:], in_=xt[xsl][:], func=AF.Square, accum_out=ss[xsl][:]),
                  reads=[b_xt[xsl]], writes=[b_xs[xsl], b_ss[xsl]])
            sc.op("act", lambda a: a.activation(out=ss[xsl][:], in_=ss[xsl][:], func=AF.Ln, scale=1.0 / D, bias=cx.epsc[:]),
                  reads=[b_ss[xsl], cx.b_const], writes=[b_ss[xsl]])
            sc.op("act", lambda a: a.activation(out=ss[xsl][:], in_=ss[xsl][:], func=AF.Exp, scale=-0.5),
                  reads=[b_ss[xsl]], writes=[b_ss[xsl]])
            sc.op("dve", lambda v: v.scalar_tensor_tensor(out=xt[xsl][:], in0=xt[xsl][:], scalar=ss[xsl][:, 0:1], in1=ab1A[:],
                                                          op0=ALU.mult, op1=ALU.mult),
                  reads=[b_xt[xsl], b_ss[xsl], b_ab1bc], writes=[b_xt[xsl]])
            sc.op("dve", lambda v: v.tensor_tensor(out=xs[xsl][:], in0=xt[xsl][:], in1=ab1B[:], op=ALU.add),
                  reads=[b_xt[xsl], b_ab1bc], writes=[b_xs[xsl]])

        def front_b(ti):
            ci, tt = ti // 4, ti % 4
            hs = ci % 2
            xsl = ti % 2
            for half in range(2):
                pb = half
                for c8 in range(8):
                    c = half * 8 + c8
                    sc.op("pe", lambda t, c8=c8, c=c: t.transpose(ptr[pb][:, c8, :], xs[xsl][:, c * 128:(c + 1) * 128], cx.ident_bf[:]),
                          reads=[b_xs[xsl], cx.b_const], writes=[b_ptr[pb]])
                sc.op("act", lambda a: a.copy(out=hT[hs][:, half * 8:(half + 1) * 8, tt * 128:(tt + 1) * 128], in_=ptr[pb][:]),
                      reads=[b_ptr[pb]], writes=[b_hT[hs]])

        def head1(ci, k):
            kind, h, col = heads[k]
            hs = ci % 2
            zl = k % 2
            sl = k % NS
            for kc in range(16):
                sc.op("pe", lambda t, kc=kc: t.matmul(zq[zl][:], lhsT=w_in[:, kc, col:col + 128], rhs=hT[hs][:, kc, :],
                                                      start=(kc == 0), stop=(kc == 15)),
                      reads=[b_win, b_hT[hs]], writes=[b_zq[zl]])
            gi = {"qa": 0, "ka": 1, "qb": 2, "kb": 3}[kind]
            sc.op("act", lambda a: a.activation(out=sq[sl][:], in_=zq[zl][:], func=AF.Square), reads=[b_zq[zl]], writes=[b_sq[sl]])
            sc.op("act", lambda a: a.activation(out=zg[sl][:], in_=zq[zl][:], func=AF.Identity, scale=gains[:, gi:gi + 1]),
                  reads=[b_zq[zl], b_c], writes=[b_zg[sl]])

        def head2(ci, k):
            kind, h, col = heads[k]
            hs = ci % 2
            zl = k % 2
            sl = k % NS
            t0 = ci * 512
            sc.op("pe", lambda t: t.matmul(ssp[zl][:], lhsT=cx.ones_bf[:], rhs=sq[sl][:], start=True, stop=True),
                  reads=[b_sq[sl], cx.b_const], writes=[b_ssp[zl]])
            sc.op("act", lambda a: a.activation(out=rs[sl][:], in_=ssp[zl][:], func=AF.Ln, scale=1.0 / 128.0, bias=cx.epsc[:]),
                  reads=[b_ssp[zl], cx.b_const], writes=[b_rs[sl]])
            sc.op("act", lambda a: a.activation(out=rs[sl][:], in_=rs[sl][:], func=AF.Exp, scale=-0.5), reads=[b_rs[sl]], writes=[b_rs[sl]])
            if kind in ("qa", "ka"):
                sc.op("pe", lambda t: t.matmul(rotp[zl][:], lhsT=rotT[:], rhs=zg[sl][:], start=True, stop=True),
                      reads=[b_zg[sl], b_c], writes=[b_rotp[zl]])
                sc.op("pool", lambda g: g.tensor_tensor(out=t1[sl][:], in0=zg[sl][:], in1=cs[hs][:, 0, :], op=ALU.mult),
                      reads=[b_zg[sl], b_cs[hs]], writes=[b_t1[sl]])
                sc.op("dve", lambda v: v.tensor_tensor(out=t2[sl][:], in0=rotp[zl][:], in1=cs[hs][:, 1, :], op=ALU.mult),
                      reads=[b_rotp[zl], b_cs[hs]], writes=[b_t2[sl]])
                sc.op("dve", lambda g: g.tensor_tensor(out=t1[sl][:], in0=t1[sl][:], in1=t2[sl][:], op=ALU.add),
                      reads=[b_t1[sl], b_t2[sl]], writes=[b_t1[sl]])
                sc.op("dve", lambda v: v.tensor_tensor(out=qf[sl][:], in0=t1[sl][:], in1=rs[sl][:], op=ALU.mult),
                      reads=[b_t1[sl], b_rs[sl]], writes=[b_qf[sl]])
            else:
                sc.op("dve", lambda v: v.tensor_tensor(out=qf[sl][:], in0=zg[sl][:], in1=rs[sl][:], op=ALU.mult),
                      reads=[b_zg[sl], b_rs[sl]], writes=[b_qf[sl]])
            if kind[0] == "q":
                hd = h if kind == "qa" else 8 + h
                dst = dr["qT_d"][hd, :, t0:t0 + 512]
                wb = cx.b_qT
            else:
                hd = h if kind == "ka" else 2 + h
                dst = dr["kT_d"][hd, :, t0:t0 + 512]
                wb = cx.b_kT
            sc.dma("sp", "st", lambda g: g.dma_start(out=dst, in_=qf[sl][:]), reads=[b_qf[sl]], writes=[])

        def vunit(ci, tt):
            hs = ci % 2
            ti = ci * 4 + tt
            sl = tt % 2
            for vi, vc in enumerate(vcols):
                for kc in range(16):
                    sc.op("pe", lambda t, kc=kc, vc=vc, vi=vi: t.matmul(
                        zq[sl][:, vi * 128:(vi + 1) * 128], lhsT=hT[hs][:, kc, tt * 128:(tt + 1) * 128],
                        rhs=w_in[:, kc, vc:vc + 128], start=(kc == 0), stop=(kc == 15)),
                        reads=[b_win, b_hT[hs]], writes=[b_zq[sl]])
            sc.op("act", lambda a: a.copy(out=vsb[sl][:], in_=zq[sl][:]), reads=[b_zq[sl]], writes=[b_vsb[sl]])
            sc.dma("sp", "st", lambda g: g.dma_start(out=dr["v_d"][ti * 128:(ti + 1) * 128, :], in_=vsb[sl][:]),
                   reads=[b_vsb[sl]], writes=[])

        front_a(0)
        front_a(1)
        front_b(0)
        front_a(2)
        front_b(1)
        front_a(3)
        front_b(2)
        front_b(3)
        NH = len(heads)
        ntile = cx.nchunk * 4
        for ci in range(cx.nchunk):
            nxt = ci + 1 < cx.nchunk
            if nxt:
                front_a((ci + 1) * 4)
            head1(ci, 0)
            for k in range(1, NH):
                head1(ci, k)
                head2(ci, k - 1)
                if nxt and k in (4, 9, 14, 19):
                    j = (4, 9, 14, 19).index(k)
                    front_b((ci + 1) * 4 + j)
                    if j + 1 < 4:
                        front_a((ci + 1) * 4 + j + 1)
            vunit(ci, 0)
            head2(ci, NH - 1)
            for tt in range(1, 4):
                vunit(ci, tt)
        sc.barrier()


def mask_prepare(cx, es):
    nc, sc, dr = cx.nc, cx.sc, cx.dr
    sb, ps = _pool(cx, es)
    oh = sb([128, 32, 384], BF16, "oh")
    rb = sb([128, 264], F32, "rb")
    erb = sb([128, 264], F32, "erb")
    rbq = sb([128, 256], F32, "rbq")
    negm = sb([128, 384], F32, "negm")
    negfull = sb([128, 128], BF16, "negfull")
    bm = sb([128, 8, 384], F32, "bm")
    bm4 = sb([128, 8, 3, 512], BF16, "bm4")
    b_oh = Buf("oh")
    b_rb = Buf("rb")
    b_bm = [Buf(f"bm{h}") for h in range(8)]
    b_bm4 = [Buf(f"bm4{h}") for h in range(8)]
    sc.dma("pool", "cst", lambda g: g.dma_start(out=oh[:], in_=dr["oh"]), writes=[b_oh])
    sc.dma("sp", "cst", lambda g: g.dma_start(out=negm[:], in_=dr["negm"]), writes=[b_oh])
    sc.dma("sp", "cst", lambda g: g.dma_start(out=rb[:], in_=dr["rbs"].partition_broadcast(128)), writes=[b_rb])
    sc.op("act", lambda a: a.activation(out=erb[:], in_=rb[:], func=AF.Exp), reads=[b_rb], writes=[b_rb])
    sc.op("dve", lambda v: v.tensor_scalar(out=rbq[:], in0=rb[:, 0:256], scalar1=1.0 / SCALE, scalar2=None, op0=ALU.mult),
          reads=[b_rb], writes=[b_rb])
    sc.op("dve", lambda v: v.memset(negfull[:], -30000.0), writes=[b_oh])

    def gen():
        for b in range(32):
            for h in range(cx.nheadB):
                src = negm[:] if b == 0 else bm[:, h, :]
                sc.op("dve", lambda v, h=h, b=b, src=src: v.scalar_tensor_tensor(
                    out=bm[:, h, :], in0=oh[:, b, :], scalar=rbq[:, b * 8 + h:b * 8 + h + 1], in1=src, op0=ALU.mult, op1=ALU.add),
                    reads=[b_oh, b_rb, b_bm[h]], writes=[b_bm[h]])
                yield
        for h in range(cx.nheadB):
            for o in range(3):
                for tt in range(4):
                    sc.op("pool", lambda g, h=h, o=o, tt=tt: g.tensor_copy(out=bm4[:, h, o, tt * 128:(tt + 1) * 128],
                                                                           in_=bm[:, h, o * 128:(o + 1) * 128]),
                          reads=[b_bm[h]], writes=[b_bm4[h]])
                    yield

    g = gen()

    def step():
        try:
            next(g)
            return True
        except StopIteration:
            return False

    def drain():
        while step():
            pass

    cx.mask_step = step
    cx.mask_drain = drain
    cx.m_oh, cx.m_negm, cx.m_erb, cx.m_negfull, cx.m_bm4 = oh, negm, erb, negfull, bm4
    cx.mb_oh, cx.mb_rb, cx.mb_bm4 = b_oh, b_rb, b_bm4


def phase_C(cx):
    nc, sc, dr = cx.nc, cx.sc, cx.dr
    with contextlib.ExitStack() as es:
        sb, ps = _pool(cx, es)
        kT = sb([128, 2, S], BF16, "kTa")
        va = sb([128, NT, 256], BF16, "va")
        ones_f = sb([128, 128], F32, "ones_fC")
        b_kv = Buf("kva")
        for g_ in range(2):
            sc.dma("sp", "ld", lambda g, g_=g_: g.dma_start(out=kT[:, g_, :], in_=dr["kT_d"][g_, :, :]),
                   reads=[cx.b_kT], writes=[b_kv])
        sc.dma("sp", "ld", lambda g: g.dma_start(out=va[:], in_=dr["v_d"].rearrange("(t p) n -> p t n", p=128)[:, :, 0:256]),
               reads=[cx.b_v], writes=[b_kv])
        sc.op("dve", lambda v: v.memset(ones_f[:], 1.0), writes=[b_kv])
        qc = [sb([128, 512], BF16, "qc") for _ in range(2)]
        b_qc = [Buf("qc0"), Buf("qc1")]
        NPT = 3
        pt = [sb([128, 2, 512], BF16, "pt") for _ in range(NPT)]
        b_pt = [Buf(f"pt{i}") for i in range(NPT)]
        accD = [sb([128, 2, 512], F32, "accD") for _ in range(2)]
        accP = [sb([128, 2, 512], F32, "accP") for _ in range(2)]
        b_accD = [Buf("accD0"), Buf("accD1")]
        b_accP = [Buf("accP0"), Buf("accP1")]
        dsum = sb([128, 512], F32, "dsum")
        dsum2 = sb([128, 512], F32, "dsum2")
        b_dsum, b_dsum2 = Buf("dsum"), Buf("dsum2")
        rden = sb([128, 512], F32, "rden")
        b_rden = Buf("rden")
        osb = [sb([128, 512], BF16, "osb") for _ in range(2)]
        b_osb = [Buf("osb0"), Buf("osb1")]
        st = [ps([128, 2, 512], F32, "st") for _ in range(2)]
        b_st = [Buf("st0"), Buf("st1")]
        ot = [ps([128, 512], F32, "ot") for _ in range(2)]
        b_ot = [Buf("ot0"), Buf("ot1")]
        dn = ps([128, 512], F32, "dn")
        b_dn = Buf("dn")
        NP = NT // 2
        iters = [(hd, ci) for hd in range(cx.nheadA) for ci in range(cx.nchunk)]
        units = [(i, kp) for i in range(len(iters)) for kp in range(NP)]

        def load_q(i):
            hd, ci = iters[i]
            qs = i % 2
            sc.dma("sp", "ld", lambda g: g.dma_start(out=qc[qs][:], in_=dr["qT_d"][hd, :, ci * 512:(ci + 1) * 512]),
                   reads=[cx.b_qT], writes=[b_qc[qs]])

        def emit_st(u):
            i, kp = units[u]
            hd, ci = iters[i]
            g_ = hd // 4
            qs = i % 2
            s_ = u % 2
            if kp == 0:
                load_q(i)
            for j in range(2):
                kt = kp * 2 + j
                sc.op("pe", lambda t, j=j, kt=kt: t.matmul(st[s_][:, j, :], lhsT=kT[:, g_, kt * 128:(kt + 1) * 128], rhs=qc[qs][:],
                                                           start=True, stop=True),
                      reads=[b_kv, b_qc[qs]], writes=[b_st[s_]])

        emit_st(0)
        for u in range(len(units)):
            i, kp = units[u]
            hd, ci = iters[i]
            g_ = hd // 4
            qs = i % 2
            s_ = u % 2
            p_ = u % NPT
            if u + 1 < len(units):
                emit_st(u + 1)
            cx.mask_step()
            sc.op("act", lambda a: a.activation(out=pt[p_][:], in_=st[s_][:], func=AF.Exp, scale=SCALE),
                  reads=[b_st[s_]], writes=[b_pt[p_]])
            for j in range(2):
                kt = kp * 2 + j
                sc.op("pe", lambda t, j=j, kt=kt: t.matmul(ot[qs][:], lhsT=va[:, kt, g_ * 128:(g_ + 1) * 128], rhs=pt[p_][:, j, :],
                                                           start=(kt == 0), stop=(kt == NT - 1)),
                      reads=[b_kv, b_pt[p_]], writes=[b_ot[qs]])
            if kp % 3 == 2:
                for j in range(2):
                    sc.op("pe", lambda t, j=j: t.matmul(dn[:], lhsT=cx.ones_bf[:], rhs=pt[p_][:, j, :],
                                                        start=(kp == 2 and j == 0), stop=False),
                          reads=[cx.b_const, b_pt[p_]], writes=[b_dn])
            else:
                acc, bacc = accD[qs], b_accD[qs]
                if kp == 0:
                    sc.op("dve", lambda v: v.tensor_copy(out=acc[:], in_=pt[p_][:]), reads=[b_pt[p_]], writes=[bacc])
                else:
                    sc.op("dve", lambda v: v.tensor_tensor(out=acc[:], in0=acc[:], in1=pt[p_][:], op=ALU.add),
                          reads=[b_pt[p_], bacc], writes=[bacc])
            if kp == NP - 1:
                sc.op("dve", lambda v: v.tensor_tensor(out=dsum[:], in0=accD[qs][:, 0, :], in1=accD[qs][:, 1, :], op=ALU.add),
                      reads=[b_accD[qs]], writes=[b_dsum])
                sc.op("pe", lambda t: t.matmul(dn[:], lhsT=ones_f[:], rhs=dsum[:], start=False, stop=True),
                      reads=[b_kv, b_dsum], writes=[b_dn])
                sc.op("act", lambda a: a.activation(out=rden[:], in_=dn[:], func=AF.Ln), reads=[b_dn], writes=[b_rden])
                sc.op("act", lambda a: a.activation(out=rden[:], in_=rden[:], func=AF.Exp, scale=-1.0), reads=[b_rden], writes=[b_rden])
                sc.op("dve", lambda v: v.tensor_tensor(out=osb[qs][:], in0=ot[qs][:], in1=rden[:], op=ALU.mult),
                      reads=[b_ot[qs], b_rden], writes=[b_osb[qs]])
                sc.dma("sp", "st", lambda g: g.dma_start(out=dr["mixT_d"][hd, :, ci * 512:(ci + 1) * 512], in_=osb[qs][:]),
                       reads=[b_osb[qs]], writes=[])
        sc.barrier()


def phase_D(cx):
    nc, sc, dr = cx.nc, cx.sc, cx.dr
    with contextlib.ExitStack() as es:
        sb, ps = _pool(cx, es)
        kT = sb([128, 2, S], BF16, "kTb")
        vb = sb([128, NT, 256], BF16, "vb")
        b_kv = Buf("kvb")
        for g_ in range(2):
            sc.dma("sp", "ld", lambda g, g_=g_: g.dma_start(out=kT[:, g_, :], in_=dr["kT_d"][2 + g_, :, :]),
                   reads=[cx.b_kT], writes=[b_kv])
        sc.dma("sp", "ld", lambda g: g.dma_start(out=vb[:], in_=dr["v_d"].rearrange("(t p) n -> p t n", p=128)[:, :, 256:512]),
               reads=[cx.b_v], writes=[b_kv])
        cx.mask_drain()
        oh, negm, erb, negfull, bm4 = cx.m_oh, cx.m_negm, cx.m_erb, cx.m_negfull, cx.m_bm4
        b_oh, b_rb, b_bm4 = cx.mb_oh, cx.mb_rb, cx.mb_bm4
        qh = [sb([128, S], BF16, "qh") for _ in range(2)]
        b_qh = [Buf("qh0"), Buf("qh1")]
        pt = [sb([128, 3, 512], BF16, "ptb") for _ in range(2)]
        b_pt = [[Buf(f"ptb{i}{o}") for o in range(3)] for i in range(2)]
        rden = sb([128, 512], F32, "rdenb")
        b_rden = Buf("rdb")
        osb = [sb([128, 512], BF16, "osbb") for _ in range(2)]
        b_osb = [Buf("osbb0"), Buf("osbb1")]
        st = [[ps([128, 512], F32, "stb") for _ in range(3)] for _ in range(2)]
        b_st = [[Buf(f"stb{i}{o}") for o in range(3)] for i in range(2)]
        ot = ps([128, 512], F32, "otb")
        b_ot = Buf("otb")
        dn = ps([128, 512], F32, "dnb")
        b_dn = Buf("dnb")
        units = [(hd, ci) for hd in range(cx.nheadB) for ci in range(cx.nchunk)]

        def emit_st(u):
            hd, ci = units[u]
            g_ = hd // 4
            hsl = hd % 2
            s_ = u % 2
            if ci == 0:
                sc.dma("sp", "ld", lambda g: g.dma_start(out=qh[hsl][:], in_=dr["qT_d"][8 + hd, :, :]),
                       reads=[cx.b_qT], writes=[b_qh[hsl]])
            for o in range(3):
                mm = [(st[s_][o][:, :], cx.ident_bf[:], bm4[:, hd, o, :], [cx.b_const, b_bm4[hd]])]
                for tt in range(4):
                    qt = ci * 4 + tt
                    kt = qt + o - 1
                    cols = st[s_][o][:, tt * 128:(tt + 1) * 128]
                    if 0 <= kt < NT:
                        mm.append((cols, kT[:, g_, kt * 128:(kt + 1) * 128], qh[hsl][:, qt * 128:(qt + 1) * 128], [b_kv, b_qh[hsl]]))
                    else:
                        mm.append((cols, cx.ident_bf[:], negfull[:], [cx.b_const, b_oh]))
                for i, (o_ap, l_ap, r_ap, rd) in enumerate(mm):
                    sc.op("pe", lambda t, o_ap=o_ap, l_ap=l_ap, r_ap=r_ap, i=i: t.matmul(
                        o_ap, lhsT=l_ap, rhs=r_ap, start=(i == 0), stop=(i == len(mm) - 1)),
                        reads=rd, writes=[b_st[s_][o]])

        emit_st(0)
        for u in range(len(units)):
            hd, ci = units[u]
            g_ = hd // 4
            s_ = u % 2
            if u + 1 < len(units):
                emit_st(u + 1)
            for o in range(3):
                sc.op("act", lambda a, o=o: a.activation(out=pt[s_][:, o, :], in_=st[s_][o][:], func=AF.Exp, scale=SCALE),
                      reads=[b_st[s_][o]], writes=[b_pt[s_][o]])
            for tt in range(4):
                qt = ci * 4 + tt
                offs = [o for o in range(3) if 0 <= qt + o - 1 < NT]
                for i, o in enumerate(offs):
                    kt = qt + o - 1
                    sc.op("pe", lambda t, o=o, kt=kt, i=i, tt=tt: t.matmul(
                        ot[:, tt * 128:(tt + 1) * 128], lhsT=vb[:, kt, g_ * 128:(g_ + 1) * 128],
                        rhs=pt[s_][:, o, tt * 128:(tt + 1) * 128], start=(i == 0), stop=(i == len(offs) - 1)),
                        reads=[b_kv, b_pt[s_][o]], writes=[b_ot])
            for o in range(3):
                sc.op("pe", lambda t, o=o: t.matmul(dn[:], lhsT=cx.ones_bf[:], rhs=pt[s_][:, o, :], start=(o == 0), stop=(o == 2)),
                      reads=[cx.b_const, b_pt[s_][o]], writes=[b_dn])
            t0 = ci * 512
            sc.op("act", lambda a: a.activation(out=rden[:], in_=dn[:], func=AF.Ln, bias=erb[:, 256 + hd:257 + hd]),
                  reads=[b_dn, b_rb], writes=[b_rden])
            sc.op("act", lambda a: a.activation(out=rden[:], in_=rden[:], func=AF.Exp, scale=-1.0), reads=[b_rden], writes=[b_rden])
            sc.op("dve", lambda v: v.tensor_tensor(out=osb[s_][:], in0=ot[:], in1=rden[:], op=ALU.mult),
                  reads=[b_ot, b_rden], writes=[b_osb[s_]])
            sc.dma("sp", "st", lambda g: g.dma_start(out=dr["mixT_d"][8 + hd, :, t0:t0 + 512], in_=osb[s_][:]),
                   reads=[b_osb[s_]], writes=[])
        sc.barrier()


def phase_E(cx, es_outer):
    nc, sc, dr = cx.nc, cx.sc, cx.dr
    sbo, pso = _pool(cx, es_outer)
    cx.logits = sbo([128, NT, NE], F32, "logits")
    cx.b_logits = Buf("logits")
    with contextlib.ExitStack() as es:
        sb, ps = _pool(cx, es)
        w_out = sb([128, 16, 2048], BF16, "w_out")
        b_wout = Buf("w_out")
        bc = sb([128, 3, 2048], F32, "bc")
        b_bc = Buf("bc")
        for j in range(3):
            sc.dma("sp", "cst", lambda g, j=j: g.dma_start(out=bc[:, j, :], in_=dr["vec_d"][j:j + 1, :].partition_broadcast(128)),
                   reads=[cx.b_vec], writes=[b_bc])
        wv = dr["w_out"].rearrange("(kc p) n -> p kc n", p=128)
        for i in range(4):
            sc.dma("pool", "wst", lambda g, i=i: g.dma_start(out=w_out[:, :, i * 512:(i + 1) * 512],
                                                            in_=wv[:, :, i * 512:(i + 1) * 512]), writes=[b_wout])
        for kc in range(16):
            sc.op("dve", lambda v, kc=kc: v.tensor_tensor(out=w_out[:, kc, :], in0=w_out[:, kc, :], in1=bc[:, 2, :], op=ALU.mult),
                  reads=[b_wout, b_bc], writes=[b_wout])
        wr = sb([128, 16, NE], F32, "wr")
        wr_hi = sb([128, 16, NE], BF16, "wr_hi")
        wr_lo = sb([128, 16, NE], BF16, "wr_lo")
        b_wr = Buf("wr")
        sc.dma("sp", "cst", lambda g: g.dma_start(out=wr[:], in_=dr["w_router"].rearrange("(kc p) e -> p kc e", p=128)),
               writes=[b_wr])
        sc.op("dve", lambda v: v.tensor_copy(out=wr_hi[:], in_=wr[:]), reads=[b_wr], writes=[b_wr])
        sc.op("dve", lambda v: v.tensor_tensor(out=wr_lo[:], in0=wr[:], in1=wr_hi[:], op=ALU.subtract), reads=[b_wr], writes=[b_wr])
        mx = [sb([128, 16, 512], BF16, "mixc") for _ in range(2)]
        b_mx = [Buf("mx0"), Buf("mx1")]
        xt = [sb([128, 2048], F32, "xtE") for _ in range(2)]
        b_xt = [Buf("xtE0"), Buf("xtE1")]
        x1 = [sb([128, 2048], F32, "x1") for _ in range(2)]
        b_x1 = [Buf("x10"), Buf("x11")]
        h2f = [sb([128, 2048], F32, "h2f") for _ in range(2)]
        b_h2f = [Buf("h2f0"), Buf("h2f1")]
        hi = [sb([128, 2048], BF16, "h2hi") for _ in range(2)]
        b_hi = [Buf("hi0"), Buf("hi1")]
        lo = [sb([128, 2048], BF16, "h2lo") for _ in range(2)]
        b_lo = [Buf("lo0"), Buf("lo1")]
        hT = [sb([128, 16, 128], BF16, "h2T") for _ in range(2)]
        b_hT = [Buf("h2Thi"), Buf("h2Tlo")]
        ss = [sb([128, 1], F32, "ssE") for _ in range(2)]
        b_ss = [Buf("ssE0"), Buf("ssE1")]
        junk = sb([128, 2048], BF16, "junkE")
        b_junk = Buf("junkE")
        xo = ps([128, 4, 512], F32, "xo")
        b_xo = [Buf(f"xo{i}") for i in range(4)]
        ptr = ps([128, 16, 128], BF16, "ptrE")
        b_ptr = Buf("ptrE")
        lg = ps([128, NE], F32, "lg")
        b_lg = Buf("lg")

        def s1(ti):
            ci, tt = ti // 4, ti % 4
            ms = ci % 2
            sl = ti % 2
            if tt == 0:
                t0 = ci * 512
                sc.dma("sp", "ld", lambda g: g.dma_start(
                    out=mx[ms][:], in_=dr["mixT_d"].rearrange("h p t -> p h t")[:, :, t0:t0 + 512]),
                    reads=[cx.b_mix], writes=[b_mx[ms]])
            sc.dma("sp", "ld", lambda g: g.dma_start(out=xt[sl][:], in_=dr["x"][ti * 128:(ti + 1) * 128, :]), writes=[b_xt[sl]])
            for nb in range(4):
                for hc in range(16):
                    sc.op("pe", lambda t, nb=nb, hc=hc: t.matmul(
                        xo[:, nb, :], lhsT=mx[ms][:, hc, tt * 128:(tt + 1) * 128], rhs=w_out[:, hc, nb * 512:(nb + 1) * 512],
                        start=(hc == 0), stop=(hc == 15)), reads=[b_mx[ms], b_wout], writes=[b_xo[nb]])
                sc.op("dve", lambda v, nb=nb: v.tensor_tensor(
                    out=x1[sl][:, nb * 512:(nb + 1) * 512], in0=xo[:, nb, :], in1=xt[sl][:, nb * 512:(nb + 1) * 512], op=ALU.add),
                    reads=[b_xo[nb], b_xt[sl]], writes=[b_x1[sl]])
            sc.dma("sp", "st", lambda g: g.dma_start(out=dr["out"][ti * 128:(ti + 1) * 128, :], in_=x1[sl][:]),
                   reads=[b_x1[sl]], writes=[])
            sc.op("act", lambda a: a.activation(out=junk[:], in_=x1[sl][:], func=AF.Square, accum_out=ss[sl][:]),
                  reads=[b_x1[sl]], writes=[b_junk, b_ss[sl]])
            sc.op("act", lambda a: a.activation(out=ss[sl][:], in_=ss[sl][:], func=AF.Sqrt, scale=1.0 / D, bias=cx.epsc[:]),
                  reads=[b_ss[sl], cx.b_const], writes=[b_ss[sl]])

        def s2(ti):
            sl = ti % 2
            sc.op("dve", lambda v: v.reciprocal(out=ss[sl][:], in_=ss[sl][:]), reads=[b_ss[sl]], writes=[b_ss[sl]])
            sc.op("dve", lambda v: v.scalar_tensor_tensor(out=h2f[sl][:], in0=x1[sl][:], scalar=ss[sl][:, 0:1], in1=bc[:, 0, :],
                                                          op0=ALU.mult, op1=ALU.mult),
                  reads=[b_x1[sl], b_ss[sl], b_bc], writes=[b_h2f[sl]])
            sc.op("dve", lambda v: v.tensor_tensor(out=h2f[sl][:], in0=h2f[sl][:], in1=bc[:, 1, :], op=ALU.add),
                  reads=[b_h2f[sl], b_bc], writes=[b_h2f[sl]])
            sc.op("act", lambda a: a.copy(out=hi[sl][:], in_=h2f[sl][:]), reads=[b_h2f[sl]], writes=[b_hi[sl]])
            sc.dma("sp", "st", lambda g: g.dma_start(out=dr["h2_d"][ti * 128:(ti + 1) * 128, :], in_=hi[sl][:]),
                   reads=[b_hi[sl]], writes=[])

        def s3(ti):
            sl = ti % 2
            sc.op("dve", lambda v: v.tensor_tensor(out=lo[sl][:], in0=h2f[sl][:], in1=hi[sl][:], op=ALU.subtract),
                  reads=[b_h2f[sl], b_hi[sl]], writes=[b_lo[sl]])
            for part, (src, bsrc) in enumerate(((hi[sl], b_hi[sl]), (lo[sl], b_lo[sl]))):
                for c in range(16):
                    sc.op("pe", lambda t, c=c, src=src: t.transpose(ptr[:, c, :], src[:, c * 128:(c + 1) * 128], cx.ident_bf[:]),
                          reads=[bsrc, cx.b_const], writes=[b_ptr])
                sc.op("act", lambda a, part=part: a.copy(out=hT[part][:], in_=ptr[:]), reads=[b_ptr], writes=[b_hT[part]])
            n_mm = 0
            for (a_, ba, w_) in ((hT[0], b_hT[0], wr_hi), (hT[0], b_hT[0], wr_lo), (hT[1], b_hT[1], wr_hi)):
                for c in range(16):
                    sc.op("pe", lambda t, a_=a_, w_=w_, c=c, n_mm=n_mm: t.matmul(
                        lg[:], lhsT=a_[:, c, :], rhs=w_[:, c, :], start=(n_mm == 0), stop=(n_mm == 47)),
                        reads=[ba, b_wr], writes=[b_lg])
                    n_mm += 1
            sc.op("act", lambda a: a.copy(out=cx.logits[:, ti, :], in_=lg[:]), reads=[b_lg], writes=[cx.b_logits])

        ntile = cx.nchunk * 4
        for i in range(ntile + 2):
            if i < ntile:
                s1(i)
            if 0 <= i - 1 < ntile:
                s2(i - 1)
            if 0 <= i - 2 < ntile:
                s3(i - 2)
        sc.barrier()


def phase_F(cx, es_outer):
    nc, sc, dr = cx.nc, cx.sc, cx.dr
    sbo, pso = _pool(cx, es_outer)
    cx.idx = sbo([128, NE, 4], I32, "idx")
    cx.gsel = sbo([128, NE, 4], F32, "gsel")
    cx.b_sel = Buf("sel")
    NIT = 30
    with contextlib.ExitStack() as es:
        sb, ps = _pool(cx, es)
        triL = sb([128, 128], BF16, "triL")
        iota = sb([128, 512], mybir.dt.float16, "iota")
        tv = sb([128, NT, 2], F32, "tv")
        ones_f = sb([128, 128], F32, "ones_f")
        b_c = Buf("cF")
        sc.dma("pool", "cst", lambda g: g.dma_start(out=triL[:], in_=dr["triL"]), writes=[b_c])
        sc.dma("pool", "cst", lambda g: g.dma_start(out=iota[:], in_=dr["iota512"]), writes=[b_c])
        sc.dma("sp", "cst", lambda g: g.dma_start(out=tv[:], in_=dr["tvals"]), writes=[b_c])
        sc.op("dve", lambda v: v.memset(ones_f[:], 1.0), writes=[b_c])
        mxv = sb([128, NT], F32, "mxv")
        sm = sb([128, NT], F32, "sm")
        aff = sb([128, NE, NT], F32, "aff")
        b_aff = Buf("aff")
        b_mx = Buf("mxv")
        b_sm = Buf("sm")
        lg = cx.logits
        sc.op("dve", lambda v: v.tensor_reduce(out=mxv[:], in_=lg[:], axis=AX.X, op=ALU.max), reads=[cx.b_logits], writes=[b_mx])
        sc.op("dve", lambda v: v.tensor_scalar(out=mxv[:], in0=mxv[:], scalar1=-1.0, scalar2=None, op0=ALU.mult), reads=[b_mx], writes=[b_mx])
        for ti in range(NT):
            sc.op("act", lambda a, ti=ti: a.activation(out=aff[:, :, ti], in_=lg[:, ti, :], func=AF.Exp, bias=mxv[:, ti:ti + 1],
                                                       accum_out=sm[:, ti:ti + 1]),
                  reads=[cx.b_logits, b_mx], writes=[b_aff, b_sm])
        sc.op("dve", lambda v: v.reciprocal(out=sm[:], in_=sm[:]), reads=[b_sm], writes=[b_sm])
        for ti in range(NT):
            sc.op("dve", lambda v, ti=ti: v.tensor_scalar(out=aff[:, :, ti], in0=aff[:, :, ti], scalar1=sm[:, ti:ti + 1], scalar2=None,
                                                          op0=ALU.mult), reads=[b_aff, b_sm], writes=[b_aff])
        lo = sb([128, NE], F32, "lo")
        mid = sb([128, NE], F32, "mid")
        gm = sb([128, NE], F32, "gm")
        cntp = sb([128, NE], F32, "cntp")
        cmp_ = sb([128, NT], F32, "cmp")
        b_lo, b_mid, b_gm, b_cntp, b_cmp = Buf("lo"), Buf("mid"), Buf("gm"), Buf("cntp"), Buf("cmp")
        tot = ps([128, NE], F32, "tot")
        b_tot = Buf("tot")
        sc.op("dve", lambda v: v.memset(lo[:], 0.0), writes=[b_lo])
        for k in range(NIT):
            w = 2.0 ** (-(k + 1))
            sc.op("dve", lambda v, w=w: v.tensor_scalar(out=mid[:], in0=lo[:], scalar1=w, scalar2=None, op0=ALU.add),
                  reads=[b_lo], writes=[b_mid])
            for e in range(NE):
                sc.op("dve", lambda v, e=e: v.tensor_scalar(out=cmp_[:], in0=aff[:, e, :], scalar1=mid[:, e:e + 1], scalar2=None,
                                                            op0=ALU.is_ge, op1=ALU.add, accum_out=cntp[:, e:e + 1]),
                      reads=[b_aff, b_mid], writes=[b_cmp, b_cntp])
            sc.op("pe", lambda t: t.matmul(tot[:], lhsT=ones_f[:], rhs=cntp[:], start=True, stop=True),
                  reads=[b_c, b_cntp], writes=[b_tot])
            sc.op("dve", lambda v: v.scalar_tensor_tensor(out=gm[:], in0=tot[:], scalar=CAP - 0.5, in1=mid[:], op0=ALU.is_gt, op1=ALU.mult),
                  reads=[b_tot, b_mid], writes=[b_gm])
            sc.op("dve", lambda v: v.tensor_tensor(out=lo[:], in0=lo[:], in1=gm[:], op=ALU.max), reads=[b_lo, b_gm], writes=[b_lo])
        maskf = sb([128, NE, NT], F32, "maskf")
        maskb = sb([128, NE, NT], BF16, "maskb")
        inc = sb([128, NE, NT], F32, "inc")
        cmx = sb([128, NE, NT], BF16, "cmx")
        posm = sb([128, NE, NT], F32, "posm")
        zer = sb([128, NT], F32, "zer")
        b_mask, b_inc, b_cmx, b_posm = Buf("mask"), Buf("inc"), Buf("cmx"), Buf("posm")
        sc.op("dve", lambda v: v.memset(zer[:], 0.0), writes=[b_c])
        for e in range(NE):
            sc.op("dve", lambda v, e=e: v.tensor_scalar(out=maskf[:, e, :], in0=aff[:, e, :], scalar1=lo[:, e:e + 1], scalar2=None,
                                                        op0=ALU.is_ge), reads=[b_aff, b_lo], writes=[b_mask])
        sc.op("dve", lambda v: v.tensor_copy(out=maskb[:], in_=maskf[:]), reads=[b_mask], writes=[b_mask])
        for e in range(NE):
            sc.op("dve", lambda v, e=e: v.tensor_tensor_scan(out=inc[:, e, :], data0=maskf[:, e, :], data1=zer[:], initial=0.0,
                                                             op0=ALU.add, op1=ALU.add), reads=[b_mask, b_c], writes=[b_inc])
        sc.op("dve", lambda v: v.tensor_tensor(out=cmx[:], in0=inc[:], in1=maskf[:], op=ALU.subtract), reads=[b_inc, b_mask], writes=[b_cmx])
        posp = ps([128, NE * NT], F32, "posp")
        b_posp = Buf("posp")
        sc.op("pe", lambda t: t.matmul(posp[:], lhsT=triL[:], rhs=maskb[:].rearrange("p e t -> p (e t)"), start=True, stop=False),
              reads=[b_c, b_mask], writes=[b_posp])
        sc.op("pe", lambda t: t.matmul(posp[:], lhsT=cx.ones_bf[:], rhs=cmx[:].rearrange("p e t -> p (e t)"), start=False, stop=True),
              reads=[cx.b_const, b_cmx], writes=[b_posp])
        sc.op("dve", lambda v: v.scalar_tensor_tensor(out=posm[:].rearrange("p e t -> p (e t)"), in0=posp[:], scalar=1.0,
                                                       in1=maskf[:].rearrange("p e t -> p (e t)"), op0=ALU.add, op1=ALU.mult),
              reads=[b_posp, b_mask], writes=[b_posm])
        sc.op("dve", lambda v: v.tensor_scalar(out=posm[:], in0=posm[:], scalar1=-1.0, scalar2=None, op0=ALU.add),
              reads=[b_posm], writes=[b_posm])
        vals = sb([128, NE, NT, 4], BF16, "vals")
        b_vals = Buf("vals")
        afr = sb([128, NE, NT], F32, "afr")
        b_afr = Buf("afr")
        for e in range(NE):
            sc.op("dve", lambda v, e=e: v.tensor_copy(out=vals[:, e, :, 0:2], in_=tv[:]), reads=[b_c], writes=[b_vals])
        sc.op("dve", lambda v: v.tensor_copy(out=vals[:, :, :, 2], in_=aff[:]), reads=[b_aff], writes=[b_vals])
        sc.op("dve", lambda v: v.tensor_tensor(out=afr[:], in0=aff[:], in1=vals[:, :, :, 2], op=ALU.subtract),
              reads=[b_aff, b_vals], writes=[b_afr])
        sc.op("dve", lambda v: v.tensor_copy(out=vals[:, :, :, 3], in_=afr[:]), reads=[b_afr], writes=[b_vals])
        oh = [sb([128, 512], BF16, "ohF") for _ in range(3)]
        b_oh = [Buf(f"ohF{i}") for i in range(3)]
        selp = [ps([128, 4], F32, "selp") for _ in range(4)]
        b_selp = [Buf(f"selp{i}") for i in range(4)]
        idxf = sb([128, 4], F32, "idxf")
        b_idxf = Buf("idxf")
        selsb = sb([128, 4, 4], F32, "selsb")
        b_selsb = Buf("selsb")
        n = 0
        for e in range(cx.nexp):
            sp_ = e % 2
            for ti in range(NT):
                o = n % 3
                n += 1
                sc.op("dve", lambda v, o=o, e=e, ti=ti: v.tensor_scalar(out=oh[o][:], in0=iota[:], scalar1=posm[:, e, ti:ti + 1],
                                                                        scalar2=None, op0=ALU.is_equal),
                      reads=[b_c, b_posm], writes=[b_oh[o]])
                for jt in range(4):
                    sc.op("pe", lambda t, o=o, e=e, ti=ti, jt=jt: t.matmul(
                        selp[jt][:], lhsT=oh[o][:, jt * 128:(jt + 1) * 128], rhs=vals[:, e, ti, :],
                        start=(ti == 0), stop=(ti == NT - 1)), reads=[b_oh[o], b_vals], writes=[b_selp[jt]])
            for jt in range(4):
                sc.op("dve", lambda v, jt=jt: v.tensor_copy(out=selsb[:, jt, :], in_=selp[jt][:]), reads=[b_selp[jt]], writes=[b_selsb])
            sc.op("dve", lambda v: v.scalar_tensor_tensor(out=idxf[:], in0=selsb[:, :, 0], scalar=64.0, in1=selsb[:, :, 1],
                                                          op0=ALU.mult, op1=ALU.add), reads=[b_selsb], writes=[b_idxf])
            sc.op("dve", lambda v: v.tensor_scalar(out=idxf[:], in0=idxf[:], scalar1=0.0, scalar2=float(S - 1), op0=ALU.max, op1=ALU.min),
                  reads=[b_idxf], writes=[b_idxf])
            sc.op("dve", lambda v, e=e: v.tensor_copy(out=cx.idx[:, e, :], in_=idxf[:]), reads=[b_idxf], writes=[cx.b_sel])
            sc.op("dve", lambda v, e=e: v.tensor_tensor(out=cx.gsel[:, e, :], in0=selsb[:, :, 2], in1=selsb[:, :, 3],
                                                        op=ALU.add), reads=[b_selsb], writes=[cx.b_sel])
        if cx.dbg is not None and "dbg_idx" in cx.dbg:
            sc.dma("sp", "st", lambda g: g.dma_start(out=cx.dbg["dbg_idx"], in_=cx.idx[:]), reads=[cx.b_sel])
            sc.dma("sp", "st", lambda g: g.dma_start(out=cx.dbg["dbg_g"], in_=cx.gsel[:]), reads=[cx.b_sel])
            sc.dma("sp", "st", lambda g: g.dma_start(out=cx.dbg["dbg_aff"], in_=aff[:]), reads=[b_aff])
            sc.dma("sp", "st", lambda g: g.dma_start(out=cx.dbg["dbg_lo"], in_=lo[:]), reads=[b_lo])
        sc.barrier()


def phase_G(cx):
    nc, sc, dr = cx.nc, cx.sc, cx.dr
    with contextlib.ExitStack() as es:
        sb, ps = _pool(cx, es)
        gt2 = sb([128, 2048], F32, "gt2bc")
        b_gt2 = Buf("gt2")
        sc.dma("sp", "cst", lambda g: g.dma_start(out=gt2[:], in_=dr["vec_d"][3:4, :].partition_broadcast(128)),
               reads=[cx.b_vec], writes=[b_gt2])
        wg = [sb([128, 16, 512], BF16, "wg") for _ in range(2)]
        wu = [sb([128, 16, 512], BF16, "wu") for _ in range(2)]
        wd = [sb([128, 16, 512], BF16, "wd") for _ in range(2)]
        b_wg = [Buf("wg0"), Buf("wg1")]
        b_wu = [Buf("wu0"), Buf("wu1")]
        b_wd = [Buf("wd0"), Buf("wd1")]
        xg = sb([128, 4, 2048], BF16, "xg")
        b_xg = Buf("xg")
        xgT = sb([128, 16, 512], BF16, "xgT")
        b_xgT = Buf("xgT")
        hT = sb([128, 16, 512], BF16, "hTG")
        b_hT = Buf("hTG")
        sa = [sb([128, 512], F32, "sa") for _ in range(2)]
        b_sa = [Buf("sa0"), Buf("sa1")]
        ysb = sb([128, 4, 2048], F32, "ysb")
        b_ysb = [Buf(f"ysb{j}") for j in range(4)]
        tp = [ps([128, 512], BF16, "tpG") for _ in range(2)]
        b_tp = [Buf("tpG0"), Buf("tpG1")]
        ap_ = [ps([128, 512], F32, "apG") for _ in range(2)]
        up_ = [ps([128, 512], F32, "upG") for _ in range(2)]
        yp = [ps([128, 512], F32, "ypG") for _ in range(2)]
        b_ap = [Buf("ap0"), Buf("ap1")]
        b_up = [Buf("up0"), Buf("up1")]
        b_yp = [Buf("yp0"), Buf("yp1")]
        wgv = dr["w_gate"].rearrange("e (kc p) n -> e p kc n", p=128)
        wuv = dr["w_up"].rearrange("e (kc p) n -> e p kc n", p=128)
        wdv = dr["w_down"].rearrange("e (kc p) n -> e p kc n", p=128)
        NX = cx.nexp

        def gather(e):
            for jt in range(4):
                sc.dma("pool", "gat", lambda g, jt=jt: g.indirect_dma_start(
                    out=xg[:, jt, :], out_offset=None, in_=dr["h2_d"],
                    in_offset=bass.IndirectOffsetOnAxis(ap=cx.idx[:, e, jt:jt + 1], axis=0)),
                    reads=[cx.b_sel, cx.b_h2], writes=[b_xg])

        def load_s1(e, fb):
            s_ = fb % 2
            sc.dma("pool", "wst", lambda g: g.dma_start(out=wg[s_][:], in_=wgv[e, :, :, fb * 512:(fb + 1) * 512]), writes=[b_wg[s_]])
            sc.dma("pool", "wst", lambda g: g.dma_start(out=wu[s_][:], in_=wuv[e, :, :, fb * 512:(fb + 1) * 512]), writes=[b_wu[s_]])

        def load_s2(e, nb):
            s_ = nb % 2
            sc.dma("pool", "wst", lambda g: g.dma_start(out=wd[s_][:], in_=wdv[e, :, :, nb * 512:(nb + 1) * 512]), writes=[b_wd[s_]])

        gather(0)
        load_s1(0, 0)
        load_s1(0, 1)
        cnt = 0
        for e in range(NX):
            for dc in range(16):
                ts_ = dc % 2
                for jt in range(4):
                    sc.op("pe", lambda t, dc=dc, jt=jt, ts_=ts_: t.transpose(
                        tp[ts_][:, jt * 128:(jt + 1) * 128], xg[:, jt, dc * 128:(dc + 1) * 128], cx.ident_bf[:]),
                        reads=[b_xg, cx.b_const], writes=[b_tp[ts_]])
                if dc % 2 == 0:
                    sc.op("act", lambda a, dc=dc, ts_=ts_: a.copy(out=xgT[:, dc, :], in_=tp[ts_][:]), reads=[b_tp[ts_]], writes=[b_xgT])
                else:
                    sc.op("dve", lambda v, dc=dc, ts_=ts_: v.tensor_copy(out=xgT[:, dc, :], in_=tp[ts_][:]), reads=[b_tp[ts_]], writes=[b_xgT])
            if e + 1 < NX:
                gather(e + 1)
            for fb in range(4):
                s_ = fb % 2
                for fc in range(4):
                    f = fb * 4 + fc
                    q = cnt % 2
                    cnt += 1
                    for dc in range(16):
                        sc.op("pe", lambda t, dc=dc, fc=fc, s_=s_, q=q: t.matmul(
                            ap_[q][:], lhsT=wg[s_][:, dc, fc * 128:(fc + 1) * 128], rhs=xgT[:, dc, :],
                            start=(dc == 0), stop=(dc == 15)), reads=[b_wg[s_], b_xgT], writes=[b_ap[q]])
                    for dc in range(16):
                        sc.op("pe", lambda t, dc=dc, fc=fc, s_=s_, q=q: t.matmul(
                            up_[q][:], lhsT=wu[s_][:, dc, fc * 128:(fc + 1) * 128], rhs=xgT[:, dc, :],
                            start=(dc == 0), stop=(dc == 15)), reads=[b_wu[s_], b_xgT], writes=[b_up[q]])
                    sc.op("act", lambda a, q=q: a.activation(out=sa[q][:], in_=ap_[q][:], func=AF.Silu), reads=[b_ap[q]], writes=[b_sa[q]])
                    sc.op("dve", lambda v, q=q, f=f: v.tensor_tensor(out=hT[:, f, :], in0=sa[q][:], in1=up_[q][:], op=ALU.mult),
                          reads=[b_sa[q], b_up[q]], writes=[b_hT])
                if fb + 2 < 4:
                    load_s1(e, fb + 2)
                if fb == 0:
                    load_s2(e, 0)
                if fb == 1:
                    load_s2(e, 1)
            for nb in range(4):
                s_ = nb % 2
                for jt in range(4):
                    q = cnt % 2
                    cnt += 1
                    for fc in range(16):
                        sc.op("pe", lambda t, fc=fc, jt=jt, s_=s_, q=q: t.matmul(
                            yp[q][:], lhsT=hT[:, fc, jt * 128:(jt + 1) * 128], rhs=wd[s_][:, fc, :],
                            start=(fc == 0), stop=(fc == 15)), reads=[b_hT, b_wd[s_]], writes=[b_yp[q]])
                    sc.op("dve", lambda v, jt=jt, nb=nb, q=q: v.scalar_tensor_tensor(
                        out=ysb[:, jt, nb * 512:(nb + 1) * 512], in0=yp[q][:], scalar=cx.gsel[:, e, jt:jt + 1],
                        in1=gt2[:, nb * 512:(nb + 1) * 512], op0=ALU.mult, op1=ALU.mult),
                        reads=[b_yp[q], cx.b_sel, b_gt2], writes=[b_ysb[jt]])
                if nb + 2 < 4:
                    load_s2(e, nb + 2)
                elif e + 1 < NX:
                    load_s1(e + 1, nb - 2)
            for jt in range(4):
                sc.dma("pool", "sca", lambda g, jt=jt: g.indirect_dma_start(
                    out=dr["out"], out_offset=bass.IndirectOffsetOnAxis(ap=cx.idx[:, e, jt:jt + 1], axis=0),
                    in_=ysb[:, jt, :], in_offset=None, compute_op=ALU.add),
                    reads=[b_ysb[jt], cx.b_sel], writes=[cx.b_out])
        sc.barrier()


def build(upto="G", dbg_names=(), nchunk=8, nheadA=8, nheadB=8, dbg_scratch=False, nexp=16):
    nc = bass.Bass("TRN2", target_bir_lowering=False)
    cx = Ctx()
    cx.nc = nc
    cx.uid = 0
    cx.nchunk = nchunk
    cx.nheadA = nheadA
    cx.nheadB = nheadB
    cx.nexp = nexp
    dr = {}

    def ext(name, shape, dt=F32):
        dr[name] = nc.dram_tensor(name, list(shape), dt, kind="ExternalInput").ap()

    ext("x", [S, D])
    ext("cT", [128, 16])
    ext("w_ada", [D, 6 * D])
    ext("b_ada", [1, 6 * D])
    ext("g1T", [128, 16])
    ext("g2T", [128, 16])
    ext("w_in", [D, 3072])
    ext("gains", [128, 4])
    ext("rbs", [1, 264])
    ext("w_out", [D, D])
    ext("w_router", [D, NE])
    ext("w_gate", [NE, D, D])
    ext("w_up", [NE, D, D])
    ext("w_down", [NE, D, D])
    ext("ident", [128, 128])
    ext("rotT", [128, 128])
    ext("cosT", [128, S])
    ext("sinT", [128, S])
    ext("oh", [128, 32, 384])
    ext("negm", [128, 384])
    ext("triL", [128, 128])
    ext("iota512", [128, 512])
    ext("tvals", [128, NT, 2])
    dr["out"] = nc.dram_tensor("out", [S, D], F32, kind="ExternalOutput").ap()
    kw = {"kind": "ExternalOutput"} if dbg_scratch else {}
    dr["vec_d"] = nc.dram_tensor("vec_d", [6, D], F32, **kw).ap()
    dr["qT_d"] = nc.dram_tensor("qT_d", [16, 128, S], BF16, **kw).ap()
    dr["kT_d"] = nc.dram_tensor("kT_d", [4, 128, S], BF16, **kw).ap()
    dr["v_d"] = nc.dram_tensor("v_d", [S, 512], BF16, **kw).ap()
    dr["mixT_d"] = nc.dram_tensor("mixT_d", [16, 128, S], BF16, **kw).ap()
    dr["h2_d"] = nc.dram_tensor("h2_d", [S, D], BF16, **kw).ap()
    cx.dr = dr
    cx.dbg = {}
    dbg_shapes = {"dbg_mod": ([128, 96], F32), "dbg_idx": ([128, NE, 4], I32), "dbg_g": ([128, NE, 4], F32),
                  "dbg_aff": ([128, NE, NT], F32), "dbg_lo": ([128, NE], F32)}
    for n_ in dbg_names:
        shp, dt = dbg_shapes[n_]
        cx.dbg[n_] = nc.dram_tensor(n_, shp, dt, kind="ExternalOutput").ap()
    cx.b_vec = Buf("vec_d")
    cx.b_qT = Buf("qT_d")
    cx.b_kT = Buf("kT_d")
    cx.b_v = Buf("v_d")
    cx.b_mix = Buf("mixT_d")
    cx.b_h2 = Buf("h2_d")
    cx.b_out = Buf("out")
    with contextlib.ExitStack() as es:
        sc = Sched(nc, es)
        cx.sc = sc
        phase_consts(cx, es)
        order = "ABCDEFG"
        n = order.index(upto)
        phase_A(cx)
        if n >= 1:
            phase_B(cx)
        es_m = contextlib.ExitStack()
        if n >= 2:
            mask_prepare(cx, es_m)
            phase_C(cx)
        if n >= 3:
            phase_D(cx)
        es_m.close()
        if n >= 4:
            phase_E(cx, es)
        if n >= 5:
            phase_F(cx, es)
        if n >= 6:
            phase_G(cx)
        sc.barrier()
    return nc, cx


def _t5_bucket(rel):
    nb = 16
    ret = np.where(rel > 0, nb, 0)
    n = np.abs(rel)
    max_exact = 8
    nf = np.maximum(n, 1).astype(np.float32)
    large = max_exact + (np.log(nf / np.float32(max_exact)) / np.float32(math.log(128 / max_exact))
                         * np.float32(nb - max_exact)).astype(np.int32)
    large = np.minimum(large, nb - 1)
    return ret + np.where(n < max_exact, n, large)


_CONST = {}


def _consts():
    if _CONST:
        return _CONST
    c = {}
    c["ident"] = np.eye(128, dtype=np.float32)
    rot = np.zeros((128, 128), np.float32)
    for m in range(128):
        if (m % 64) < 32:
            rot[m + 32, m] = -1.0
        else:
            rot[m - 32, m] = 1.0
    c["rotT"] = rot
    t = np.arange(S)
    row = (t // 64).astype(np.float32)
    col = (t % 64).astype(np.float32)
    inv = (1.0 / (np.float32(10000.0) ** (np.arange(0, 64, 2, dtype=np.float32) / np.float32(64)))).astype(np.float32)
    ang_r = (row[:, None] * inv[None, :]).astype(np.float32)
    ang_c = (col[:, None] * inv[None, :]).astype(np.float32)
    cosT = np.concatenate([np.cos(ang_r), np.cos(ang_r), np.cos(ang_c), np.cos(ang_c)], axis=1).T
    sinT = np.concatenate([np.sin(ang_r), np.sin(ang_r), np.sin(ang_c), np.sin(ang_c)], axis=1).T
    c["cosT"] = np.ascontiguousarray(cosT.astype(np.float32))
    c["sinT"] = np.ascontiguousarray(sinT.astype(np.float32))
    k = np.arange(128)[:, None, None]
    off = np.arange(3)[None, :, None] - 1
    q = np.arange(128)[None, None, :]
    rel = k + off * 128 - q
    valid = np.abs(rel) <= 128
    bk = _t5_bucket(rel)
    oh = np.zeros((128, 32, 3, 128), np.float32)
    for b in range(32):
        oh[:, b] = ((bk == b) & valid).astype(np.float32)
    c["oh"] = np.ascontiguousarray(oh.reshape(128, 32, 384))
    c["negm"] = np.ascontiguousarray(np.where(valid, 0.0, -30000.0).astype(np.float32).reshape(128, 384))
    tri = (np.arange(128)[:, None] < np.arange(128)[None, :]).astype(np.float32)
    c["triL"] = tri
    c["iota512"] = np.ascontiguousarray(np.broadcast_to(np.arange(512, dtype=np.float32), (128, 512)))
    tg = np.arange(NT)[None, :] * 128 + np.arange(128)[:, None]
    c["tvals"] = np.ascontiguousarray(np.stack([tg // 64, tg % 64], axis=-1).astype(np.float32))
    _CONST.update(c)
    return _CONST


def make_in_map(b, x, c, w_ada, b_ada, g_norm1, w_in, qn_a, kn_a, qn_b, kn_b, sink_b, rel_bias, w_out, g_norm2,
                w_router, w_gate, w_up, w_down):
    f = lambda a: np.ascontiguousarray(np.asarray(a, dtype=np.float32))
    m = dict(_consts())
    m["x"] = f(x[b])
    m["cT"] = f(np.asarray(c[b]).reshape(16, 128).T)
    m["w_ada"] = f(w_ada)
    m["b_ada"] = f(np.asarray(b_ada).reshape(1, -1))
    m["g1T"] = f(np.asarray(g_norm1).reshape(16, 128).T)
    m["g2T"] = f(np.asarray(g_norm2).reshape(16, 128).T)
    m["w_in"] = f(w_in)
    m["gains"] = f(np.stack([np.asarray(qn_a), np.asarray(kn_a), np.asarray(qn_b), np.asarray(kn_b)], axis=1))
    m["rbs"] = f(np.concatenate([np.asarray(rel_bias).reshape(-1), np.asarray(sink_b).reshape(-1)]).reshape(1, 264))
    m["w_out"] = f(w_out)
    m["w_router"] = f(w_router)
    m["w_gate"] = f(w_gate)
    m["w_up"] = f(w_up)
    m["w_down"] = f(w_down)
    return m


_NC = {}


def kernel(**inputs):
    if "nc" not in _NC:
        _NC["nc"] = build("G")[0]
    nc = _NC["nc"]
    in_maps = [make_in_map(i % 4, **inputs) for i in range(N_CORES)]
    res = run_bass_kernel_spmd(nc, in_maps, core_ids=list(range(N_CORES)))
    out = np.stack([np.asarray(res.results[b]["out"]) for b in range(4)], axis=0)
    return out.astype(np.float32)
```

```python
import contextlib
import math

import numpy as np

import concourse.bass as bass
import concourse.mybir as mybir
from concourse.bass_utils import run_bass_kernel_spmd

F32 = mybir.dt.float32
BF16 = mybir.dt.bfloat16
I32 = mybir.dt.int32
AF = mybir.ActivationFunctionType
ALU = mybir.AluOpType
AX = mybir.AxisListType

S = 4096
D = 2048
NT = S // 128
NE = 16
CAP = 512
EPS = 1e-6
SCALE = 1.0 / math.sqrt(128.0)
N_CORES = 8


class Buf:
    __slots__ = ("name", "w", "r", "dsem")

    def __init__(self, name):
        self.name = name
        self.w = None
        self.r = {}
        self.dsem = None


class Sched:
    LIMIT = 30000

    def __init__(self, nc, es):
        self.nc = nc
        self.es = es
        self.eng = {"pe": nc.tensor, "act": nc.scalar, "dve": nc.vector,
                    "pool": nc.gpsimd, "sp": nc.sync}
        self.sem = {}
        self.cnt = {}
        self.nsem = 0
        self.waited = {e: {} for e in self.eng}
        self.dbufs = []
        self.retired = []
        for e in self.eng:
            self._newsem(e)

    def _mk(self, tag):
        self.nsem += 1
        return self.es.enter_context(self.nc.semaphore(f"s{self.nsem}_{tag}"))

    def _newsem(self, e):
        if e in self.sem and self.cnt[e] > 0:
            self.retired.append((self.sem[e][0], self.sem[e][1], self.cnt[e], e))
        self.sem[e] = (self._mk(e), self.nsem)
        self.cnt[e] = 0

    def _wait(self, e, tok):
        if tok is None:
            return
        s, key, v, src = tok
        if src == "pe" and e == "pe":
            return
        if self.waited[e].get(key, 0) >= v:
            return
        self.eng[e].wait_ge(s, v)
        self.waited[e][key] = v

    def _deps(self, e, reads, writes):
        for b in reads:
            self._wait(e, b.w)
        for b in writes:
            self._wait(e, b.w)
            for t in b.r.values():
                self._wait(e, t)

    def _mark(self, tok, reads, writes):
        for b in reads:
            b.r[tok[1]] = tok
        for b in writes:
            b.w = tok
            b.r = {}

    def op(self, e, fn, reads=(), writes=()):
        self._deps(e, reads, writes)
        if self.cnt[e] >= self.LIMIT:
            self._newsem(e)
        ins = fn(self.eng[e])
        self.cnt[e] += 1
        s, key = self.sem[e]
        ins.then_inc(s, 1)
        tok = (s, key, self.cnt[e], e)
        self._mark(tok, reads, writes)
        return tok

    def dma(self, e, chan, fn, reads=(), writes=()):
        self._deps(e, reads, writes)
        owner = writes[0] if len(writes) else reads[0]
        if owner.dsem is None:
            owner.dsem = [self._mk("d_" + owner.name), self.nsem, 0]
            self.dbufs.append(owner)
        c = owner.dsem
        ins = fn(self.eng[e])
        c[2] += 1
        ins.then_inc(c[0], 16)
        tok = (c[0], c[1], 16 * c[2], "dma")
        self._mark(tok, reads, writes)
        return tok

    def all_tokens(self):
        toks = list(self.retired)
        for e in self.eng:
            if self.cnt[e] > 0:
                s, key = self.sem[e]
                toks.append((s, key, self.cnt[e], e))
        for bf in self.dbufs:
            c = bf.dsem
            toks.append((c[0], c[1], 16 * c[2], "dma"))
        return toks

    def barrier(self):
        toks = self.all_tokens()
        for e in self.eng:
            for t in toks:
                if t[3] == e:
                    continue
                self._wait(e, t)


class Ctx:
    pass


def _pool(cx, es):
    nc = cx.nc
    n = [0]

    def sb(shape, dt, name=None):
        n[0] += 1
        cx.uid += 1
        return es.enter_context(nc.sbuf_tensor(f"{name or 't'}_{cx.uid}", list(shape), dt))

    def ps(shape, dt, name=None):
        cx.uid += 1
        return es.enter_context(nc.psum_tensor(f"{name or 'p'}_{cx.uid}", list(shape), dt))

    return sb, ps


def phase_consts(cx, es):
    nc, sc, dr = cx.nc, cx.sc, cx.dr
    sb, ps = _pool(cx, es)
    cx.ident_bf = sb([128, 128], BF16, "identbf")
    cx.ident_f = sb([128, 128], F32, "identf")
    cx.ones_bf = sb([128, 128], BF16, "onesbf")
    cx.b_const = Buf("consts")
    sc.dma("pool", "cst", lambda g: g.dma_start(out=cx.ident_bf[:], in_=dr["ident"]), writes=[cx.b_const])
    sc.dma("sp", "cst", lambda g: g.dma_start(out=cx.ident_f[:], in_=dr["ident"]), writes=[cx.b_const])
    sc.op("dve", lambda v: v.memset(cx.ones_bf[:], 1.0), writes=[cx.b_const])
    cx.A1col = sb([128, 16], F32, "A1col")
    cx.B1col = sb([128, 16], F32, "B1col")
    cx.b_ab1 = Buf("ab1")
    cx.epsc = sb([128, 1], F32, "epsc")
    sc.op("dve", lambda v: v.memset(cx.epsc[:], EPS), writes=[cx.b_const])


def phase_A(cx):
    nc, sc, dr = cx.nc, cx.sc, cx.dr
    with contextlib.ExitStack() as es:
        sb, ps = _pool(cx, es)
        cT = sb([128, 16], F32)
        sT = sb([128, 16], F32)
        g1T = sb([128, 16], F32)
        g2T = sb([128, 16], F32)
        wblk = [sb([128, 16, 512], BF16, "wada") for _ in range(3)]
        modT = sb([128, 96], F32)
        m2 = sb([128, 96, 2], F32)
        s2 = sb([128, 16, 2], BF16)
        sres = sb([128, 16], F32)
        bT96 = sb([96, 128], F32)
        v4 = sb([128, 96], F32)
        v4T = sb([96, 128], F32)
        modp = ps([128, 96, 2], F32)
        bTp = ps([128, 96], F32)
        v4p = ps([96, 128], F32)
        b_small = Buf("small")
        b_w = [Buf("wada0"), Buf("wada1"), Buf("wada2")]
        b_modp = Buf("modp")
        b_modT = Buf("modT")
        b_bTp = Buf("bTp")
        b_v4 = Buf("v4")
        b_v4p = Buf("v4p")
        b_v4T = Buf("v4T")
        sc.dma("sp", "ld", lambda g: g.dma_start(out=cT[:], in_=dr["cT"]), writes=[b_small])
        sc.dma("sp", "ld", lambda g: g.dma_start(out=g1T[:], in_=dr["g1T"]), writes=[b_small])
        sc.dma("sp", "ld", lambda g: g.dma_start(out=g2T[:], in_=dr["g2T"]), writes=[b_small])
        sc.dma("sp", "ld", lambda g: g.dma_start(out=bT96[:], in_=dr["b_ada"].rearrange("o (n p) -> (o n) p", p=128)), writes=[b_small])
        sc.op("act", lambda a: a.activation(out=sT[:], in_=cT[:], func=AF.Silu), reads=[b_small], writes=[b_small])
        sc.op("dve", lambda v: v.tensor_copy(out=s2[:, :, 0], in_=sT[:]), reads=[b_small], writes=[b_small])
        sc.op("dve", lambda v: v.tensor_tensor(out=sres[:], in0=sT[:], in1=s2[:, :, 0], op=ALU.subtract), reads=[b_small], writes=[b_small])
        sc.op("dve", lambda v: v.tensor_copy(out=s2[:, :, 1], in_=sres[:]), reads=[b_small], writes=[b_small])
        sc.op("pe", lambda t: t.transpose(bTp[:], bT96[:], cx.ident_f[0:96, 0:96]), reads=[b_small, cx.b_const], writes=[b_bTp])
        wv = dr["w_ada"].rearrange("(kc p) n -> p kc n", p=128)
        for nb in range(24):
            slot = nb % 3
            w = wblk[slot]
            sc.dma("pool", "wst", lambda g, w=w, nb=nb: g.dma_start(out=w[:], in_=wv[:, :, nb * 512:(nb + 1) * 512]),
                   writes=[b_w[slot]])
            for j in range(4):
                n = nb * 4 + j
                for kc in range(16):
                    sc.op("pe", lambda t, w=w, j=j, kc=kc, n=n: t.matmul(
                        modp[:, n, :], lhsT=w[:, kc, j * 128:(j + 1) * 128], rhs=s2[:, kc, :],
                        start=(kc == 0), stop=(kc == 15)), reads=[b_w[slot], b_small], writes=[b_modp])
        sc.op("dve", lambda v: v.tensor_copy(out=m2[:], in_=modp[:]), reads=[b_modp], writes=[b_modT])
        sc.op("dve", lambda v: v.tensor_tensor(out=modT[:], in0=m2[:, :, 0], in1=m2[:, :, 1], op=ALU.add), reads=[b_modT], writes=[b_modT])
        sc.op("dve", lambda v: v.tensor_tensor(out=modT[:], in0=modT[:], in1=bTp[:], op=ALU.add), reads=[b_modT, b_bTp], writes=[b_modT])
        sc.op("dve", lambda v: v.scalar_tensor_tensor(out=cx.A1col[:], in0=modT[:, 16:32], scalar=1.0, in1=g1T[:],
                                                       op0=ALU.add, op1=ALU.mult),
              reads=[b_modT, b_small], writes=[cx.b_ab1])
        sc.op("dve", lambda v: v.tensor_copy(out=cx.B1col[:], in_=modT[:, 0:16]), reads=[b_modT], writes=[cx.b_ab1])
        sc.op("dve", lambda v: v.scalar_tensor_tensor(out=v4[:, 0:16], in0=modT[:, 64:80], scalar=1.0, in1=g2T[:],
                                                       op0=ALU.add, op1=ALU.mult),
              reads=[b_modT, b_small], writes=[b_v4])
        sc.op("dve", lambda v: v.tensor_copy(out=v4[:, 16:32], in_=modT[:, 48:64]), reads=[b_modT], writes=[b_v4])
        sc.op("dve", lambda v: v.tensor_copy(out=v4[:, 32:48], in_=modT[:, 32:48]), reads=[b_modT], writes=[b_v4])
        sc.op("dve", lambda v: v.tensor_copy(out=v4[:, 48:64], in_=modT[:, 80:96]), reads=[b_modT], writes=[b_v4])
        sc.op("dve", lambda v: v.tensor_copy(out=v4[:, 64:80], in_=cx.A1col[:]), reads=[cx.b_ab1], writes=[b_v4])
        sc.op("dve", lambda v: v.tensor_copy(out=v4[:, 80:96], in_=cx.B1col[:]), reads=[cx.b_ab1], writes=[b_v4])
        sc.op("pe", lambda t: t.transpose(v4p[:], v4[:], cx.ident_f[:]), reads=[b_v4, cx.b_const], writes=[b_v4p])
        sc.op("dve", lambda v: v.tensor_copy(out=v4T[:], in_=v4p[:]), reads=[b_v4p], writes=[b_v4T])
        sc.dma("sp", "st", lambda g: g.dma_start(out=dr["vec_d"].rearrange("j (c p) -> (j c) p", p=128), in_=v4T[:]),
               reads=[b_v4T], writes=[cx.b_vec])
        if cx.dbg is not None and "dbg_mod" in cx.dbg:
            sc.dma("sp", "st", lambda g: g.dma_start(out=cx.dbg["dbg_mod"], in_=modT[:]), reads=[b_modT])
        sc.barrier()


def _head_cols():
    cols = []
    for h in range(8):
        cols.append(("qa", h, h * 128))
    for h in range(2):
        cols.append(("ka", h, 1024 + h * 128))
    for h in range(8):
        cols.append(("qb", h, 1536 + h * 128))
    for h in range(2):
        cols.append(("kb", h, 1536 + 1024 + h * 128))
    return cols


def phase_B(cx):
    nc, sc, dr = cx.nc, cx.sc, cx.dr
    with contextlib.ExitStack() as es:
        sb, ps = _pool(cx, es)
        w_in = sb([128, 16, 3072], BF16, "w_in")
        b_wins = [Buf(f"w_in{i}") for i in range(6)]
        wv = dr["w_in"].rearrange("(kc p) n -> p kc n", p=128)
        for i in (0, 1, 2, 3, 4, 5):
            sc.dma("pool", "wst", lambda g, i=i: g.dma_start(out=w_in[:, :, i * 512:(i + 1) * 512],
                                                            in_=wv[:, :, i * 512:(i + 1) * 512]), writes=[b_wins[i]])
        ab1A = sb([128, 2048], F32, "ab1A")
        ab1B = sb([128, 2048], BF16, "ab1B")
        b_ab1bc = Buf("ab1bc")
        sc.dma("sp", "cst", lambda g: g.dma_start(out=ab1A[:], in_=dr["vec_d"][4:5, :].partition_broadcast(128)),
               reads=[cx.b_vec], writes=[b_ab1bc])
        sc.dma("pool", "cst", lambda g: g.dma_start(out=ab1B[:], in_=dr["vec_d"][5:6, :].partition_broadcast(128)),
               reads=[cx.b_vec], writes=[b_ab1bc])
        rotT = sb([128, 128], BF16, "rotT")
        gains = sb([128, 4], F32, "gains")
        b_c = Buf("bconst")
        sc.dma("pool", "cst", lambda g: g.dma_start(out=rotT[:], in_=dr["rotT"]), writes=[b_c])
        sc.dma("sp", "cst", lambda g: g.dma_start(out=gains[:], in_=dr["gains"]), writes=[b_c])
        xt = [sb([128, 2048], F32, "xt") for _ in range(2)]
        b_xt = [Buf("xt0"), Buf("xt1")]
        xs = [sb([128, 2048], BF16, "xs") for _ in range(2)]
        b_xs = [Buf("xs0"), Buf("xs1")]
        ss = [sb([128, 1], F32, "ss") for _ in range(2)]
        b_ss = [Buf("ss0"), Buf("ss1")]
        hT = [sb([128, 16, 512], BF16, "hT") for _ in range(2)]
        b_hT = [Buf("hT0"), Buf("hT1")]
        cs = [sb([128, 2, 512], F32, "cs") for _ in range(2)]
        b_cs = [Buf("cs0"), Buf("cs1")]
        NS = 3
        sq = [sb([128, 512], BF16, "sq") for _ in range(NS)]
        zg = [sb([128, 512], BF16, "zg") for _ in range(NS)]
        rs = [sb([128, 512], F32, "rs") for _ in range(NS)]
        t1 = [sb([128, 512], F32, "t1") for _ in range(NS)]
        t2 = [sb([128, 512], F32, "t2") for _ in range(NS)]
        qf = [sb([128, 512], BF16, "qf") for _ in range(NS)]
        vsb = [sb([128, 512], BF16, "vsb") for _ in range(2)]
        b_sq = [Buf(f"sq{i}") for i in range(NS)]
        b_zg = [Buf(f"zg{i}") for i in range(NS)]
        b_rs = [Buf(f"rs{i}") for i in range(NS)]
        b_t1 = [Buf(f"t1{i}") for i in range(NS)]
        b_t2 = [Buf(f"t2{i}") for i in range(NS)]
        b_qf = [Buf(f"qf{i}") for i in range(NS)]
        b_vsb = [Buf("v0"), Buf("v1")]
        ptr = [ps([128, 8, 128], BF16, "ptr") for _ in range(2)]
        b_ptr = [Buf("ptr0"), Buf("ptr1")]
        zq = [ps([128, 512], F32, "zq") for _ in range(2)]
        b_zq = [Buf("zq0"), Buf("zq1")]
        ssp = [ps([128, 512], F32, "ssp") for _ in range(2)]
        b_ssp = [Buf("ssp0"), Buf("ssp1")]
        rotp = [ps([128, 512], F32, "rotp") for _ in range(2)]
        b_rotp = [Buf("rotp0"), Buf("rotp1")]
        heads = _head_cols()
        vcols = [1280, 1408, 2816, 2944]

        def load_cs(ci):
            hs = ci % 2
            t0 = ci * 512
            sc.dma("sp", "ld", lambda g: g.dma_start(out=cs[hs][:, 0, :], in_=dr["cosT"][:, t0:t0 + 512]), writes=[b_cs[hs]])
            sc.dma("sp", "ld", lambda g: g.dma_start(out=cs[hs][:, 1, :], in_=dr["sinT"][:, t0:t0 + 512]), writes=[b_cs[hs]])

        def front_a(ti):
            ci, tt = ti // 4, ti % 4
            hs = ci % 2
            xsl = ti % 2
            if tt == 0:
                load_cs(ci)
            sc.dma("sp", "ld", lambda g: g.dma_start(out=xt[xsl][:], in_=dr["x"][ti * 128:(ti + 1) * 128, :]), writes=[b_xt[xsl]])
            sc.op("act", lambda a: a.activation(out=xs[xsl][:], in_=xt[xsl][:], func=AF.Square, accum_out=ss[xsl][:]),
                  reads=[b_xt[xsl]], writes=[b_xs[xsl], b_ss[xsl]])
            sc.op("act", lambda a: a.activation(out=ss[xsl][:], in_=ss[xsl][:], func=AF.Ln, scale=1.0 / D, bias=cx.epsc[:]),
                  reads=[b_ss[xsl], cx.b_const], writes=[b_ss[xsl]])
            sc.op("act", lambda a: a.activation(out=ss[xsl][:], in_=ss[xsl][:], func=AF.Exp, scale=-0.5),
                  reads=[b_ss[xsl]], writes=[b_ss[xsl]])
            sc.op("dve", lambda v: v.scalar_tensor_tensor(out=xt[xsl][:], in0=xt[xsl][:], scalar=ss[xsl][:, 0:1], in1=ab1A[:],
                                                          op0=ALU.mult, op1=ALU.mult),
                  reads=[b_xt[xsl], b_ss[xsl], b_ab1bc], writes=[b_xt[xsl]])
            sc.op("dve", lambda v: v.tensor_tensor(out=xs[xsl][:], in0=xt[xsl][:], in1=ab1B[:], op=ALU.add),
                  reads=[b_xt[xsl], b_ab1bc], writes=[b_xs[xsl]])

        def front_b(ti):
            ci, tt = ti // 4, ti % 4
            hs = ci % 2
            xsl = ti % 2
            for half in range(2):
                pb = half
                for c8 in range(8):
                    c = half * 8 + c8
                    sc.op("pe", lambda t, c8=c8, c=c: t.transpose(ptr[pb][:, c8, :], xs[xsl][:, c * 128:(c + 1) * 128], cx.ident_bf[:]),
                          reads=[b_xs[xsl], cx.b_const], writes=[b_ptr[pb]])
                sc.op("act", lambda a: a.copy(out=hT[hs][:, half * 8:(half + 1) * 8, tt * 128:(tt + 1) * 128], in_=ptr[pb][:]),
                      reads=[b_ptr[pb]], writes=[b_hT[hs]])

        def head1(ci, k):
            kind, h, col = heads[k]
            hs = ci % 2
            zl = k % 2
            sl = k % NS
            for kc in range(16):
                sc.op("pe", lambda t, kc=kc: t.matmul(zq[zl][:], lhsT=w_in[:, kc, col:col + 128], rhs=hT[hs][:, kc, :],
                                                      start=(kc == 0), stop=(kc == 15)),
                      reads=[b_wins[col // 512], b_hT[hs]], writes=[b_zq[zl]])
            gi = {"qa": 0, "ka": 1, "qb": 2, "kb": 3}[kind]
            sc.op("act", lambda a: a.activation(out=sq[sl][:], in_=zq[zl][:], func=AF.Square), reads=[b_zq[zl]], writes=[b_sq[sl]])
            sc.op("act", lambda a: a.activation(out=zg[sl][:], in_=zq[zl][:], func=AF.Identity, scale=gains[:, gi:gi + 1]),
                  reads=[b_zq[zl], b_c], writes=[b_zg[sl]])

        def head2(ci, k):
            kind, h, col = heads[k]
            hs = ci % 2
            zl = k % 2
            sl = k % NS
            t0 = ci * 512
            sc.op("pe", lambda t: t.matmul(ssp[zl][:], lhsT=cx.ones_bf[:], rhs=sq[sl][:], start=True, stop=True),
                  reads=[b_sq[sl], cx.b_const], writes=[b_ssp[zl]])
            sc.op("act", lambda a: a.activation(out=rs[sl][:], in_=ssp[zl][:], func=AF.Ln, scale=1.0 / 128.0, bias=cx.epsc[:]),
                  reads=[b_ssp[zl], cx.b_const], writes=[b_rs[sl]])
            sc.op("act", lambda a: a.activation(out=rs[sl][:], in_=rs[sl][:], func=AF.Exp, scale=-0.5), reads=[b_rs[sl]], writes=[b_rs[sl]])
            if kind in ("qa", "ka"):
                sc.op("pe", lambda t: t.matmul(rotp[zl][:], lhsT=rotT[:], rhs=zg[sl][:], start=True, stop=True),
                      reads=[b_zg[sl], b_c], writes=[b_rotp[zl]])
                sc.op("pool", lambda g: g.tensor_tensor(out=t1[sl][:], in0=zg[sl][:], in1=cs[hs][:, 0, :], op=ALU.mult),
                      reads=[b_zg[sl], b_cs[hs]], writes=[b_t1[sl]])
                sc.op("dve", lambda v: v.tensor_tensor(out=t2[sl][:], in0=rotp[zl][:], in1=cs[hs][:, 1, :], op=ALU.mult),
                      reads=[b_rotp[zl], b_cs[hs]], writes=[b_t2[sl]])
                sc.op("dve", lambda g: g.tensor_tensor(out=t1[sl][:], in0=t1[sl][:], in1=t2[sl][:], op=ALU.add),
                      reads=[b_t1[sl], b_t2[sl]], writes=[b_t1[sl]])
                sc.op("dve", lambda v: v.tensor_tensor(out=qf[sl][:], in0=t1[sl][:], in1=rs[sl][:], op=ALU.mult),
                      reads=[b_t1[sl], b_rs[sl]], writes=[b_qf[sl]])
            else:
                sc.op("dve", lambda v: v.tensor_tensor(out=qf[sl][:], in0=zg[sl][:], in1=rs[sl][:], op=ALU.mult),
                      reads=[b_zg[sl], b_rs[sl]], writes=[b_qf[sl]])
            if kind[0] == "q":
                hd = h if kind == "qa" else 8 + h
                dst = dr["qT_d"][hd, :, t0:t0 + 512]
                wb = cx.b_qT
            else:
                hd = h if kind == "ka" else 2 + h
                dst = dr["kT_d"][hd, :, t0:t0 + 512]
                wb = cx.b_kT
            sc.dma("sp", "st", lambda g: g.dma_start(out=dst, in_=qf[sl][:]), reads=[b_qf[sl]], writes=[])

        def vunit(ci, tt):
            hs = ci % 2
            ti = ci * 4 + tt
            sl = tt % 2
            for vi, vc in enumerate(vcols):
                for kc in range(16):
                    sc.op("pe", lambda t, kc=kc, vc=vc, vi=vi: t.matmul(
                        zq[sl][:, vi * 128:(vi + 1) * 128], lhsT=hT[hs][:, kc, tt * 128:(tt + 1) * 128],
                        rhs=w_in[:, kc, vc:vc + 128], start=(kc == 0), stop=(kc == 15)),
                        reads=[b_wins[vc // 512], b_hT[hs]], writes=[b_zq[sl]])
            sc.op("act", lambda a: a.copy(out=vsb[sl][:], in_=zq[sl][:]), reads=[b_zq[sl]], writes=[b_vsb[sl]])
            sc.dma("sp", "st", lambda g: g.dma_start(out=dr["v_d"][ti * 128:(ti + 1) * 128, :], in_=vsb[sl][:]),
                   reads=[b_vsb[sl]], writes=[])

        front_a(0)
        front_a(1)
        front_b(0)
        front_a(2)
        front_b(1)
        front_a(3)
        front_b(2)
        front_b(3)
        NH = len(heads)
        ntile = cx.nchunk * 4
        for ci in range(cx.nchunk):
            nxt = ci + 1 < cx.nchunk
            if nxt:
                front_a((ci + 1) * 4)
            head1(ci, 0)
            for k in range(1, NH):
                head1(ci, k)
                head2(ci, k - 1)
                if nxt and k in (4, 9, 14, 19):
                    j = (4, 9, 14, 19).index(k)
                    front_b((ci + 1) * 4 + j)
                    if j + 1 < 4:
                        front_a((ci + 1) * 4 + j + 1)
            vunit(ci, 0)
            head2(ci, NH - 1)
            for tt in range(1, 4):
                vunit(ci, tt)
        sc.barrier()


def mask_prepare(cx, es):
    nc, sc, dr = cx.nc, cx.sc, cx.dr
    sb, ps = _pool(cx, es)
    oh = sb([128, 32, 384], BF16, "oh")
    rb = sb([128, 264], F32, "rb")
    erb = sb([128, 264], F32, "erb")
    rbq = sb([128, 256], F32, "rbq")
    negm = sb([128, 384], F32, "negm")
    negfull = sb([128, 128], BF16, "negfull")
    bm = sb([128, 8, 384], F32, "bm")
    bm4 = sb([128, 8, 3, 512], BF16, "bm4")
    b_oh = Buf("oh")
    b_rb = Buf("rb")
    b_bm = [Buf(f"bm{h}") for h in range(8)]
    b_bm4 = [Buf(f"bm4{h}") for h in range(8)]
    sc.dma("pool", "cst", lambda g: g.dma_start(out=oh[:], in_=dr["oh"]), writes=[b_oh])
    sc.dma("sp", "cst", lambda g: g.dma_start(out=negm[:], in_=dr["negm"]), writes=[b_oh])
    sc.dma("sp", "cst", lambda g: g.dma_start(out=rb[:], in_=dr["rbs"].partition_broadcast(128)), writes=[b_rb])
    sc.op("act", lambda a: a.activation(out=erb[:], in_=rb[:], func=AF.Exp), reads=[b_rb], writes=[b_rb])
    sc.op("dve", lambda v: v.tensor_scalar(out=rbq[:], in0=rb[:, 0:256], scalar1=1.0 / SCALE, scalar2=None, op0=ALU.mult),
          reads=[b_rb], writes=[b_rb])
    sc.op("dve", lambda v: v.memset(negfull[:], -30000.0), writes=[b_oh])

    def gen():
        for b in range(32):
            for h in range(cx.nheadB):
                src = negm[:] if b == 0 else bm[:, h, :]
                sc.op("dve", lambda v, h=h, b=b, src=src: v.scalar_tensor_tensor(
                    out=bm[:, h, :], in0=oh[:, b, :], scalar=rbq[:, b * 8 + h:b * 8 + h + 1], in1=src, op0=ALU.mult, op1=ALU.add),
                    reads=[b_oh, b_rb, b_bm[h]], writes=[b_bm[h]])
                yield
        for h in range(cx.nheadB):
            for o in range(3):
                for tt in range(4):
                    sc.op("pool", lambda g, h=h, o=o, tt=tt: g.tensor_copy(out=bm4[:, h, o, tt * 128:(tt + 1) * 128],
                                                                           in_=bm[:, h, o * 128:(o + 1) * 128]),
                          reads=[b_bm[h]], writes=[b_bm4[h]])
                    yield

    g = gen()

    def step():
        try:
            next(g)
            return True
        except StopIteration:
            return False

    def drain():
        while step():
            pass

    cx.mask_step = step
    cx.mask_drain = drain
    cx.m_oh, cx.m_negm, cx.m_erb, cx.m_negfull, cx.m_bm4 = oh, negm, erb, negfull, bm4
    cx.mb_oh, cx.mb_rb, cx.mb_bm4 = b_oh, b_rb, b_bm4


def phase_C(cx):
    nc, sc, dr = cx.nc, cx.sc, cx.dr
    with contextlib.ExitStack() as es:
        sb, ps = _pool(cx, es)
        kT = sb([128, 2, S], BF16, "kTa")
        va = sb([128, NT, 256], BF16, "va")
        ones_f = sb([128, 128], F32, "ones_fC")
        b_kv = Buf("kva")
        for g_ in range(2):
            sc.dma("sp", "ld", lambda g, g_=g_: g.dma_start(out=kT[:, g_, :], in_=dr["kT_d"][g_, :, :]),
                   reads=[cx.b_kT], writes=[b_kv])
        sc.dma("sp", "ld", lambda g: g.dma_start(out=va[:], in_=dr["v_d"].rearrange("(t p) n -> p t n", p=128)[:, :, 0:256]),
               reads=[cx.b_v], writes=[b_kv])
        sc.op("dve", lambda v: v.memset(ones_f[:], 1.0), writes=[b_kv])
        qc = [sb([128, 512], BF16, "qc") for _ in range(2)]
        b_qc = [Buf("qc0"), Buf("qc1")]
        NPT = 3
        pt = [sb([128, 2, 512], BF16, "pt") for _ in range(NPT)]
        b_pt = [Buf(f"pt{i}") for i in range(NPT)]
        accD = [sb([128, 2, 512], F32, "accD") for _ in range(2)]
        accP = [sb([128, 2, 512], F32, "accP") for _ in range(2)]
        b_accD = [Buf("accD0"), Buf("accD1")]
        b_accP = [Buf("accP0"), Buf("accP1")]
        dsum = sb([128, 512], F32, "dsum")
        dsum2 = sb([128, 512], F32, "dsum2")
        b_dsum, b_dsum2 = Buf("dsum"), Buf("dsum2")
        rden = sb([128, 512], F32, "rden")
        b_rden = Buf("rden")
        osb = [sb([128, 512], BF16, "osb") for _ in range(2)]
        b_osb = [Buf("osb0"), Buf("osb1")]
        st = [ps([128, 2, 512], F32, "st") for _ in range(2)]
        b_st = [Buf("st0"), Buf("st1")]
        ot = [ps([128, 512], F32, "ot") for _ in range(2)]
        b_ot = [Buf("ot0"), Buf("ot1")]
        dn = ps([128, 512], F32, "dn")
        b_dn = Buf("dn")
        NP = NT // 2
        iters = [(hd, ci) for hd in range(cx.nheadA) for ci in range(cx.nchunk)]
        units = [(i, kp) for i in range(len(iters)) for kp in range(NP)]

        def load_q(i):
            hd, ci = iters[i]
            qs = i % 2
            sc.dma("sp", "ld", lambda g: g.dma_start(out=qc[qs][:], in_=dr["qT_d"][hd, :, ci * 512:(ci + 1) * 512]),
                   reads=[cx.b_qT], writes=[b_qc[qs]])

        def emit_st(u):
            i, kp = units[u]
            hd, ci = iters[i]
            g_ = hd // 4
            qs = i % 2
            s_ = u % 2
            if kp == 0:
                load_q(i)
            for j in range(2):
                kt = kp * 2 + j
                sc.op("pe", lambda t, j=j, kt=kt: t.matmul(st[s_][:, j, :], lhsT=kT[:, g_, kt * 128:(kt + 1) * 128], rhs=qc[qs][:],
                                                           start=True, stop=True),
                      reads=[b_kv, b_qc[qs]], writes=[b_st[s_]])

        emit_st(0)
        for u in range(len(units)):
            i, kp = units[u]
            hd, ci = iters[i]
            g_ = hd // 4
            qs = i % 2
            s_ = u % 2
            p_ = u % NPT
            if u + 1 < len(units):
                emit_st(u + 1)
            cx.mask_step()
            sc.op("act", lambda a: a.activation(out=pt[p_][:], in_=st[s_][:], func=AF.Exp, scale=SCALE),
                  reads=[b_st[s_]], writes=[b_pt[p_]])
            for j in range(2):
                kt = kp * 2 + j
                sc.op("pe", lambda t, j=j, kt=kt: t.matmul(ot[qs][:], lhsT=va[:, kt, g_ * 128:(g_ + 1) * 128], rhs=pt[p_][:, j, :],
                                                           start=(kt == 0), stop=(kt == NT - 1)),
                      reads=[b_kv, b_pt[p_]], writes=[b_ot[qs]])
            if kp % 3 == 2:
                for j in range(2):
                    sc.op("pe", lambda t, j=j: t.matmul(dn[:], lhsT=cx.ones_bf[:], rhs=pt[p_][:, j, :],
                                                        start=(kp == 2 and j == 0), stop=False),
                          reads=[cx.b_const, b_pt[p_]], writes=[b_dn])
            else:
                acc, bacc = accD[qs], b_accD[qs]
                if kp == 0:
                    sc.op("dve", lambda v: v.tensor_copy(out=acc[:], in_=pt[p_][:]), reads=[b_pt[p_]], writes=[bacc])
                else:
                    sc.op("dve", lambda v: v.tensor_tensor(out=acc[:], in0=acc[:], in1=pt[p_][:], op=ALU.add),
                          reads=[b_pt[p_], bacc], writes=[bacc])
            if kp == NP - 1:
                sc.op("dve", lambda v: v.tensor_tensor(out=dsum[:], in0=accD[qs][:, 0, :], in1=accD[qs][:, 1, :], op=ALU.add),
                      reads=[b_accD[qs]], writes=[b_dsum])
                sc.op("pe", lambda t: t.matmul(dn[:], lhsT=ones_f[:], rhs=dsum[:], start=False, stop=True),
                      reads=[b_kv, b_dsum], writes=[b_dn])
                sc.op("act", lambda a: a.activation(out=rden[:], in_=dn[:], func=AF.Ln), reads=[b_dn], writes=[b_rden])
                sc.op("act", lambda a: a.activation(out=rden[:], in_=rden[:], func=AF.Exp, scale=-1.0), reads=[b_rden], writes=[b_rden])
                sc.op("dve", lambda v: v.tensor_tensor(out=osb[qs][:], in0=ot[qs][:], in1=rden[:], op=ALU.mult),
                      reads=[b_ot[qs], b_rden], writes=[b_osb[qs]])
                sc.dma("sp", "st", lambda g: g.dma_start(out=dr["mixT_d"][hd, :, ci * 512:(ci + 1) * 512], in_=osb[qs][:]),
                       reads=[b_osb[qs]], writes=[])
        sc.barrier()


def phase_D(cx):
    nc, sc, dr = cx.nc, cx.sc, cx.dr
    with contextlib.ExitStack() as es:
        sb, ps = _pool(cx, es)
        kT = sb([128, 2, S], BF16, "kTb")
        vb = sb([128, NT, 256], BF16, "vb")
        b_kv = Buf("kvb")
        for g_ in range(2):
            sc.dma("sp", "ld", lambda g, g_=g_: g.dma_start(out=kT[:, g_, :], in_=dr["kT_d"][2 + g_, :, :]),
                   reads=[cx.b_kT], writes=[b_kv])
        sc.dma("sp", "ld", lambda g: g.dma_start(out=vb[:], in_=dr["v_d"].rearrange("(t p) n -> p t n", p=128)[:, :, 256:512]),
               reads=[cx.b_v], writes=[b_kv])
        cx.mask_drain()
        oh, negm, erb, negfull, bm4 = cx.m_oh, cx.m_negm, cx.m_erb, cx.m_negfull, cx.m_bm4
        b_oh, b_rb, b_bm4 = cx.mb_oh, cx.mb_rb, cx.mb_bm4
        qh = [sb([128, S], BF16, "qh") for _ in range(2)]
        b_qh = [Buf("qh0"), Buf("qh1")]
        pt = [sb([128, 3, 512], BF16, "ptb") for _ in range(2)]
        b_pt = [[Buf(f"ptb{i}{o}") for o in range(3)] for i in range(2)]
        rden = sb([128, 512], F32, "rdenb")
        b_rden = Buf("rdb")
        osb = [sb([128, 512], BF16, "osbb") for _ in range(2)]
        b_osb = [Buf("osbb0"), Buf("osbb1")]
        st = [[ps([128, 512], F32, "stb") for _ in range(3)] for _ in range(2)]
        b_st = [[Buf(f"stb{i}{o}") for o in range(3)] for i in range(2)]
        ot = ps([128, 512], F32, "otb")
        b_ot = Buf("otb")
        dn = ps([128, 512], F32, "dnb")
        b_dn = Buf("dnb")
        units = [(hd, ci) for hd in range(cx.nheadB) for ci in range(cx.nchunk)]

        def emit_st(u):
            hd, ci = units[u]
            g_ = hd // 4
            hsl = hd % 2
            s_ = u % 2
            if ci == 0:
                sc.dma("sp", "ld", lambda g: g.dma_start(out=qh[hsl][:], in_=dr["qT_d"][8 + hd, :, :]),
                       reads=[cx.b_qT], writes=[b_qh[hsl]])
            for o in range(3):
                mm = [(st[s_][o][:, :], cx.ident_bf[:], bm4[:, hd, o, :], [cx.b_const, b_bm4[hd]])]
                for tt in range(4):
                    qt = ci * 4 + tt
                    kt = qt + o - 1
                    cols = st[s_][o][:, tt * 128:(tt + 1) * 128]
                    if 0 <= kt < NT:
                        mm.append((cols, kT[:, g_, kt * 128:(kt + 1) * 128], qh[hsl][:, qt * 128:(qt + 1) * 128], [b_kv, b_qh[hsl]]))
                    else:
                        mm.append((cols, cx.ident_bf[:], negfull[:], [cx.b_const, b_oh]))
                for i, (o_ap, l_ap, r_ap, rd) in enumerate(mm):
                    sc.op("pe", lambda t, o_ap=o_ap, l_ap=l_ap, r_ap=r_ap, i=i: t.matmul(
                        o_ap, lhsT=l_ap, rhs=r_ap, start=(i == 0), stop=(i == len(mm) - 1)),
                        reads=rd, writes=[b_st[s_][o]])

        emit_st(0)
        for u in range(len(units)):
            hd, ci = units[u]
            g_ = hd // 4
            s_ = u % 2
            if u + 1 < len(units):
                emit_st(u + 1)
            for o in range(3):
                sc.op("act", lambda a, o=o: a.activation(out=pt[s_][:, o, :], in_=st[s_][o][:], func=AF.Exp, scale=SCALE),
                      reads=[b_st[s_][o]], writes=[b_pt[s_][o]])
            for tt in range(4):
                qt = ci * 4 + tt
                offs = [o for o in range(3) if 0 <= qt + o - 1 < NT]
                for i, o in enumerate(offs):
                    kt = qt + o - 1
                    sc.op("pe", lambda t, o=o, kt=kt, i=i, tt=tt: t.matmul(
                        ot[:, tt * 128:(tt + 1) * 128], lhsT=vb[:, kt, g_ * 128:(g_ + 1) * 128],
                        rhs=pt[s_][:, o, tt * 128:(tt + 1) * 128], start=(i == 0), stop=(i == len(offs) - 1)),
                        reads=[b_kv, b_pt[s_][o]], writes=[b_ot])
            for o in range(3):
                sc.op("pe", lambda t, o=o: t.matmul(dn[:], lhsT=cx.ones_bf[:], rhs=pt[s_][:, o, :], start=(o == 0), stop=(o == 2)),
                      reads=[cx.b_const, b_pt[s_][o]], writes=[b_dn])
            t0 = ci * 512
            sc.op("act", lambda a: a.activation(out=rden[:], in_=dn[:], func=AF.Ln, bias=erb[:, 256 + hd:257 + hd]),
                  reads=[b_dn, b_rb], writes=[b_rden])
            sc.op("act", lambda a: a.activation(out=rden[:], in_=rden[:], func=AF.Exp, scale=-1.0), reads=[b_rden], writes=[b_rden])
            sc.op("dve", lambda v: v.tensor_tensor(out=osb[s_][:], in0=ot[:], in1=rden[:], op=ALU.mult),
                  reads=[b_ot, b_rden], writes=[b_osb[s_]])
            sc.dma("sp", "st", lambda g: g.dma_start(out=dr["mixT_d"][8 + hd, :, t0:t0 + 512], in_=osb[s_][:]),
                   reads=[b_osb[s_]], writes=[])
        sc.barrier()


def phase_E(cx, es_outer):
    nc, sc, dr = cx.nc, cx.sc, cx.dr
    sbo, pso = _pool(cx, es_outer)
    cx.logits = sbo([128, NT, NE], F32, "logits")
    cx.b_logits = Buf("logits")
    with contextlib.ExitStack() as es:
        sb, ps = _pool(cx, es)
        w_out = sb([128, 16, 2048], BF16, "w_out")
        b_wout = Buf("w_out")
        bc = sb([128, 3, 2048], F32, "bc")
        b_bc = Buf("bc")
        for j in range(3):
            sc.dma("sp", "cst", lambda g, j=j: g.dma_start(out=bc[:, j, :], in_=dr["vec_d"][j:j + 1, :].partition_broadcast(128)),
                   reads=[cx.b_vec], writes=[b_bc])
        wv = dr["w_out"].rearrange("(kc p) n -> p kc n", p=128)
        for i in range(4):
            sc.dma("pool", "wst", lambda g, i=i: g.dma_start(out=w_out[:, :, i * 512:(i + 1) * 512],
                                                            in_=wv[:, :, i * 512:(i + 1) * 512]), writes=[b_wout])
        for kc in range(16):
            sc.op("dve", lambda v, kc=kc: v.tensor_tensor(out=w_out[:, kc, :], in0=w_out[:, kc, :], in1=bc[:, 2, :], op=ALU.mult),
                  reads=[b_wout, b_bc], writes=[b_wout])
        wr = sb([128, 16, NE], F32, "wr")
        wr_hi = sb([128, 16, NE], BF16, "wr_hi")
        wr_lo = sb([128, 16, NE], BF16, "wr_lo")
        b_wr = Buf("wr")
        sc.dma("sp", "cst", lambda g: g.dma_start(out=wr[:], in_=dr["w_router"].rearrange("(kc p) e -> p kc e", p=128)),
               writes=[b_wr])
        sc.op("dve", lambda v: v.tensor_copy(out=wr_hi[:], in_=wr[:]), reads=[b_wr], writes=[b_wr])
        sc.op("dve", lambda v: v.tensor_tensor(out=wr_lo[:], in0=wr[:], in1=wr_hi[:], op=ALU.subtract), reads=[b_wr], writes=[b_wr])
        mx = [sb([128, 16, 512], BF16, "mixc") for _ in range(2)]
        b_mx = [Buf("mx0"), Buf("mx1")]
        xt = [sb([128, 2048], F32, "xtE") for _ in range(2)]
        b_xt = [Buf("xtE0"), Buf("xtE1")]
        x1 = [sb([128, 2048], F32, "x1") for _ in range(2)]
        b_x1 = [Buf("x10"), Buf("x11")]
        h2f = [sb([128, 2048], F32, "h2f") for _ in range(2)]
        b_h2f = [Buf("h2f0"), Buf("h2f1")]
        hi = [sb([128, 2048], BF16, "h2hi") for _ in range(2)]
        b_hi = [Buf("hi0"), Buf("hi1")]
        lo = [sb([128, 2048], BF16, "h2lo") for _ in range(2)]
        b_lo = [Buf("lo0"), Buf("lo1")]
        hT = [sb([128, 16, 128], BF16, "h2T") for _ in range(2)]
        b_hT = [Buf("h2Thi"), Buf("h2Tlo")]
        ss = [sb([128, 1], F32, "ssE") for _ in range(2)]
        b_ss = [Buf("ssE0"), Buf("ssE1")]
        junk = sb([128, 2048], BF16, "junkE")
        b_junk = Buf("junkE")
        xo = ps([128, 4, 512], F32, "xo")
        b_xo = [Buf(f"xo{i}") for i in range(4)]
        ptr = ps([128, 16, 128], BF16, "ptrE")
        b_ptr = Buf("ptrE")
        lg = ps([128, NE], F32, "lg")
        b_lg = Buf("lg")

        def s1(ti):
            ci, tt = ti // 4, ti % 4
            ms = ci % 2
            sl = ti % 2
            if tt == 0:
                t0 = ci * 512
                sc.dma("sp", "ld", lambda g: g.dma_start(
                    out=mx[ms][:], in_=dr["mixT_d"].rearrange("h p t -> p h t")[:, :, t0:t0 + 512]),
                    reads=[cx.b_mix], writes=[b_mx[ms]])
            sc.dma("sp", "ld", lambda g: g.dma_start(out=xt[sl][:], in_=dr["x"][ti * 128:(ti + 1) * 128, :]), writes=[b_xt[sl]])
            for nb in range(4):
                for hc in range(16):
                    sc.op("pe", lambda t, nb=nb, hc=hc: t.matmul(
                        xo[:, nb, :], lhsT=mx[ms][:, hc, tt * 128:(tt + 1) * 128], rhs=w_out[:, hc, nb * 512:(nb + 1) * 512],
                        start=(hc == 0), stop=(hc == 15)), reads=[b_mx[ms], b_wout], writes=[b_xo[nb]])
                sc.op("dve", lambda v, nb=nb: v.tensor_tensor(
                    out=x1[sl][:, nb * 512:(nb + 1) * 512], in0=xo[:, nb, :], in1=xt[sl][:, nb * 512:(nb + 1) * 512], op=ALU.add),
                    reads=[b_xo[nb], b_xt[sl]], writes=[b_x1[sl]])
            sc.dma("sp", "st", lambda g: g.dma_start(out=dr["out"][ti * 128:(ti + 1) * 128, :], in_=x1[sl][:]),
                   reads=[b_x1[sl]], writes=[])
            sc.op("act", lambda a: a.activation(out=junk[:], in_=x1[sl][:], func=AF.Square, accum_out=ss[sl][:]),
                  reads=[b_x1[sl]], writes=[b_junk, b_ss[sl]])
            sc.op("act", lambda a: a.activation(out=ss[sl][:], in_=ss[sl][:], func=AF.Sqrt, scale=1.0 / D, bias=cx.epsc[:]),
                  reads=[b_ss[sl], cx.b_const], writes=[b_ss[sl]])

        def s2(ti):
            sl = ti % 2
            sc.op("dve", lambda v: v.reciprocal(out=ss[sl][:], in_=ss[sl][:]), reads=[b_ss[sl]], writes=[b_ss[sl]])
            sc.op("dve", lambda v: v.scalar_tensor_tensor(out=h2f[sl][:], in0=x1[sl][:], scalar=ss[sl][:, 0:1], in1=bc[:, 0, :],
                                                          op0=ALU.mult, op1=ALU.mult),
                  reads=[b_x1[sl], b_ss[sl], b_bc], writes=[b_h2f[sl]])
            sc.op("dve", lambda v: v.tensor_tensor(out=h2f[sl][:], in0=h2f[sl][:], in1=bc[:, 1, :], op=ALU.add),
                  reads=[b_h2f[sl], b_bc], writes=[b_h2f[sl]])
            sc.op("act", lambda a: a.copy(out=hi[sl][:], in_=h2f[sl][:]), reads=[b_h2f[sl]], writes=[b_hi[sl]])
            sc.dma("sp", "st", lambda g: g.dma_start(out=dr["h2_d"][ti * 128:(ti + 1) * 128, :], in_=hi[sl][:]),
                   reads=[b_hi[sl]], writes=[])

        def s3(ti):
            sl = ti % 2
            sc.op("dve", lambda v: v.tensor_tensor(out=lo[sl][:], in0=h2f[sl][:], in1=hi[sl][:], op=ALU.subtract),
                  reads=[b_h2f[sl], b_hi[sl]], writes=[b_lo[sl]])
            for part, (src, bsrc) in enumerate(((hi[sl], b_hi[sl]), (lo[sl], b_lo[sl]))):
                for c in range(16):
                    sc.op("pe", lambda t, c=c, src=src: t.transpose(ptr[:, c, :], src[:, c * 128:(c + 1) * 128], cx.ident_bf[:]),
                          reads=[bsrc, cx.b_const], writes=[b_ptr])
                sc.op("act", lambda a, part=part: a.copy(out=hT[part][:], in_=ptr[:]), reads=[b_ptr], writes=[b_hT[part]])
            n_mm = 0
            for (a_, ba, w_) in ((hT[0], b_hT[0], wr_hi), (hT[0], b_hT[0], wr_lo), (hT[1], b_hT[1], wr_hi)):
                for c in range(16):
                    sc.op("pe", lambda t, a_=a_, w_=w_, c=c, n_mm=n_mm: t.matmul(
                        lg[:], lhsT=a_[:, c, :], rhs=w_[:, c, :], start=(n_mm == 0), stop=(n_mm == 47)),
                        reads=[ba, b_wr], writes=[b_lg])
                    n_mm += 1
            sc.op("act", lambda a: a.copy(out=cx.logits[:, ti, :], in_=lg[:]), reads=[b_lg], writes=[cx.b_logits])

        ntile = cx.nchunk * 4
        for i in range(ntile + 2):
            if i < ntile:
                s1(i)
            if 0 <= i - 1 < ntile:
                s2(i - 1)
            if 0 <= i - 2 < ntile:
                s3(i - 2)
        sc.barrier()


def phase_F(cx, es_outer):
    nc, sc, dr = cx.nc, cx.sc, cx.dr
    sbo, pso = _pool(cx, es_outer)
    cx.idx = sbo([128, NE, 4], I32, "idx")
    cx.gsel = sbo([128, NE, 4], F32, "gsel")
    cx.b_sel = Buf("sel")
    NIT = 30
    with contextlib.ExitStack() as es:
        sb, ps = _pool(cx, es)
        triL = sb([128, 128], BF16, "triL")
        iota = sb([128, 512], mybir.dt.float16, "iota")
        tv = sb([128, NT, 2], F32, "tv")
        ones_f = sb([128, 128], F32, "ones_f")
        b_c = Buf("cF")
        sc.dma("pool", "cst", lambda g: g.dma_start(out=triL[:], in_=dr["triL"]), writes=[b_c])
        sc.dma("pool", "cst", lambda g: g.dma_start(out=iota[:], in_=dr["iota512"]), writes=[b_c])
        sc.dma("sp", "cst", lambda g: g.dma_start(out=tv[:], in_=dr["tvals"]), writes=[b_c])
        sc.op("dve", lambda v: v.memset(ones_f[:], 1.0), writes=[b_c])
        mxv = sb([128, NT], F32, "mxv")
        sm = sb([128, NT], F32, "sm")
        aff = sb([128, NE, NT], F32, "aff")
        b_aff = Buf("aff")
        b_mx = Buf("mxv")
        b_sm = Buf("sm")
        lg = cx.logits
        sc.op("dve", lambda v: v.tensor_reduce(out=mxv[:], in_=lg[:], axis=AX.X, op=ALU.max), reads=[cx.b_logits], writes=[b_mx])
        sc.op("dve", lambda v: v.tensor_scalar(out=mxv[:], in0=mxv[:], scalar1=-1.0, scalar2=None, op0=ALU.mult), reads=[b_mx], writes=[b_mx])
        for ti in range(NT):
            sc.op("act", lambda a, ti=ti: a.activation(out=aff[:, :, ti], in_=lg[:, ti, :], func=AF.Exp, bias=mxv[:, ti:ti + 1],
                                                       accum_out=sm[:, ti:ti + 1]),
                  reads=[cx.b_logits, b_mx], writes=[b_aff, b_sm])
        sc.op("dve", lambda v: v.reciprocal(out=sm[:], in_=sm[:]), reads=[b_sm], writes=[b_sm])
        for ti in range(NT):
            sc.op("dve", lambda v, ti=ti: v.tensor_scalar(out=aff[:, :, ti], in0=aff[:, :, ti], scalar1=sm[:, ti:ti + 1], scalar2=None,
                                                          op0=ALU.mult), reads=[b_aff, b_sm], writes=[b_aff])
        lo = sb([128, NE], F32, "lo")
        mid = sb([128, NE], F32, "mid")
        gm = sb([128, NE], F32, "gm")
        cntp = sb([128, NE], F32, "cntp")
        cmp_ = sb([128, NT], F32, "cmp")
        b_lo, b_mid, b_gm, b_cntp, b_cmp = Buf("lo"), Buf("mid"), Buf("gm"), Buf("cntp"), Buf("cmp")
        tot = ps([128, NE], F32, "tot")
        b_tot = Buf("tot")
        sc.op("dve", lambda v: v.memset(lo[:], 0.0), writes=[b_lo])
        for k in range(NIT):
            w = 2.0 ** (-(k + 1))
            sc.op("dve", lambda v, w=w: v.tensor_scalar(out=mid[:], in0=lo[:], scalar1=w, scalar2=None, op0=ALU.add),
                  reads=[b_lo], writes=[b_mid])
            for e in range(NE):
                sc.op("dve", lambda v, e=e: v.tensor_scalar(out=cmp_[:], in0=aff[:, e, :], scalar1=mid[:, e:e + 1], scalar2=None,
                                                            op0=ALU.is_ge, op1=ALU.add, accum_out=cntp[:, e:e + 1]),
                      reads=[b_aff, b_mid], writes=[b_cmp, b_cntp])
            sc.op("pe", lambda t: t.matmul(tot[:], lhsT=ones_f[:], rhs=cntp[:], start=True, stop=True),
                  reads=[b_c, b_cntp], writes=[b_tot])
            sc.op("dve", lambda v: v.scalar_tensor_tensor(out=gm[:], in0=tot[:], scalar=CAP - 0.5, in1=mid[:], op0=ALU.is_gt, op1=ALU.mult),
                  reads=[b_tot, b_mid], writes=[b_gm])
            sc.op("dve", lambda v: v.tensor_tensor(out=lo[:], in0=lo[:], in1=gm[:], op=ALU.max), reads=[b_lo, b_gm], writes=[b_lo])
        maskf = sb([128, NE, NT], F32, "maskf")
        maskb = sb([128, NE, NT], BF16, "maskb")
        inc = sb([128, NE, NT], F32, "inc")
        cmx = sb([128, NE, NT], BF16, "cmx")
        posm = sb([128, NE, NT], F32, "posm")
        zer = sb([128, NT], F32, "zer")
        b_mask, b_inc, b_cmx, b_posm = Buf("mask"), Buf("inc"), Buf("cmx"), Buf("posm")
        sc.op("dve", lambda v: v.memset(zer[:], 0.0), writes=[b_c])
        for e in range(NE):
            sc.op("dve", lambda v, e=e: v.tensor_scalar(out=maskf[:, e, :], in0=aff[:, e, :], scalar1=lo[:, e:e + 1], scalar2=None,
                                                        op0=ALU.is_ge), reads=[b_aff, b_lo], writes=[b_mask])
        sc.op("dve", lambda v: v.tensor_copy(out=maskb[:], in_=maskf[:]), reads=[b_mask], writes=[b_mask])
        for e in range(NE):
            sc.op("dve", lambda v, e=e: v.tensor_tensor_scan(out=inc[:, e, :], data0=maskf[:, e, :], data1=zer[:], initial=0.0,
                                                             op0=ALU.add, op1=ALU.add), reads=[b_mask, b_c], writes=[b_inc])
        sc.op("dve", lambda v: v.tensor_tensor(out=cmx[:], in0=inc[:], in1=maskf[:], op=ALU.subtract), reads=[b_inc, b_mask], writes=[b_cmx])
        posp = ps([128, NE * NT], F32, "posp")
        b_posp = Buf("posp")
        sc.op("pe", lambda t: t.matmul(posp[:], lhsT=triL[:], rhs=maskb[:].rearrange("p e t -> p (e t)"), start=True, stop=False),
              reads=[b_c, b_mask], writes=[b_posp])
        sc.op("pe", lambda t: t.matmul(posp[:], lhsT=cx.ones_bf[:], rhs=cmx[:].rearrange("p e t -> p (e t)"), start=False, stop=True),
              reads=[cx.b_const, b_cmx], writes=[b_posp])
        sc.op("dve", lambda v: v.scalar_tensor_tensor(out=posm[:].rearrange("p e t -> p (e t)"), in0=posp[:], scalar=1.0,
                                                       in1=maskf[:].rearrange("p e t -> p (e t)"), op0=ALU.add, op1=ALU.mult),
              reads=[b_posp, b_mask], writes=[b_posm])
        sc.op("dve", lambda v: v.tensor_scalar(out=posm[:], in0=posm[:], scalar1=-1.0, scalar2=None, op0=ALU.add),
              reads=[b_posm], writes=[b_posm])
        vals = sb([128, NE, NT, 4], BF16, "vals")
        b_vals = Buf("vals")
        afr = sb([128, NE, NT], F32, "afr")
        b_afr = Buf("afr")
        for e in range(NE):
            sc.op("dve", lambda v, e=e: v.tensor_copy(out=vals[:, e, :, 0:2], in_=tv[:]), reads=[b_c], writes=[b_vals])
        sc.op("dve", lambda v: v.tensor_copy(out=vals[:, :, :, 2], in_=aff[:]), reads=[b_aff], writes=[b_vals])
        sc.op("dve", lambda v: v.tensor_tensor(out=afr[:], in0=aff[:], in1=vals[:, :, :, 2], op=ALU.subtract),
              reads=[b_aff, b_vals], writes=[b_afr])
        sc.op("dve", lambda v: v.tensor_copy(out=vals[:, :, :, 3], in_=afr[:]), reads=[b_afr], writes=[b_vals])
        oh = [sb([128, 512], BF16, "ohF") for _ in range(3)]
        b_oh = [Buf(f"ohF{i}") for i in range(3)]
        selp = [ps([128, 4], F32, "selp") for _ in range(4)]
        b_selp = [Buf(f"selp{i}") for i in range(4)]
        idxf = sb([128, 4], F32, "idxf")
        b_idxf = Buf("idxf")
        selsb = sb([128, 4, 4], F32, "selsb")
        b_selsb = Buf("selsb")
        n = 0
        for e in range(cx.nexp):
            sp_ = e % 2
            for ti in range(NT):
                o = n % 3
                n += 1
                sc.op("dve", lambda v, o=o, e=e, ti=ti: v.tensor_scalar(out=oh[o][:], in0=iota[:], scalar1=posm[:, e, ti:ti + 1],
                                                                        scalar2=None, op0=ALU.is_equal),
                      reads=[b_c, b_posm], writes=[b_oh[o]])
                for jt in range(4):
                    sc.op("pe", lambda t, o=o, e=e, ti=ti, jt=jt: t.matmul(
                        selp[jt][:], lhsT=oh[o][:, jt * 128:(jt + 1) * 128], rhs=vals[:, e, ti, :],
                        start=(ti == 0), stop=(ti == NT - 1)), reads=[b_oh[o], b_vals], writes=[b_selp[jt]])
            for jt in range(4):
                sc.op("dve", lambda v, jt=jt: v.tensor_copy(out=selsb[:, jt, :], in_=selp[jt][:]), reads=[b_selp[jt]], writes=[b_selsb])
            sc.op("dve", lambda v: v.scalar_tensor_tensor(out=idxf[:], in0=selsb[:, :, 0], scalar=64.0, in1=selsb[:, :, 1],
                                                          op0=ALU.mult, op1=ALU.add), reads=[b_selsb], writes=[b_idxf])
            sc.op("dve", lambda v: v.tensor_scalar(out=idxf[:], in0=idxf[:], scalar1=0.0, scalar2=float(S - 1), op0=ALU.max, op1=ALU.min),
                  reads=[b_idxf], writes=[b_idxf])
            sc.op("dve", lambda v, e=e: v.tensor_copy(out=cx.idx[:, e, :], in_=idxf[:]), reads=[b_idxf], writes=[cx.b_sel])
            sc.op("dve", lambda v, e=e: v.tensor_tensor(out=cx.gsel[:, e, :], in0=selsb[:, :, 2], in1=selsb[:, :, 3],
                                                        op=ALU.add), reads=[b_selsb], writes=[cx.b_sel])
        if cx.dbg is not None and "dbg_idx" in cx.dbg:
            sc.dma("sp", "st", lambda g: g.dma_start(out=cx.dbg["dbg_idx"], in_=cx.idx[:]), reads=[cx.b_sel])
            sc.dma("sp", "st", lambda g: g.dma_start(out=cx.dbg["dbg_g"], in_=cx.gsel[:]), reads=[cx.b_sel])
            sc.dma("sp", "st", lambda g: g.dma_start(out=cx.dbg["dbg_aff"], in_=aff[:]), reads=[b_aff])
            sc.dma("sp", "st", lambda g: g.dma_start(out=cx.dbg["dbg_lo"], in_=lo[:]), reads=[b_lo])
        sc.barrier()


def phase_G(cx):
    nc, sc, dr = cx.nc, cx.sc, cx.dr
    with contextlib.ExitStack() as es:
        sb, ps = _pool(cx, es)
        gt2 = sb([128, 2048], F32, "gt2bc")
        b_gt2 = Buf("gt2")
        sc.dma("sp", "cst", lambda g: g.dma_start(out=gt2[:], in_=dr["vec_d"][3:4, :].partition_broadcast(128)),
               reads=[cx.b_vec], writes=[b_gt2])
        wg = [sb([128, 16, 512], BF16, "wg") for _ in range(2)]
        wu = [sb([128, 16, 512], BF16, "wu") for _ in range(2)]
        wd = [sb([128, 16, 512], BF16, "wd") for _ in range(2)]
        b_wg = [Buf("wg0"), Buf("wg1")]
        b_wu = [Buf("wu0"), Buf("wu1")]
        b_wd = [Buf("wd0"), Buf("wd1")]
        xg = sb([128, 4, 2048], BF16, "xg")
        b_xg = [Buf(f"xg{j}") for j in range(4)]
        xgT = sb([128, 16, 512], BF16, "xgT")
        b_xgT = Buf("xgT")
        hT = sb([128, 16, 512], BF16, "hTG")
        b_hT = Buf("hTG")
        sa = [sb([128, 512], F32, "sa") for _ in range(2)]
        b_sa = [Buf("sa0"), Buf("sa1")]
        ysb = sb([128, 4, 2048], F32, "ysb")
        b_ysb = [Buf(f"ysb{j}") for j in range(4)]
        tp = [ps([128, 512], BF16, "tpG") for _ in range(2)]
        b_tp = [Buf("tpG0"), Buf("tpG1")]
        ap_ = [ps([128, 512], F32, "apG") for _ in range(2)]
        up_ = [ps([128, 512], F32, "upG") for _ in range(2)]
        yp = [ps([128, 512], F32, "ypG") for _ in range(2)]
        b_ap = [Buf("ap0"), Buf("ap1")]
        b_up = [Buf("up0"), Buf("up1")]
        b_yp = [Buf("yp0"), Buf("yp1")]
        wgv = dr["w_gate"].rearrange("e (kc p) n -> e p kc n", p=128)
        wuv = dr["w_up"].rearrange("e (kc p) n -> e p kc n", p=128)
        wdv = dr["w_down"].rearrange("e (kc p) n -> e p kc n", p=128)
        NX = cx.nexp

        def gather(e):
            for jt in range(4):
                sc.dma("pool", "gat", lambda g, jt=jt: g.indirect_dma_start(
                    out=xg[:, jt, :], out_offset=None, in_=dr["h2_d"],
                    in_offset=bass.IndirectOffsetOnAxis(ap=cx.idx[:, e, jt:jt + 1], axis=0)),
                    reads=[cx.b_sel, cx.b_h2], writes=[b_xg[jt]])

        def load_s1(e, fb):
            s_ = fb % 2
            sc.dma("pool", "wst", lambda g: g.dma_start(out=wg[s_][:], in_=wgv[e, :, :, fb * 512:(fb + 1) * 512]), writes=[b_wg[s_]])
            sc.dma("pool", "wst", lambda g: g.dma_start(out=wu[s_][:], in_=wuv[e, :, :, fb * 512:(fb + 1) * 512]), writes=[b_wu[s_]])

        def load_s2(e, nb):
            s_ = nb % 2
            sc.dma("pool", "wst", lambda g: g.dma_start(out=wd[s_][:], in_=wdv[e, :, :, nb * 512:(nb + 1) * 512]), writes=[b_wd[s_]])

        gather(0)
        load_s1(0, 0)
        load_s1(0, 1)
        cnt = 0
        for e in range(NX):
            for dc in range(16):
                ts_ = dc % 2
                for jt in range(4):
                    sc.op("pe", lambda t, dc=dc, jt=jt, ts_=ts_: t.transpose(
                        tp[ts_][:, jt * 128:(jt + 1) * 128], xg[:, jt, dc * 128:(dc + 1) * 128], cx.ident_bf[:]),
                        reads=[b_xg[jt], cx.b_const], writes=[b_tp[ts_]])
                if dc % 2 == 0:
                    sc.op("act", lambda a, dc=dc, ts_=ts_: a.copy(out=xgT[:, dc, :], in_=tp[ts_][:]), reads=[b_tp[ts_]], writes=[b_xgT])
                else:
                    sc.op("dve", lambda v, dc=dc, ts_=ts_: v.tensor_copy(out=xgT[:, dc, :], in_=tp[ts_][:]), reads=[b_tp[ts_]], writes=[b_xgT])
            if e + 1 < NX:
                gather(e + 1)
            for fb in range(4):
                s_ = fb % 2
                for fc in range(4):
                    f = fb * 4 + fc
                    q = cnt % 2
                    cnt += 1
                    for dc in range(16):
                        sc.op("pe", lambda t, dc=dc, fc=fc, s_=s_, q=q: t.matmul(
                            ap_[q][:], lhsT=wg[s_][:, dc, fc * 128:(fc + 1) * 128], rhs=xgT[:, dc, :],
                            start=(dc == 0), stop=(dc == 15)), reads=[b_wg[s_], b_xgT], writes=[b_ap[q]])
                    for dc in range(16):
                        sc.op("pe", lambda t, dc=dc, fc=fc, s_=s_, q=q: t.matmul(
                            up_[q][:], lhsT=wu[s_][:, dc, fc * 128:(fc + 1) * 128], rhs=xgT[:, dc, :],
                            start=(dc == 0), stop=(dc == 15)), reads=[b_wu[s_], b_xgT], writes=[b_up[q]])
                    sc.op("act", lambda a, q=q: a.activation(out=sa[q][:], in_=ap_[q][:], func=AF.Silu), reads=[b_ap[q]], writes=[b_sa[q]])
                    sc.op("dve", lambda v, q=q, f=f: v.tensor_tensor(out=hT[:, f, :], in0=sa[q][:], in1=up_[q][:], op=ALU.mult),
                          reads=[b_sa[q], b_up[q]], writes=[b_hT])
                if fb + 2 < 4:
                    load_s1(e, fb + 2)
                if fb == 0:
                    load_s2(e, 0)
                if fb == 1:
                    load_s2(e, 1)
            for nb in range(4):
                s_ = nb % 2
                for jt in range(4):
                    q = cnt % 2
                    cnt += 1
                    for fc in range(16):
                        sc.op("pe", lambda t, fc=fc, jt=jt, s_=s_, q=q: t.matmul(
                            yp[q][:], lhsT=hT[:, fc, jt * 128:(jt + 1) * 128], rhs=wd[s_][:, fc, :],
                            start=(fc == 0), stop=(fc == 15)), reads=[b_hT, b_wd[s_]], writes=[b_yp[q]])
                    sc.op("dve", lambda v, jt=jt, nb=nb, q=q: v.scalar_tensor_tensor(
                        out=ysb[:, jt, nb * 512:(nb + 1) * 512], in0=yp[q][:], scalar=cx.gsel[:, e, jt:jt + 1],
                        in1=gt2[:, nb * 512:(nb + 1) * 512], op0=ALU.mult, op1=ALU.mult),
                        reads=[b_yp[q], cx.b_sel, b_gt2], writes=[b_ysb[jt]])
                if nb + 2 < 4:
                    load_s2(e, nb + 2)
                elif e + 1 < NX:
                    load_s1(e + 1, nb - 2)
            for jt in range(4):
                sc.dma("pool", "sca", lambda g, jt=jt: g.indirect_dma_start(
                    out=dr["out"], out_offset=bass.IndirectOffsetOnAxis(ap=cx.idx[:, e, jt:jt + 1], axis=0),
                    in_=ysb[:, jt, :], in_offset=None, compute_op=ALU.add),
                    reads=[b_ysb[jt], cx.b_sel], writes=[cx.b_out])
        sc.barrier()


def build(upto="G", dbg_names=(), nchunk=8, nheadA=8, nheadB=8, dbg_scratch=False, nexp=16):
    nc = bass.Bass("TRN2", target_bir_lowering=False)
    cx = Ctx()
    cx.nc = nc
    cx.uid = 0
    cx.nchunk = nchunk
    cx.nheadA = nheadA
    cx.nheadB = nheadB
    cx.nexp = nexp
    dr = {}

    def ext(name, shape, dt=F32):
        dr[name] = nc.dram_tensor(name, list(shape), dt, kind="ExternalInput").ap()

    ext("x", [S, D])
    ext("cT", [128, 16])
    ext("w_ada", [D, 6 * D])
    ext("b_ada", [1, 6 * D])
    ext("g1T", [128, 16])
    ext("g2T", [128, 16])
    ext("w_in", [D, 3072])
    ext("gains", [128, 4])
    ext("rbs", [1, 264])
    ext("w_out", [D, D])
    ext("w_router", [D, NE])
    ext("w_gate", [NE, D, D])
    ext("w_up", [NE, D, D])
    ext("w_down", [NE, D, D])
    ext("ident", [128, 128])
    ext("rotT", [128, 128])
    ext("cosT", [128, S])
    ext("sinT", [128, S])
    ext("oh", [128, 32, 384])
    ext("negm", [128, 384])
    ext("triL", [128, 128])
    ext("iota512", [128, 512])
    ext("tvals", [128, NT, 2])
    dr["out"] = nc.dram_tensor("out", [S, D], F32, kind="ExternalOutput").ap()
    kw = {"kind": "ExternalOutput"} if dbg_scratch else {}
    dr["vec_d"] = nc.dram_tensor("vec_d", [6, D], F32, **kw).ap()
    dr["qT_d"] = nc.dram_tensor("qT_d", [16, 128, S], BF16, **kw).ap()
    dr["kT_d"] = nc.dram_tensor("kT_d", [4, 128, S], BF16, **kw).ap()
    dr["v_d"] = nc.dram_tensor("v_d", [S, 512], BF16, **kw).ap()
    dr["mixT_d"] = nc.dram_tensor("mixT_d", [16, 128, S], BF16, **kw).ap()
    dr["h2_d"] = nc.dram_tensor("h2_d", [S, D], BF16, **kw).ap()
    cx.dr = dr
    cx.dbg = {}
    dbg_shapes = {"dbg_mod": ([128, 96], F32), "dbg_idx": ([128, NE, 4], I32), "dbg_g": ([128, NE, 4], F32),
                  "dbg_aff": ([128, NE, NT], F32), "dbg_lo": ([128, NE], F32)}
    for n_ in dbg_names:
        shp, dt = dbg_shapes[n_]
        cx.dbg[n_] = nc.dram_tensor(n_, shp, dt, kind="ExternalOutput").ap()
    cx.b_vec = Buf("vec_d")
    cx.b_qT = Buf("qT_d")
    cx.b_kT = Buf("kT_d")
    cx.b_v = Buf("v_d")
    cx.b_mix = Buf("mixT_d")
    cx.b_h2 = Buf("h2_d")
    cx.b_out = Buf("out")
    with contextlib.ExitStack() as es:
        sc = Sched(nc, es)
        cx.sc = sc
        phase_consts(cx, es)
        order = "ABCDEFG"
        n = order.index(upto)
        phase_A(cx)
        if n >= 1:
            phase_B(cx)
        es_m = contextlib.ExitStack()
        if n >= 2:
            mask_prepare(cx, es_m)
            phase_C(cx)
        if n >= 3:
            phase_D(cx)
        es_m.close()
        if n >= 4:
            phase_E(cx, es)
        if n >= 5:
            phase_F(cx, es)
        if n >= 6:
            phase_G(cx)
        sc.barrier()
    return nc, cx


def _t5_bucket(rel):
    nb = 16
    ret = np.where(rel > 0, nb, 0)
    n = np.abs(rel)
    max_exact = 8
    nf = np.maximum(n, 1).astype(np.float32)
    large = max_exact + (np.log(nf / np.float32(max_exact)) / np.float32(math.log(128 / max_exact))
                         * np.float32(nb - max_exact)).astype(np.int32)
    large = np.minimum(large, nb - 1)
    return ret + np.where(n < max_exact, n, large)


_CONST = {}


def _consts():
    if _CONST:
        return _CONST
    c = {}
    c["ident"] = np.eye(128, dtype=np.float32)
    rot = np.zeros((128, 128), np.float32)
    for m in range(128):
        if (m % 64) < 32:
            rot[m + 32, m] = -1.0
        else:
            rot[m - 32, m] = 1.0
    c["rotT"] = rot
    t = np.arange(S)
    row = (t // 64).astype(np.float32)
    col = (t % 64).astype(np.float32)
    inv = (1.0 / (np.float32(10000.0) ** (np.arange(0, 64, 2, dtype=np.float32) / np.float32(64)))).astype(np.float32)
    ang_r = (row[:, None] * inv[None, :]).astype(np.float32)
    ang_c = (col[:, None] * inv[None, :]).astype(np.float32)
    cosT = np.concatenate([np.cos(ang_r), np.cos(ang_r), np.cos(ang_c), np.cos(ang_c)], axis=1).T
    sinT = np.concatenate([np.sin(ang_r), np.sin(ang_r), np.sin(ang_c), np.sin(ang_c)], axis=1).T
    c["cosT"] = np.ascontiguousarray(cosT.astype(np.float32))
    c["sinT"] = np.ascontiguousarray(sinT.astype(np.float32))
    k = np.arange(128)[:, None, None]
    off = np.arange(3)[None, :, None] - 1
    q = np.arange(128)[None, None, :]
    rel = k + off * 128 - q
    valid = np.abs(rel) <= 128
    bk = _t5_bucket(rel)
    oh = np.zeros((128, 32, 3, 128), np.float32)
    for b in range(32):
        oh[:, b] = ((bk == b) & valid).astype(np.float32)
    c["oh"] = np.ascontiguousarray(oh.reshape(128, 32, 384))
    c["negm"] = np.ascontiguousarray(np.where(valid, 0.0, -30000.0).astype(np.float32).reshape(128, 384))
    tri = (np.arange(128)[:, None] < np.arange(128)[None, :]).astype(np.float32)
    c["triL"] = tri
    c["iota512"] = np.ascontiguousarray(np.broadcast_to(np.arange(512, dtype=np.float32), (128, 512)))
    tg = np.arange(NT)[None, :] * 128 + np.arange(128)[:, None]
    c["tvals"] = np.ascontiguousarray(np.stack([tg // 64, tg % 64], axis=-1).astype(np.float32))
    _CONST.update(c)
    return _CONST


def make_in_map(b, x, c, w_ada, b_ada, g_norm1, w_in, qn_a, kn_a, qn_b, kn_b, sink_b, rel_bias, w_out, g_norm2,
                w_router, w_gate, w_up, w_down):
    f = lambda a: np.ascontiguousarray(np.asarray(a, dtype=np.float32))
    m = dict(_consts())
    m["x"] = f(x[b])
    m["cT"] = f(np.asarray(c[b]).reshape(16, 128).T)
    m["w_ada"] = f(w_ada)
    m["b_ada"] = f(np.asarray(b_ada).reshape(1, -1))
    m["g1T"] = f(np.asarray(g_norm1).reshape(16, 128).T)
    m["g2T"] = f(np.asarray(g_norm2).reshape(16, 128).T)
    m["w_in"] = f(w_in)
    m["gains"] = f(np.stack([np.asarray(qn_a), np.asarray(kn_a), np.asarray(qn_b), np.asarray(kn_b)], axis=1))
    m["rbs"] = f(np.concatenate([np.asarray(rel_bias).reshape(-1), np.asarray(sink_b).reshape(-1)]).reshape(1, 264))
    m["w_out"] = f(w_out)
    m["w_router"] = f(w_router)
    m["w_gate"] = f(w_gate)
    m["w_up"] = f(w_up)
    m["w_down"] = f(w_down)
    return m


_NC = {}


def kernel(**inputs):
    if "nc" not in _NC:
        _NC["nc"] = build("G")[0]
    nc = _NC["nc"]
    in_maps = [make_in_map(i % 4, **inputs) for i in range(N_CORES)]
    res = run_bass_kernel_spmd(nc, in_maps, core_ids=list(range(N_CORES)))
    out = np.stack([np.asarray(res.results[b]["out"]) for b in range(4)], axis=0)
    return out.astype(np.float32)
```

```python
import contextlib
import math

import numpy as np

import concourse.bass as bass
import concourse.mybir as mybir
from concourse.bass_utils import run_bass_kernel_spmd

F32 = mybir.dt.float32
BF16 = mybir.dt.bfloat16
I32 = mybir.dt.int32
AF = mybir.ActivationFunctionType
ALU = mybir.AluOpType
AX = mybir.AxisListType

S = 4096
D = 2048
NT = S // 128
NE = 16
CAP = 512
EPS = 1e-6
SCALE = 1.0 / math.sqrt(128.0)
N_CORES = 8


class Buf:
    __slots__ = ("name", "w", "r", "dsem")

    def __init__(self, name):
        self.name = name
        self.w = None
        self.r = {}
        self.dsem = None


class Sched:
    LIMIT = 30000

    def __init__(self, nc, es):
        self.nc = nc
        self.es = es
        self.eng = {"pe": nc.tensor, "act": nc.scalar, "dve": nc.vector,
                    "pool": nc.gpsimd, "sp": nc.sync}
        self.sem = {}
        self.cnt = {}
        self.nsem = 0
        self.waited = {e: {} for e in self.eng}
        self.dbufs = []
        self.retired = []
        for e in self.eng:
            self._newsem(e)

    def _mk(self, tag):
        self.nsem += 1
        return self.es.enter_context(self.nc.semaphore(f"s{self.nsem}_{tag}"))

    def _newsem(self, e):
        if e in self.sem and self.cnt[e] > 0:
            self.retired.append((self.sem[e][0], self.sem[e][1], self.cnt[e], e))
        self.sem[e] = (self._mk(e), self.nsem)
        self.cnt[e] = 0

    def _wait(self, e, tok):
        if tok is None:
            return
        s, key, v, src = tok
        if src == "pe" and e == "pe":
            return
        if self.waited[e].get(key, 0) >= v:
            return
        self.eng[e].wait_ge(s, v)
        self.waited[e][key] = v

    def _deps(self, e, reads, writes):
        for b in reads:
            self._wait(e, b.w)
        for b in writes:
            self._wait(e, b.w)
            for t in b.r.values():
                self._wait(e, t)

    def _mark(self, tok, reads, writes):
        for b in reads:
            b.r[tok[1]] = tok
        for b in writes:
            b.w = tok
            b.r = {}

    def op(self, e, fn, reads=(), writes=()):
        self._deps(e, reads, writes)
        if self.cnt[e] >= self.LIMIT:
            self._newsem(e)
        ins = fn(self.eng[e])
        self.cnt[e] += 1
        s, key = self.sem[e]
        ins.then_inc(s, 1)
        tok = (s, key, self.cnt[e], e)
        self._mark(tok, reads, writes)
        return tok

    def dma(self, e, chan, fn, reads=(), writes=()):
        self._deps(e, reads, writes)
        owner = writes[0] if len(writes) else reads[0]
        if owner.dsem is None:
            owner.dsem = [self._mk("d_" + owner.name), self.nsem, 0]
            self.dbufs.append(owner)
        c = owner.dsem
        ins = fn(self.eng[e])
        c[2] += 1
        ins.then_inc(c[0], 16)
        tok = (c[0], c[1], 16 * c[2], "dma")
        self._mark(tok, reads, writes)
        return tok

    def all_tokens(self):
        toks = list(self.retired)
        for e in self.eng:
            if self.cnt[e] > 0:
                s, key = self.sem[e]
                toks.append((s, key, self.cnt[e], e))
        for bf in self.dbufs:
            c = bf.dsem
            toks.append((c[0], c[1], 16 * c[2], "dma"))
        return toks

    def barrier(self):
        toks = self.all_tokens()
        for e in self.eng:
            for t in toks:
                if t[3] == e:
                    continue
                self._wait(e, t)


class Ctx:
    pass


def _pool(cx, es):
    nc = cx.nc
    n = [0]

    def sb(shape, dt, name=None):
        n[0] += 1
        cx.uid += 1
        return es.enter_context(nc.sbuf_tensor(f"{name or 't'}_{cx.uid}", list(shape), dt))

    def ps(shape, dt, name=None):
        cx.uid += 1
        return es.enter_context(nc.psum_tensor(f"{name or 'p'}_{cx.uid}", list(shape), dt))

    return sb, ps


def phase_consts(cx, es):
    nc, sc, dr = cx.nc, cx.sc, cx.dr
    sb, ps = _pool(cx, es)
    cx.ident_bf = sb([128, 128], BF16, "identbf")
    cx.ident_f = sb([128, 128], F32, "identf")
    cx.ones_bf = sb([128, 128], BF16, "onesbf")
    cx.b_const = Buf("consts")
    sc.dma("pool", "cst", lambda g: g.dma_start(out=cx.ident_bf[:], in_=dr["ident"]), writes=[cx.b_const])
    sc.dma("sp", "cst", lambda g: g.dma_start(out=cx.ident_f[:], in_=dr["ident"]), writes=[cx.b_const])
    sc.op("dve", lambda v: v.memset(cx.ones_bf[:], 1.0), writes=[cx.b_const])
    cx.A1col = sb([128, 16], F32, "A1col")
    cx.B1col = sb([128, 16], F32, "B1col")
    cx.b_ab1 = Buf("ab1")
    cx.epsc = sb([128, 1], F32, "epsc")
    sc.op("dve", lambda v: v.memset(cx.epsc[:], EPS), writes=[cx.b_const])


def phase_A(cx):
    nc, sc, dr = cx.nc, cx.sc, cx.dr
    with contextlib.ExitStack() as es:
        sb, ps = _pool(cx, es)
        cT = sb([128, 16], F32)
        sT = sb([128, 16], F32)
        g1T = sb([128, 16], F32)
        g2T = sb([128, 16], F32)
        wblk = [sb([128, 16, 512], BF16, "wada") for _ in range(3)]
        modT = sb([128, 96], F32)
        m2 = sb([128, 96, 2], F32)
        s2 = sb([128, 16, 2], BF16)
        sres = sb([128, 16], F32)
        bT96 = sb([96, 128], F32)
        v4 = sb([128, 96], F32)
        v4T = sb([96, 128], F32)
        modp = ps([128, 96, 2], F32)
        bTp = ps([128, 96], F32)
        v4p = ps([96, 128], F32)
        b_small = Buf("small")
        b_w = [Buf("wada0"), Buf("wada1"), Buf("wada2")]
        b_modp = Buf("modp")
        b_modT = Buf("modT")
        b_bTp = Buf("bTp")
        b_v4 = Buf("v4")
        b_v4p = Buf("v4p")
        b_v4T = Buf("v4T")
        sc.dma("sp", "ld", lambda g: g.dma_start(out=cT[:], in_=dr["cT"]), writes=[b_small])
        sc.dma("sp", "ld", lambda g: g.dma_start(out=g1T[:], in_=dr["g1T"]), writes=[b_small])
        sc.dma("sp", "ld", lambda g: g.dma_start(out=g2T[:], in_=dr["g2T"]), writes=[b_small])
        sc.dma("sp", "ld", lambda g: g.dma_start(out=bT96[:], in_=dr["b_ada"].rearrange("o (n p) -> (o n) p", p=128)), writes=[b_small])
        sc.op("act", lambda a: a.activation(out=sT[:], in_=cT[:], func=AF.Silu), reads=[b_small], writes=[b_small])
        sc.op("dve", lambda v: v.tensor_copy(out=s2[:, :, 0], in_=sT[:]), reads=[b_small], writes=[b_small])
        sc.op("dve", lambda v: v.tensor_tensor(out=sres[:], in0=sT[:], in1=s2[:, :, 0], op=ALU.subtract), reads=[b_small], writes=[b_small])
        sc.op("dve", lambda v: v.tensor_copy(out=s2[:, :, 1], in_=sres[:]), reads=[b_small], writes=[b_small])
        sc.op("pe", lambda t: t.transpose(bTp[:], bT96[:], cx.ident_f[0:96, 0:96]), reads=[b_small, cx.b_const], writes=[b_bTp])
        wv = dr["w_ada"].rearrange("(kc p) n -> p kc n", p=128)
        for nb in range(24):
            slot = nb % 3
            w = wblk[slot]
            sc.dma("pool", "wst", lambda g, w=w, nb=nb: g.dma_start(out=w[:], in_=wv[:, :, nb * 512:(nb + 1) * 512]),
                   writes=[b_w[slot]])
            for j in range(4):
                n = nb * 4 + j
                for kc in range(16):
                    sc.op("pe", lambda t, w=w, j=j, kc=kc, n=n: t.matmul(
                        modp[:, n, :], lhsT=w[:, kc, j * 128:(j + 1) * 128], rhs=s2[:, kc, :],
                        start=(kc == 0), stop=(kc == 15)), reads=[b_w[slot], b_small], writes=[b_modp])
        sc.op("dve", lambda v: v.tensor_copy(out=m2[:], in_=modp[:]), reads=[b_modp], writes=[b_modT])
        sc.op("dve", lambda v: v.tensor_tensor(out=modT[:], in0=m2[:, :, 0], in1=m2[:, :, 1], op=ALU.add), reads=[b_modT], writes=[b_modT])
        sc.op("dve", lambda v: v.tensor_tensor(out=modT[:], in0=modT[:], in1=bTp[:], op=ALU.add), reads=[b_modT, b_bTp], writes=[b_modT])
        sc.op("dve", lambda v: v.scalar_tensor_tensor(out=cx.A1col[:], in0=modT[:, 16:32], scalar=1.0, in1=g1T[:],
                                                       op0=ALU.add, op1=ALU.mult),
              reads=[b_modT, b_small], writes=[cx.b_ab1])
        sc.op("dve", lambda v: v.tensor_copy(out=cx.B1col[:], in_=modT[:, 0:16]), reads=[b_modT], writes=[cx.b_ab1])
        sc.op("dve", lambda v: v.scalar_tensor_tensor(out=v4[:, 0:16], in0=modT[:, 64:80], scalar=1.0, in1=g2T[:],
                                                       op0=ALU.add, op1=ALU.mult),
              reads=[b_modT, b_small], writes=[b_v4])
        sc.op("dve", lambda v: v.tensor_copy(out=v4[:, 16:32], in_=modT[:, 48:64]), reads=[b_modT], writes=[b_v4])
        sc.op("dve", lambda v: v.tensor_copy(out=v4[:, 32:48], in_=modT[:, 32:48]), reads=[b_modT], writes=[b_v4])
        sc.op("dve", lambda v: v.tensor_copy(out=v4[:, 48:64], in_=modT[:, 80:96]), reads=[b_modT], writes=[b_v4])
        sc.op("dve", lambda v: v.tensor_copy(out=v4[:, 64:80], in_=cx.A1col[:]), reads=[cx.b_ab1], writes=[b_v4])
        sc.op("dve", lambda v: v.tensor_copy(out=v4[:, 80:96], in_=cx.B1col[:]), reads=[cx.b_ab1], writes=[b_v4])
        sc.op("pe", lambda t: t.transpose(v4p[:], v4[:], cx.ident_f[:]), reads=[b_v4, cx.b_const], writes=[b_v4p])
        sc.op("dve", lambda v: v.tensor_copy(out=v4T[:], in_=v4p[:]), reads=[b_v4p], writes=[b_v4T])
        sc.dma("sp", "st", lambda g: g.dma_start(out=dr["vec_d"].rearrange("j (c p) -> (j c) p", p=128), in_=v4T[:]),
               reads=[b_v4T], writes=[cx.b_vec])
        if cx.dbg is not None and "dbg_mod" in cx.dbg:
            sc.dma("sp", "st", lambda g: g.dma_start(out=cx.dbg["dbg_mod"], in_=modT[:]), reads=[b_modT])
        sc.barrier()


def _head_cols():
    cols = []
    for h in range(8):
        cols.append(("qa", h, h * 128))
    for h in range(2):
        cols.append(("ka", h, 1024 + h * 128))
    for h in range(8):
        cols.append(("qb", h, 1536 + h * 128))
    for h in range(2):
        cols.append(("kb", h, 1536 + 1024 + h * 128))
    return cols


def phase_B(cx):
    nc, sc, dr = cx.nc, cx.sc, cx.dr
    with contextlib.ExitStack() as es:
        sb, ps = _pool(cx, es)
        w_in = sb([128, 16, 3072], BF16, "w_in")
        b_wins = [Buf(f"w_in{i}") for i in range(6)]
        wv = dr["w_in"].rearrange("(kc p) n -> p kc n", p=128)
        for i in (0, 1, 2, 3, 4, 5):
            sc.dma("pool", "wst", lambda g, i=i: g.dma_start(out=w_in[:, :, i * 512:(i + 1) * 512],
                                                            in_=wv[:, :, i * 512:(i + 1) * 512]), writes=[b_wins[i]])
        ab1A = sb([128, 2048], F32, "ab1A")
        ab1B = sb([128, 2048], BF16, "ab1B")
        b_ab1bc = Buf("ab1bc")
        sc.dma("sp", "cst", lambda g: g.dma_start(out=ab1A[:], in_=dr["vec_d"][4:5, :].partition_broadcast(128)),
               reads=[cx.b_vec], writes=[b_ab1bc])
        sc.dma("pool", "cst", lambda g: g.dma_start(out=ab1B[:], in_=dr["vec_d"][5:6, :].partition_broadcast(128)),
               reads=[cx.b_vec], writes=[b_ab1bc])
        rotT = sb([128, 128], BF16, "rotT")
        gains = sb([128, 4], F32, "gains")
        b_c = Buf("bconst")
        sc.dma("pool", "cst", lambda g: g.dma_start(out=rotT[:], in_=dr["rotT"]), writes=[b_c])
        sc.dma("sp", "cst", lambda g: g.dma_start(out=gains[:], in_=dr["gains"]), writes=[b_c])
        xt = [sb([128, 2048], F32, "xt") for _ in range(2)]
        b_xt = [Buf("xt0"), Buf("xt1")]
        xs = [sb([128, 2048], BF16, "xs") for _ in range(2)]
        b_xs = [Buf("xs0"), Buf("xs1")]
        ss = [sb([128, 1], F32, "ss") for _ in range(2)]
        b_ss = [Buf("ss0"), Buf("ss1")]
        hT = [sb([128, 16, 512], BF16, "hT") for _ in range(2)]
        b_hT = [Buf("hT0"), Buf("hT1")]
        cs = [sb([128, 2, 512], F32, "cs") for _ in range(2)]
        b_cs = [Buf("cs0"), Buf("cs1")]
        NS = 3
        sq = [sb([128, 512], BF16, "sq") for _ in range(NS)]
        zg = [sb([128, 512], BF16, "zg") for _ in range(NS)]
        rs = [sb([128, 512], F32, "rs") for _ in range(NS)]
        t1 = [sb([128, 512], F32, "t1") for _ in range(NS)]
        t2 = [sb([128, 512], F32, "t2") for _ in range(NS)]
        qf = [sb([128, 512], BF16, "qf") for _ in range(NS)]
        vsb = [sb([128, 512], BF16, "vsb") for _ in range(2)]
        b_sq = [Buf(f"sq{i}") for i in range(NS)]
        b_zg = [Buf(f"zg{i}") for i in range(NS)]
        b_rs = [Buf(f"rs{i}") for i in range(NS)]
        b_t1 = [Buf(f"t1{i}") for i in range(NS)]
        b_t2 = [Buf(f"t2{i}") for i in range(NS)]
        b_qf = [Buf(f"qf{i}") for i in range(NS)]
        b_vsb = [Buf("v0"), Buf("v1")]
        ptr = [ps([128, 8, 128], BF16, "ptr") for _ in range(2)]
        b_ptr = [Buf("ptr0"), Buf("ptr1")]
        zq = [ps([128, 512], F32, "zq") for _ in range(2)]
        b_zq = [Buf("zq0"), Buf("zq1")]
        ssp = [ps([128, 512], F32, "ssp") for _ in range(2)]
        b_ssp = [Buf("ssp0"), Buf("ssp1")]
        rotp = [ps([128, 512], F32, "rotp") for _ in range(2)]
        b_rotp = [Buf("rotp0"), Buf("rotp1")]
        heads = _head_cols()
        vcols = [1280, 1408, 2816, 2944]

        def load_cs(ci):
            hs = ci % 2
            t0 = ci * 512
            sc.dma("sp", "ld", lambda g: g.dma_start(out=cs[hs][:, 0, :], in_=dr["cosT"][:, t0:t0 + 512]), writes=[b_cs[hs]])
            sc.dma("sp", "ld", lambda g: g.dma_start(out=cs[hs][:, 1, :], in_=dr["sinT"][:, t0:t0 + 512]), writes=[b_cs[hs]])

        def front_a(ti):
            ci, tt = ti // 4, ti % 4
            hs = ci % 2
            xsl = ti % 2
            if tt == 0:
                load_cs(ci)
            sc.dma("sp", "ld", lambda g: g.dma_start(out=xt[xsl][:], in_=dr["x"][ti * 128:(ti + 1) * 128, :]), writes=[b_xt[xsl]])
            sc.op("act", lambda a: a.activation(out=xs[xsl][:], in_=xt[xsl][:], func=AF.Square, accum_out=ss[xsl][:]),
                  reads=[b_xt[xsl]], writes=[b_xs[xsl], b_ss[xsl]])
            sc.op("act", lambda a: a.activation(out=ss[xsl][:], in_=ss[xsl][:], func=AF.Ln, scale=1.0 / D, bias=cx.epsc[:]),
                  reads=[b_ss[xsl], cx.b_const], writes=[b_ss[xsl]])
            sc.op("act", lambda a: a.activation(out=ss[xsl][:], in_=ss[xsl][:], func=AF.Exp, scale=-0.5),
                  reads=[b_ss[xsl]], writes=[b_ss[xsl]])
            sc.op("dve", lambda v: v.scalar_tensor_tensor(out=xt[xsl][:], in0=xt[xsl][:], scalar=ss[xsl][:, 0:1], in1=ab1A[:],
                                                          op0=ALU.mult, op1=ALU.mult),
                  reads=[b_xt[xsl], b_ss[xsl], b_ab1bc], writes=[b_xt[xsl]])
            sc.op("dve", lambda v: v.tensor_tensor(out=xs[xsl][:], in0=xt[xsl][:], in1=ab1B[:], op=ALU.add),
                  reads=[b_xt[xsl], b_ab1bc], writes=[b_xs[xsl]])

        def front_b(ti):
            ci, tt = ti // 4, ti % 4
            hs = ci % 2
            xsl = ti % 2
            for half in range(2):
                pb = half
                for c8 in range(8):
                    c = half * 8 + c8
                    sc.op("pe", lambda t, c8=c8, c=c: t.transpose(ptr[pb][:, c8, :], xs[xsl][:, c * 128:(c + 1) * 128], cx.ident_bf[:]),
                          reads=[b_xs[xsl], cx.b_const], writes=[b_ptr[pb]])
                sc.op("act", lambda a: a.copy(out=hT[hs][:, half * 8:(half + 1) * 8, tt * 128:(tt + 1) * 128], in_=ptr[pb][:]),
                      reads=[b_ptr[pb]], writes=[b_hT[hs]])

        def head1(ci, k):
            kind, h, col = heads[k]
            hs = ci % 2
            zl = k % 2
            sl = k % NS
            for kc in range(16):
                sc.op("pe", lambda t, kc=kc: t.matmul(zq[zl][:], lhsT=w_in[:, kc, col:col + 128], rhs=hT[hs][:, kc, :],
                                                      start=(kc == 0), stop=(kc == 15)),
                      reads=[b_wins[col // 512], b_hT[hs]], writes=[b_zq[zl]])
            gi = {"qa": 0, "ka": 1, "qb": 2, "kb": 3}[kind]
            sc.op("act", lambda a: a.activation(out=sq[sl][:], in_=zq[zl][:], func=AF.Square), reads=[b_zq[zl]], writes=[b_sq[sl]])
            sc.op("act", lambda a: a.activation(out=zg[sl][:], in_=zq[zl][:], func=AF.Identity, scale=gains[:, gi:gi + 1]),
                  reads=[b_zq[zl], b_c], writes=[b_zg[sl]])

        def head2(ci, k):
            kind, h, col = heads[k]
            hs = ci % 2
            zl = k % 2
            sl = k % NS
            t0 = ci * 512
            sc.op("pe", lambda t: t.matmul(ssp[zl][:], lhsT=cx.ones_bf[:], rhs=sq[sl][:], start=True, stop=True),
                  reads=[b_sq[sl], cx.b_const], writes=[b_ssp[zl]])
            sc.op("act", lambda a: a.activation(out=rs[sl][:], in_=ssp[zl][:], func=AF.Ln, scale=1.0 / 128.0, bias=cx.epsc[:]),
                  reads=[b_ssp[zl], cx.b_const], writes=[b_rs[sl]])
            sc.op("act", lambda a: a.activation(out=rs[sl][:], in_=rs[sl][:], func=AF.Exp, scale=-0.5), reads=[b_rs[sl]], writes=[b_rs[sl]])
            if kind in ("qa", "ka"):
                sc.op("pe", lambda t: t.matmul(rotp[zl][:], lhsT=rotT[:], rhs=zg[sl][:], start=True, stop=True),
                      reads=[b_zg[sl], b_c], writes=[b_rotp[zl]])
                sc.op("pool", lambda g: g.tensor_tensor(out=t1[sl][:], in0=zg[sl][:], in1=cs[hs][:, 0, :], op=ALU.mult),
                      reads=[b_zg[sl], b_cs[hs]], writes=[b_t1[sl]])
                sc.op("dve", lambda v: v.tensor_tensor(out=t2[sl][:], in0=rotp[zl][:], in1=cs[hs][:, 1, :], op=ALU.mult),
                      reads=[b_rotp[zl], b_cs[hs]], writes=[b_t2[sl]])
                sc.op("dve", lambda g: g.tensor_tensor(out=t1[sl][:], in0=t1[sl][:], in1=t2[sl][:], op=ALU.add),
                      reads=[b_t1[sl], b_t2[sl]], writes=[b_t1[sl]])
                sc.op("dve", lambda v: v.tensor_tensor(out=qf[sl][:], in0=t1[sl][:], in1=rs[sl][:], op=ALU.mult),
                      reads=[b_t1[sl], b_rs[sl]], writes=[b_qf[sl]])
            else:
                sc.op("dve", lambda v: v.tensor_tensor(out=qf[sl][:], in0=zg[sl][:], in1=rs[sl][:], op=ALU.mult),
                      reads=[b_zg[sl], b_rs[sl]], writes=[b_qf[sl]])
            if kind[0] == "q":
                hd = h if kind == "qa" else 8 + h
                dst = dr["qT_d"][hd, :, t0:t0 + 512]
                wb = cx.b_qT
            else:
                hd = h if kind == "ka" else 2 + h
                dst = dr["kT_d"][hd, :, t0:t0 + 512]
                wb = cx.b_kT
            sc.dma("sp", "st", lambda g: g.dma_start(out=dst, in_=qf[sl][:]), reads=[b_qf[sl]], writes=[])

        def vunit(ci, tt):
            hs = ci % 2
            ti = ci * 4 + tt
            sl = tt % 2
            for vi, vc in enumerate(vcols):
                for kc in range(16):
                    sc.op("pe", lambda t, kc=kc, vc=vc, vi=vi: t.matmul(
                        zq[sl][:, vi * 128:(vi + 1) * 128], lhsT=hT[hs][:, kc, tt * 128:(tt + 1) * 128],
                        rhs=w_in[:, kc, vc:vc + 128], start=(kc == 0), stop=(kc == 15)),
                        reads=[b_wins[vc // 512], b_hT[hs]], writes=[b_zq[sl]])
            sc.op("act", lambda a: a.copy(out=vsb[sl][:], in_=zq[sl][:]), reads=[b_zq[sl]], writes=[b_vsb[sl]])
            sc.dma("sp", "st", lambda g: g.dma_start(out=dr["v_d"][ti * 128:(ti + 1) * 128, :], in_=vsb[sl][:]),
                   reads=[b_vsb[sl]], writes=[])

        front_a(0)
        front_a(1)
        front_b(0)
        front_a(2)
        front_b(1)
        front_a(3)
        front_b(2)
        front_b(3)
        NH = len(heads)
        ntile = cx.nchunk * 4
        for ci in range(cx.nchunk):
            nxt = ci + 1 < cx.nchunk
            if nxt:
                front_a((ci + 1) * 4)
            head1(ci, 0)
            for k in range(1, NH):
                head1(ci, k)
                head2(ci, k - 1)
                if nxt and k in (4, 9, 14, 19):
                    j = (4, 9, 14, 19).index(k)
                    front_b((ci + 1) * 4 + j)
                    if j + 1 < 4:
                        front_a((ci + 1) * 4 + j + 1)
            vunit(ci, 0)
            head2(ci, NH - 1)
            for tt in range(1, 4):
                vunit(ci, tt)
        sc.barrier()


def mask_prepare(cx, es):
    nc, sc, dr = cx.nc, cx.sc, cx.dr
    sb, ps = _pool(cx, es)
    oh = sb([128, 32, 384], BF16, "oh")
    rb = sb([128, 264], F32, "rb")
    erb = sb([128, 264], F32, "erb")
    rbq = sb([128, 256], F32, "rbq")
    negm = sb([128, 384], F32, "negm")
    negfull = sb([128, 128], BF16, "negfull")
    bm = sb([128, 8, 384], F32, "bm")
    bm4 = sb([128, 8, 3, 512], BF16, "bm4")
    b_oh = Buf("oh")
    b_rb = Buf("rb")
    b_bm = [Buf(f"bm{h}") for h in range(8)]
    b_bm4 = [Buf(f"bm4{h}") for h in range(8)]
    sc.dma("pool", "cst", lambda g: g.dma_start(out=oh[:], in_=dr["oh"]), writes=[b_oh])
    sc.dma("sp", "cst", lambda g: g.dma_start(out=negm[:], in_=dr["negm"]), writes=[b_oh])
    sc.dma("sp", "cst", lambda g: g.dma_start(out=rb[:], in_=dr["rbs"].partition_broadcast(128)), writes=[b_rb])
    sc.op("act", lambda a: a.activation(out=erb[:], in_=rb[:], func=AF.Exp), reads=[b_rb], writes=[b_rb])
    sc.op("dve", lambda v: v.tensor_scalar(out=rbq[:], in0=rb[:, 0:256], scalar1=1.0 / SCALE, scalar2=None, op0=ALU.mult),
          reads=[b_rb], writes=[b_rb])
    sc.op("dve", lambda v: v.memset(negfull[:], -30000.0), writes=[b_oh])

    def gen():
        for b in range(32):
            for h in range(cx.nheadB):
                src = negm[:] if b == 0 else bm[:, h, :]
                sc.op("dve", lambda v, h=h, b=b, src=src: v.scalar_tensor_tensor(
                    out=bm[:, h, :], in0=oh[:, b, :], scalar=rbq[:, b * 8 + h:b * 8 + h + 1], in1=src, op0=ALU.mult, op1=ALU.add),
                    reads=[b_oh, b_rb, b_bm[h]], writes=[b_bm[h]])
                yield
        for h in range(cx.nheadB):
            for o in range(3):
                for tt in range(4):
                    sc.op("pool", lambda g, h=h, o=o, tt=tt: g.tensor_copy(out=bm4[:, h, o, tt * 128:(tt + 1) * 128],
                                                                           in_=bm[:, h, o * 128:(o + 1) * 128]),
                          reads=[b_bm[h]], writes=[b_bm4[h]])
                    yield

    g = gen()

    def step():
        try:
            next(g)
            return True
        except StopIteration:
            return False

    def drain():
        while step():
            pass

    cx.mask_step = step
    cx.mask_drain = drain
    cx.m_oh, cx.m_negm, cx.m_erb, cx.m_negfull, cx.m_bm4 = oh, negm, erb, negfull, bm4
    cx.mb_oh, cx.mb_rb, cx.mb_bm4 = b_oh, b_rb, b_bm4


def phase_C(cx):
    nc, sc, dr = cx.nc, cx.sc, cx.dr
    with contextlib.ExitStack() as es:
        sb, ps = _pool(cx, es)
        kT = sb([128, 2, S], BF16, "kTa")
        va = sb([128, NT, 256], BF16, "va")
        ones_f = sb([128, 128], F32, "ones_fC")
        b_kv = Buf("kva")
        for g_ in range(2):
            sc.dma("sp", "ld", lambda g, g_=g_: g.dma_start(out=kT[:, g_, :], in_=dr["kT_d"][g_, :, :]),
                   reads=[cx.b_kT], writes=[b_kv])
        sc.dma("sp", "ld", lambda g: g.dma_start(out=va[:], in_=dr["v_d"].rearrange("(t p) n -> p t n", p=128)[:, :, 0:256]),
               reads=[cx.b_v], writes=[b_kv])
        sc.op("dve", lambda v: v.memset(ones_f[:], 1.0), writes=[b_kv])
        qc = [sb([128, 512], BF16, "qc") for _ in range(2)]
        b_qc = [Buf("qc0"), Buf("qc1")]
        NPT = 3
        pt = [sb([128, 2, 512], BF16, "pt") for _ in range(NPT)]
        b_pt = [Buf(f"pt{i}") for i in range(NPT)]
        accD = [sb([128, 2, 512], F32, "accD") for _ in range(2)]
        accP = [sb([128, 2, 512], F32, "accP") for _ in range(2)]
        b_accD = [Buf("accD0"), Buf("accD1")]
        b_accP = [Buf("accP0"), Buf("accP1")]
        dsum = sb([128, 512], F32, "dsum")
        dsum2 = sb([128, 512], F32, "dsum2")
        b_dsum, b_dsum2 = Buf("dsum"), Buf("dsum2")
        rden = sb([128, 512], F32, "rden")
        b_rden = Buf("rden")
        osb = [sb([128, 512], BF16, "osb") for _ in range(2)]
        b_osb = [Buf("osb0"), Buf("osb1")]
        st = [ps([128, 2, 512], F32, "st") for _ in range(2)]
        b_st = [Buf("st0"), Buf("st1")]
        ot = [ps([128, 512], F32, "ot") for _ in range(2)]
        b_ot = [Buf("ot0"), Buf("ot1")]
        dn = ps([128, 512], F32, "dn")
        b_dn = Buf("dn")
        NP = NT // 2
        iters = [(hd, ci) for hd in range(cx.nheadA) for ci in range(cx.nchunk)]
        units = [(i, kp) for i in range(len(iters)) for kp in range(NP)]

        def load_q(i):
            hd, ci = iters[i]
            qs = i % 2
            sc.dma("sp", "ld", lambda g: g.dma_start(out=qc[qs][:], in_=dr["qT_d"][hd, :, ci * 512:(ci + 1) * 512]),
                   reads=[cx.b_qT], writes=[b_qc[qs]])

        def emit_st(u):
            i, kp = units[u]
            hd, ci = iters[i]
            g_ = hd // 4
            qs = i % 2
            s_ = u % 2
            if kp == 0:
                load_q(i)
            for j in range(2):
                kt = kp * 2 + j
                sc.op("pe", lambda t, j=j, kt=kt: t.matmul(st[s_][:, j, :], lhsT=kT[:, g_, kt * 128:(kt + 1) * 128], rhs=qc[qs][:],
                                                           start=True, stop=True),
                      reads=[b_kv, b_qc[qs]], writes=[b_st[s_]])

        emit_st(0)
        for u in range(len(units)):
            i, kp = units[u]
            hd, ci = iters[i]
            g_ = hd // 4
            qs = i % 2
            s_ = u % 2
            p_ = u % NPT
            if u + 1 < len(units):
                emit_st(u + 1)
            cx.mask_step()
            sc.op("act", lambda a: a.activation(out=pt[p_][:], in_=st[s_][:], func=AF.Exp, scale=SCALE),
                  reads=[b_st[s_]], writes=[b_pt[p_]])
            for j in range(2):
                kt = kp * 2 + j
                sc.op("pe", lambda t, j=j, kt=kt: t.matmul(ot[qs][:], lhsT=va[:, kt, g_ * 128:(g_ + 1) * 128], rhs=pt[p_][:, j, :],
                                                           start=(kt == 0), stop=(kt == NT - 1)),
                      reads=[b_kv, b_pt[p_]], writes=[b_ot[qs]])
            if kp % 3 == 2:
                for j in range(2):
                    sc.op("pe", lambda t, j=j: t.matmul(dn[:], lhsT=cx.ones_bf[:], rhs=pt[p_][:, j, :],
                                                        start=(kp == 2 and j == 0), stop=False),
                          reads=[cx.b_const, b_pt[p_]], writes=[b_dn])
            else:
                acc, bacc = accD[qs], b_accD[qs]
                if kp == 0:
                    sc.op("dve", lambda v: v.tensor_copy(out=acc[:], in_=pt[p_][:]), reads=[b_pt[p_]], writes=[bacc])
                else:
                    sc.op("dve", lambda v: v.tensor_tensor(out=acc[:], in0=acc[:], in1=pt[p_][:], op=ALU.add),
                          reads=[b_pt[p_], bacc], writes=[bacc])
            if kp == NP - 1:
                sc.op("dve", lambda v: v.tensor_tensor(out=dsum[:], in0=accD[qs][:, 0, :], in1=accD[qs][:, 1, :], op=ALU.add),
                      reads=[b_accD[qs]], writes=[b_dsum])
                sc.op("pe", lambda t: t.matmul(dn[:], lhsT=ones_f[:], rhs=dsum[:], start=False, stop=True),
                      reads=[b_kv, b_dsum], writes=[b_dn])
                sc.op("act", lambda a: a.activation(out=rden[:], in_=dn[:], func=AF.Ln), reads=[b_dn], writes=[b_rden])
                sc.op("act", lambda a: a.activation(out=rden[:], in_=rden[:], func=AF.Exp, scale=-1.0), reads=[b_rden], writes=[b_rden])
                sc.op("dve", lambda v: v.tensor_tensor(out=osb[qs][:], in0=ot[qs][:], in1=rden[:], op=ALU.mult),
                      reads=[b_ot[qs], b_rden], writes=[b_osb[qs]])
                sc.dma("sp", "st", lambda g: g.dma_start(out=dr["mixT_d"][hd, :, ci * 512:(ci + 1) * 512], in_=osb[qs][:]),
                       reads=[b_osb[qs]], writes=[])
        sc.barrier()


def phase_D(cx):
    nc, sc, dr = cx.nc, cx.sc, cx.dr
    with contextlib.ExitStack() as es:
        sb, ps = _pool(cx, es)
        kT = sb([128, 2, S], BF16, "kTb")
        vb = sb([128, NT, 256], BF16, "vb")
        b_kv = Buf("kvb")
        for g_ in range(2):
            sc.dma("sp", "ld", lambda g, g_=g_: g.dma_start(out=kT[:, g_, :], in_=dr["kT_d"][2 + g_, :, :]),
                   reads=[cx.b_kT], writes=[b_kv])
        sc.dma("sp", "ld", lambda g: g.dma_start(out=vb[:], in_=dr["v_d"].rearrange("(t p) n -> p t n", p=128)[:, :, 256:512]),
               reads=[cx.b_v], writes=[b_kv])
        cx.mask_drain()
        oh, negm, erb, negfull, bm4 = cx.m_oh, cx.m_negm, cx.m_erb, cx.m_negfull, cx.m_bm4
        b_oh, b_rb, b_bm4 = cx.mb_oh, cx.mb_rb, cx.mb_bm4
        qh = [sb([128, S], BF16, "qh") for _ in range(2)]
        b_qh = [Buf("qh0"), Buf("qh1")]
        pt = [sb([128, 3, 512], BF16, "ptb") for _ in range(2)]
        b_pt = [[Buf(f"ptb{i}{o}") for o in range(3)] for i in range(2)]
        rden = sb([128, 512], F32, "rdenb")
        b_rden = Buf("rdb")
        osb = [sb([128, 512], BF16, "osbb") for _ in range(2)]
        b_osb = [Buf("osbb0"), Buf("osbb1")]
        st = [[ps([128, 512], F32, "stb") for _ in range(3)] for _ in range(2)]
        b_st = [[Buf(f"stb{i}{o}") for o in range(3)] for i in range(2)]
        ot = ps([128, 512], F32, "otb")
        b_ot = Buf("otb")
        dn = ps([128, 512], F32, "dnb")
        b_dn = Buf("dnb")
        units = [(hd, ci) for hd in range(cx.nheadB) for ci in range(cx.nchunk)]

        def emit_st(u):
            hd, ci = units[u]
            g_ = hd // 4
            hsl = hd % 2
            s_ = u % 2
            if ci == 0:
                sc.dma("sp", "ld", lambda g: g.dma_start(out=qh[hsl][:], in_=dr["qT_d"][8 + hd, :, :]),
                       reads=[cx.b_qT], writes=[b_qh[hsl]])
            for o in range(3):
                mm = [(st[s_][o][:, :], cx.ident_bf[:], bm4[:, hd, o, :], [cx.b_const, b_bm4[hd]])]
                for tt in range(4):
                    qt = ci * 4 + tt
                    kt = qt + o - 1
                    cols = st[s_][o][:, tt * 128:(tt + 1) * 128]
                    if 0 <= kt < NT:
                        mm.append((cols, kT[:, g_, kt * 128:(kt + 1) * 128], qh[hsl][:, qt * 128:(qt + 1) * 128], [b_kv, b_qh[hsl]]))
                    else:
                        mm.append((cols, cx.ident_bf[:], negfull[:], [cx.b_const, b_oh]))
                for i, (o_ap, l_ap, r_ap, rd) in enumerate(mm):
                    sc.op("pe", lambda t, o_ap=o_ap, l_ap=l_ap, r_ap=r_ap, i=i: t.matmul(
                        o_ap, lhsT=l_ap, rhs=r_ap, start=(i == 0), stop=(i == len(mm) - 1)),
                        reads=rd, writes=[b_st[s_][o]])

        emit_st(0)
        for u in range(len(units)):
            hd, ci = units[u]
            g_ = hd // 4
            s_ = u % 2
            if u + 1 < len(units):
                emit_st(u + 1)
            for o in range(3):
                sc.op("act", lambda a, o=o: a.activation(out=pt[s_][:, o, :], in_=st[s_][o][:], func=AF.Exp, scale=SCALE),
                      reads=[b_st[s_][o]], writes=[b_pt[s_][o]])
            for tt in range(4):
                qt = ci * 4 + tt
                offs = [o for o in range(3) if 0 <= qt + o - 1 < NT]
                for i, o in enumerate(offs):
                    kt = qt + o - 1
                    sc.op("pe", lambda t, o=o, kt=kt, i=i, tt=tt: t.matmul(
                        ot[:, tt * 128:(tt + 1) * 128], lhsT=vb[:, kt, g_ * 128:(g_ + 1) * 128],
                        rhs=pt[s_][:, o, tt * 128:(tt + 1) * 128], start=(i == 0), stop=(i == len(offs) - 1)),
                        reads=[b_kv, b_pt[s_][o]], writes=[b_ot])
            for o in range(3):
                sc.op("pe", lambda t, o=o: t.matmul(dn[:], lhsT=cx.ones_bf[:], rhs=pt[s_][:, o, :], start=(o == 0), stop=(o == 2)),
                      reads=[cx.b_const, b_pt[s_][o]], writes=[b_dn])
            t0 = ci * 512
            sc.op("act", lambda a: a.activation(out=rden[:], in_=dn[:], func=AF.Ln, bias=erb[:, 256 + hd:257 + hd]),
                  reads=[b_dn, b_rb], writes=[b_rden])
            sc.op("act", lambda a: a.activation(out=rden[:], in_=rden[:], func=AF.Exp, scale=-1.0), reads=[b_rden], writes=[b_rden])
            sc.op("dve", lambda v: v.tensor_tensor(out=osb[s_][:], in0=ot[:], in1=rden[:], op=ALU.mult),
                  reads=[b_ot, b_rden], writes=[b_osb[s_]])
            sc.dma("sp", "st", lambda g: g.dma_start(out=dr["mixT_d"][8 + hd, :, t0:t0 + 512], in_=osb[s_][:]),
                   reads=[b_osb[s_]], writes=[])
        sc.barrier()


def phase_E(cx, es_outer):
    nc, sc, dr = cx.nc, cx.sc, cx.dr
    sbo, pso = _pool(cx, es_outer)
    cx.logits = sbo([128, NT, NE], F32, "logits")
    cx.b_logits = Buf("logits")
    with contextlib.ExitStack() as es:
        sb, ps = _pool(cx, es)
        w_out = sb([128, 16, 2048], BF16, "w_out")
        b_wout = Buf("w_out")
        bc = sb([128, 3, 2048], F32, "bc")
        b_bc = Buf("bc")
        for j in range(3):
            sc.dma("sp", "cst", lambda g, j=j: g.dma_start(out=bc[:, j, :], in_=dr["vec_d"][j:j + 1, :].partition_broadcast(128)),
                   reads=[cx.b_vec], writes=[b_bc])
        wv = dr["w_out"].rearrange("(kc p) n -> p kc n", p=128)
        for i in range(4):
            sc.dma("pool", "wst", lambda g, i=i: g.dma_start(out=w_out[:, :, i * 512:(i + 1) * 512],
                                                            in_=wv[:, :, i * 512:(i + 1) * 512]), writes=[b_wout])
        for kc in range(16):
            sc.op("dve", lambda v, kc=kc: v.tensor_tensor(out=w_out[:, kc, :], in0=w_out[:, kc, :], in1=bc[:, 2, :], op=ALU.mult),
                  reads=[b_wout, b_bc], writes=[b_wout])
        wr = sb([128, 16, NE], F32, "wr")
        wr_hi = sb([128, 16, NE], BF16, "wr_hi")
        wr_lo = sb([128, 16, NE], BF16, "wr_lo")
        b_wr = Buf("wr")
        sc.dma("sp", "cst", lambda g: g.dma_start(out=wr[:], in_=dr["w_router"].rearrange("(kc p) e -> p kc e", p=128)),
               writes=[b_wr])
        sc.op("dve", lambda v: v.tensor_copy(out=wr_hi[:], in_=wr[:]), reads=[b_wr], writes=[b_wr])
        sc.op("dve", lambda v: v.tensor_tensor(out=wr_lo[:], in0=wr[:], in1=wr_hi[:], op=ALU.subtract), reads=[b_wr], writes=[b_wr])
        mx = [sb([128, 16, 512], BF16, "mixc") for _ in range(2)]
        b_mx = [Buf("mx0"), Buf("mx1")]
        xt = [sb([128, 2048], F32, "xtE") for _ in range(2)]
        b_xt = [Buf("xtE0"), Buf("xtE1")]
        x1 = [sb([128, 2048], F32, "x1") for _ in range(2)]
        b_x1 = [Buf("x10"), Buf("x11")]
        h2f = [sb([128, 2048], F32, "h2f") for _ in range(2)]
        b_h2f = [Buf("h2f0"), Buf("h2f1")]
        hi = [sb([128, 2048], BF16, "h2hi") for _ in range(2)]
        b_hi = [Buf("hi0"), Buf("hi1")]
        lo = [sb([128, 2048], BF16, "h2lo") for _ in range(2)]
        b_lo = [Buf("lo0"), Buf("lo1")]
        hT = [sb([128, 16, 128], BF16, "h2T") for _ in range(2)]
        b_hT = [Buf("h2Thi"), Buf("h2Tlo")]
        ss = [sb([128, 1], F32, "ssE") for _ in range(2)]
        b_ss = [Buf("ssE0"), Buf("ssE1")]
        junk = sb([128, 2048], BF16, "junkE")
        b_junk = Buf("junkE")
        xo = ps([128, 4, 512], F32, "xo")
        b_xo = [Buf(f"xo{i}") for i in range(4)]
        ptr = ps([128, 16, 128], BF16, "ptrE")
        b_ptr = Buf("ptrE")
        lg = ps([128, NE], F32, "lg")
        b_lg = Buf("lg")

        def s1(ti):
            ci, tt = ti // 4, ti % 4
            ms = ci % 2
            sl = ti % 2
            if tt == 0:
                t0 = ci * 512
                sc.dma("sp", "ld", lambda g: g.dma_start(
                    out=mx[ms][:], in_=dr["mixT_d"].rearrange("h p t -> p h t")[:, :, t0:t0 + 512]),
                    reads=[cx.b_mix], writes=[b_mx[ms]])
            sc.dma("sp", "ld", lambda g: g.dma_start(out=xt[sl][:], in_=dr["x"][ti * 128:(ti + 1) * 128, :]), writes=[b_xt[sl]])
            for nb in range(4):
                for hc in range(16):
                    sc.op("pe", lambda t, nb=nb, hc=hc: t.matmul(
                        xo[:, nb, :], lhsT=mx[ms][:, hc, tt * 128:(tt + 1) * 128], rhs=w_out[:, hc, nb * 512:(nb + 1) * 512],
                        start=(hc == 0), stop=(hc == 15)), reads=[b_mx[ms], b_wout], writes=[b_xo[nb]])
                sc.op("dve", lambda v, nb=nb: v.tensor_tensor(
                    out=x1[sl][:, nb * 512:(nb + 1) * 512], in0=xo[:, nb, :], in1=xt[sl][:, nb * 512:(nb + 1) * 512], op=ALU.add),
                    reads=[b_xo[nb], b_xt[sl]], writes=[b_x1[sl]])
            sc.dma("sp", "st", lambda g: g.dma_start(out=dr["out"][ti * 128:(ti + 1) * 128, :], in_=x1[sl][:]),
                   reads=[b_x1[sl]], writes=[])
            sc.op("act", lambda a: a.activation(out=junk[:], in_=x1[sl][:], func=AF.Square, accum_out=ss[sl][:]),
                  reads=[b_x1[sl]], writes=[b_junk, b_ss[sl]])
            sc.op("act", lambda a: a.activation(out=ss[sl][:], in_=ss[sl][:], func=AF.Sqrt, scale=1.0 / D, bias=cx.epsc[:]),
                  reads=[b_ss[sl], cx.b_const], writes=[b_ss[sl]])

        def s2(ti):
            sl = ti % 2
            sc.op("dve", lambda v: v.reciprocal(out=ss[sl][:], in_=ss[sl][:]), reads=[b_ss[sl]], writes=[b_ss[sl]])
            sc.op("dve", lambda v: v.scalar_tensor_tensor(out=h2f[sl][:], in0=x1[sl][:], scalar=ss[sl][:, 0:1], in1=bc[:, 0, :],
                                                          op0=ALU.mult, op1=ALU.mult),
                  reads=[b_x1[sl], b_ss[sl], b_bc], writes=[b_h2f[sl]])
            sc.op("dve", lambda v: v.tensor_tensor(out=h2f[sl][:], in0=h2f[sl][:], in1=bc[:, 1, :], op=ALU.add),
                  reads=[b_h2f[sl], b_bc], writes=[b_h2f[sl]])
            sc.op("act", lambda a: a.copy(out=hi[sl][:], in_=h2f[sl][:]), reads=[b_h2f[sl]], writes=[b_hi[sl]])
            sc.dma("sp", "st", lambda g: g.dma_start(out=dr["h2_d"][ti * 128:(ti + 1) * 128, :], in_=hi[sl][:]),
                   reads=[b_hi[sl]], writes=[])

        def s3(ti):
            sl = ti % 2
            sc.op("dve", lambda v: v.tensor_tensor(out=lo[sl][:], in0=h2f[sl][:], in1=hi[sl][:], op=ALU.subtract),
                  reads=[b_h2f[sl], b_hi[sl]], writes=[b_lo[sl]])
            for part, (src, bsrc) in enumerate(((hi[sl], b_hi[sl]), (lo[sl], b_lo[sl]))):
                for c in range(16):
                    sc.op("pe", lambda t, c=c, src=src: t.transpose(ptr[:, c, :], src[:, c * 128:(c + 1) * 128], cx.ident_bf[:]),
                          reads=[bsrc, cx.b_const], writes=[b_ptr])
                sc.op("act", lambda a, part=part: a.copy(out=hT[part][:], in_=ptr[:]), reads=[b_ptr], writes=[b_hT[part]])
            n_mm = 0
            for (a_, ba, w_) in ((hT[0], b_hT[0], wr_hi), (hT[0], b_hT[0], wr_lo), (hT[1], b_hT[1], wr_hi)):
                for c in range(16):
                    sc.op("pe", lambda t, a_=a_, w_=w_, c=c, n_mm=n_mm: t.matmul(
                        lg[:], lhsT=a_[:, c, :], rhs=w_[:, c, :], start=(n_mm == 0), stop=(n_mm == 47)),
                        reads=[ba, b_wr], writes=[b_lg])
                    n_mm += 1
            sc.op("act", lambda a: a.copy(out=cx.logits[:, ti, :], in_=lg[:]), reads=[b_lg], writes=[cx.b_logits])

        ntile = cx.nchunk * 4
        for i in range(ntile + 2):
            if 0 <= i - 2 < ntile:
                s3(i - 2)
            if 0 <= i - 1 < ntile:
                s2(i - 1)
            if i < ntile:
                s1(i)
        sc.barrier()


def phase_F(cx, es_outer):
    nc, sc, dr = cx.nc, cx.sc, cx.dr
    sbo, pso = _pool(cx, es_outer)
    cx.idx = sbo([128, NE, 4], I32, "idx")
    cx.gsel = sbo([128, NE, 4], F32, "gsel")
    cx.b_sel = Buf("sel")
    NIT = 30
    with contextlib.ExitStack() as es:
        sb, ps = _pool(cx, es)
        triL = sb([128, 128], BF16, "triL")
        iota = sb([128, 512], mybir.dt.float16, "iota")
        tv = sb([128, NT, 2], F32, "tv")
        ones_f = sb([128, 128], F32, "ones_f")
        b_c = Buf("cF")
        sc.dma("pool", "cst", lambda g: g.dma_start(out=triL[:], in_=dr["triL"]), writes=[b_c])
        sc.dma("pool", "cst", lambda g: g.dma_start(out=iota[:], in_=dr["iota512"]), writes=[b_c])
        sc.dma("sp", "cst", lambda g: g.dma_start(out=tv[:], in_=dr["tvals"]), writes=[b_c])
        sc.op("dve", lambda v: v.memset(ones_f[:], 1.0), writes=[b_c])
        mxv = sb([128, NT], F32, "mxv")
        sm = sb([128, NT], F32, "sm")
        aff = sb([128, NE, NT], F32, "aff")
        b_aff = Buf("aff")
        b_mx = Buf("mxv")
        b_sm = Buf("sm")
        lg = cx.logits
        sc.op("dve", lambda v: v.tensor_reduce(out=mxv[:], in_=lg[:], axis=AX.X, op=ALU.max), reads=[cx.b_logits], writes=[b_mx])
        sc.op("dve", lambda v: v.tensor_scalar(out=mxv[:], in0=mxv[:], scalar1=-1.0, scalar2=None, op0=ALU.mult), reads=[b_mx], writes=[b_mx])
        for ti in range(NT):
            sc.op("act", lambda a, ti=ti: a.activation(out=aff[:, :, ti], in_=lg[:, ti, :], func=AF.Exp, bias=mxv[:, ti:ti + 1],
                                                       accum_out=sm[:, ti:ti + 1]),
                  reads=[cx.b_logits, b_mx], writes=[b_aff, b_sm])
        sc.op("dve", lambda v: v.reciprocal(out=sm[:], in_=sm[:]), reads=[b_sm], writes=[b_sm])
        for ti in range(NT):
            sc.op("dve", lambda v, ti=ti: v.tensor_scalar(out=aff[:, :, ti], in0=aff[:, :, ti], scalar1=sm[:, ti:ti + 1], scalar2=None,
                                                          op0=ALU.mult), reads=[b_aff, b_sm], writes=[b_aff])
        lo = sb([128, NE], F32, "lo")
        mid = sb([128, NE], F32, "mid")
        gm = sb([128, NE], F32, "gm")
        cntp = sb([128, NE], F32, "cntp")
        cmp_ = sb([128, NT], F32, "cmp")
        b_lo, b_mid, b_gm, b_cntp, b_cmp = Buf("lo"), Buf("mid"), Buf("gm"), Buf("cntp"), Buf("cmp")
        tot = ps([128, NE], F32, "tot")
        b_tot = Buf("tot")
        sc.op("dve", lambda v: v.memset(lo[:], 0.0), writes=[b_lo])
        for k in range(NIT):
            w = 2.0 ** (-(k + 1))
            sc.op("dve", lambda v, w=w: v.tensor_scalar(out=mid[:], in0=lo[:], scalar1=w, scalar2=None, op0=ALU.add),
                  reads=[b_lo], writes=[b_mid])
            for e in range(NE):
                sc.op("dve", lambda v, e=e: v.tensor_scalar(out=cmp_[:], in0=aff[:, e, :], scalar1=mid[:, e:e + 1], scalar2=None,
                                                            op0=ALU.is_ge, op1=ALU.add, accum_out=cntp[:, e:e + 1]),
                      reads=[b_aff, b_mid], writes=[b_cmp, b_cntp])
            sc.op("pe", lambda t: t.matmul(tot[:], lhsT=ones_f[:], rhs=cntp[:], start=True, stop=True),
                  reads=[b_c, b_cntp], writes=[b_tot])
            sc.op("dve", lambda v: v.scalar_tensor_tensor(out=gm[:], in0=tot[:], scalar=CAP - 0.5, in1=mid[:], op0=ALU.is_gt, op1=ALU.mult),
                  reads=[b_tot, b_mid], writes=[b_gm])
            sc.op("dve", lambda v: v.tensor_tensor(out=lo[:], in0=lo[:], in1=gm[:], op=ALU.max), reads=[b_lo, b_gm], writes=[b_lo])
        maskf = sb([128, NE, NT], F32, "maskf")
        maskb = sb([128, NE, NT], BF16, "maskb")
        inc = sb([128, NE, NT], F32, "inc")
        cmx = sb([128, NE, NT], BF16, "cmx")
        posm = sb([128, NE, NT], F32, "posm")
        zer = sb([128, NT], F32, "zer")
        b_mask, b_inc, b_cmx, b_posm = Buf("mask"), Buf("inc"), Buf("cmx"), Buf("posm")
        sc.op("dve", lambda v: v.memset(zer[:], 0.0), writes=[b_c])
        for e in range(NE):
            sc.op("dve", lambda v, e=e: v.tensor_scalar(out=maskf[:, e, :], in0=aff[:, e, :], scalar1=lo[:, e:e + 1], scalar2=None,
                                                        op0=ALU.is_ge), reads=[b_aff, b_lo], writes=[b_mask])
        sc.op("dve", lambda v: v.tensor_copy(out=maskb[:], in_=maskf[:]), reads=[b_mask], writes=[b_mask])
        for e in range(NE):
            sc.op("dve", lambda v, e=e: v.tensor_tensor_scan(out=inc[:, e, :], data0=maskf[:, e, :], data1=zer[:], initial=0.0,
                                                             op0=ALU.add, op1=ALU.add), reads=[b_mask, b_c], writes=[b_inc])
        sc.op("dve", lambda v: v.tensor_tensor(out=cmx[:], in0=inc[:], in1=maskf[:], op=ALU.subtract), reads=[b_inc, b_mask], writes=[b_cmx])
        posp = ps([128, NE * NT], F32, "posp")
        b_posp = Buf("posp")
        sc.op("pe", lambda t: t.matmul(posp[:], lhsT=triL[:], rhs=maskb[:].rearrange("p e t -> p (e t)"), start=True, stop=False),
              reads=[b_c, b_mask], writes=[b_posp])
        sc.op("pe", lambda t: t.matmul(posp[:], lhsT=cx.ones_bf[:], rhs=cmx[:].rearrange("p e t -> p (e t)"), start=False, stop=True),
              reads=[cx.b_const, b_cmx], writes=[b_posp])
        sc.op("dve", lambda v: v.scalar_tensor_tensor(out=posm[:].rearrange("p e t -> p (e t)"), in0=posp[:], scalar=1.0,
                                                       in1=maskf[:].rearrange("p e t -> p (e t)"), op0=ALU.add, op1=ALU.mult),
              reads=[b_posp, b_mask], writes=[b_posm])
        sc.op("dve", lambda v: v.tensor_scalar(out=posm[:], in0=posm[:], scalar1=-1.0, scalar2=None, op0=ALU.add),
              reads=[b_posm], writes=[b_posm])
        vals = sb([128, NE, NT, 4], BF16, "vals")
        b_vals = Buf("vals")
        afr = sb([128, NE, NT], F32, "afr")
        b_afr = Buf("afr")
        for e in range(NE):
            sc.op("dve", lambda v, e=e: v.tensor_copy(out=vals[:, e, :, 0:2], in_=tv[:]), reads=[b_c], writes=[b_vals])
        sc.op("dve", lambda v: v.tensor_copy(out=vals[:, :, :, 2], in_=aff[:]), reads=[b_aff], writes=[b_vals])
        sc.op("dve", lambda v: v.tensor_tensor(out=afr[:], in0=aff[:], in1=vals[:, :, :, 2], op=ALU.subtract),
              reads=[b_aff, b_vals], writes=[b_afr])
        sc.op("dve", lambda v: v.tensor_copy(out=vals[:, :, :, 3], in_=afr[:]), reads=[b_afr], writes=[b_vals])
        oh = [sb([128, 512], BF16, "ohF") for _ in range(3)]
        b_oh = [Buf(f"ohF{i}") for i in range(3)]
        selp = [ps([128, 4], F32, "selp") for _ in range(4)]
        b_selp = [Buf(f"selp{i}") for i in range(4)]
        idxf = sb([128, 4], F32, "idxf")
        b_idxf = Buf("idxf")
        selsb = sb([128, 4, 4], F32, "selsb")
        b_selsb = Buf("selsb")
        n = 0
        for e in range(cx.nexp):
            sp_ = e % 2
            for ti in range(NT):
                o = n % 3
                n += 1
                sc.op("dve", lambda v, o=o, e=e, ti=ti: v.tensor_scalar(out=oh[o][:], in0=iota[:], scalar1=posm[:, e, ti:ti + 1],
                                                                        scalar2=None, op0=ALU.is_equal),
                      reads=[b_c, b_posm], writes=[b_oh[o]])
                for jt in range(4):
                    sc.op("pe", lambda t, o=o, e=e, ti=ti, jt=jt: t.matmul(
                        selp[jt][:], lhsT=oh[o][:, jt * 128:(jt + 1) * 128], rhs=vals[:, e, ti, :],
                        start=(ti == 0), stop=(ti == NT - 1)), reads=[b_oh[o], b_vals], writes=[b_selp[jt]])
            for jt in range(4):
                sc.op("dve", lambda v, jt=jt: v.tensor_copy(out=selsb[:, jt, :], in_=selp[jt][:]), reads=[b_selp[jt]], writes=[b_selsb])
            sc.op("dve", lambda v: v.scalar_tensor_tensor(out=idxf[:], in0=selsb[:, :, 0], scalar=64.0, in1=selsb[:, :, 1],
                                                          op0=ALU.mult, op1=ALU.add), reads=[b_selsb], writes=[b_idxf])
            sc.op("dve", lambda v: v.tensor_scalar(out=idxf[:], in0=idxf[:], scalar1=0.0, scalar2=float(S - 1), op0=ALU.max, op1=ALU.min),
                  reads=[b_idxf], writes=[b_idxf])
            sc.op("dve", lambda v, e=e: v.tensor_copy(out=cx.idx[:, e, :], in_=idxf[:]), reads=[b_idxf], writes=[cx.b_sel])
            sc.op("dve", lambda v, e=e: v.tensor_tensor(out=cx.gsel[:, e, :], in0=selsb[:, :, 2], in1=selsb[:, :, 3],
                                                        op=ALU.add), reads=[b_selsb], writes=[cx.b_sel])
        if cx.dbg is not None and "dbg_idx" in cx.dbg:
            sc.dma("sp", "st", lambda g: g.dma_start(out=cx.dbg["dbg_idx"], in_=cx.idx[:]), reads=[cx.b_sel])
            sc.dma("sp", "st", lambda g: g.dma_start(out=cx.dbg["dbg_g"], in_=cx.gsel[:]), reads=[cx.b_sel])
            sc.dma("sp", "st", lambda g: g.dma_start(out=cx.dbg["dbg_aff"], in_=aff[:]), reads=[b_aff])
            sc.dma("sp", "st", lambda g: g.dma_start(out=cx.dbg["dbg_lo"], in_=lo[:]), reads=[b_lo])
        sc.barrier()


def phase_G(cx):
    nc, sc, dr = cx.nc, cx.sc, cx.dr
    with contextlib.ExitStack() as es:
        sb, ps = _pool(cx, es)
        gt2 = sb([128, 2048], F32, "gt2bc")
        b_gt2 = Buf("gt2")
        sc.dma("sp", "cst", lambda g: g.dma_start(out=gt2[:], in_=dr["vec_d"][3:4, :].partition_broadcast(128)),
               reads=[cx.b_vec], writes=[b_gt2])
        wg = [sb([128, 16, 512], BF16, "wg") for _ in range(2)]
        wu = [sb([128, 16, 512], BF16, "wu") for _ in range(2)]
        wd = [sb([128, 16, 512], BF16, "wd") for _ in range(2)]
        b_wg = [Buf("wg0"), Buf("wg1")]
        b_wu = [Buf("wu0"), Buf("wu1")]
        b_wd = [Buf("wd0"), Buf("wd1")]
        xg = sb([128, 4, 2048], BF16, "xg")
        b_xg = [Buf(f"xg{j}") for j in range(4)]
        xgT = sb([128, 16, 512], BF16, "xgT")
        b_xgT = Buf("xgT")
        hT = sb([128, 16, 512], BF16, "hTG")
        b_hT = Buf("hTG")
        sa = [sb([128, 512], F32, "sa") for _ in range(2)]
        b_sa = [Buf("sa0"), Buf("sa1")]
        ysb = sb([128, 4, 2048], F32, "ysb")
        b_ysb = [Buf(f"ysb{j}") for j in range(4)]
        tp = [ps([128, 512], BF16, "tpG") for _ in range(2)]
        b_tp = [Buf("tpG0"), Buf("tpG1")]
        ap_ = [ps([128, 512], F32, "apG") for _ in range(2)]
        up_ = [ps([128, 512], F32, "upG") for _ in range(2)]
        yp = [ps([128, 512], F32, "ypG") for _ in range(2)]
        b_ap = [Buf("ap0"), Buf("ap1")]
        b_up = [Buf("up0"), Buf("up1")]
        b_yp = [Buf("yp0"), Buf("yp1")]
        wgv = dr["w_gate"].rearrange("e (kc p) n -> e p kc n", p=128)
        wuv = dr["w_up"].rearrange("e (kc p) n -> e p kc n", p=128)
        wdv = dr["w_down"].rearrange("e (kc p) n -> e p kc n", p=128)
        NX = cx.nexp

        def gather(e):
            for jt in range(4):
                sc.dma("pool", "gat", lambda g, jt=jt: g.indirect_dma_start(
                    out=xg[:, jt, :], out_offset=None, in_=dr["h2_d"],
                    in_offset=bass.IndirectOffsetOnAxis(ap=cx.idx[:, e, jt:jt + 1], axis=0)),
                    reads=[cx.b_sel, cx.b_h2], writes=[b_xg[jt]])

        def load_s1(e, fb):
            s_ = fb % 2
            sc.dma("pool", "wst", lambda g: g.dma_start(out=wg[s_][:], in_=wgv[e, :, :, fb * 512:(fb + 1) * 512]), writes=[b_wg[s_]])
            sc.dma("pool", "wst", lambda g: g.dma_start(out=wu[s_][:], in_=wuv[e, :, :, fb * 512:(fb + 1) * 512]), writes=[b_wu[s_]])

        def load_s2(e, nb):
            s_ = nb % 2
            sc.dma("pool", "wst", lambda g: g.dma_start(out=wd[s_][:], in_=wdv[e, :, :, nb * 512:(nb + 1) * 512]), writes=[b_wd[s_]])

        gather(0)
        load_s1(0, 0)
        load_s1(0, 1)
        cnt = 0
        for e in range(NX):
            for dc in range(16):
                ts_ = dc % 2
                for jt in range(4):
                    sc.op("pe", lambda t, dc=dc, jt=jt, ts_=ts_: t.transpose(
                        tp[ts_][:, jt * 128:(jt + 1) * 128], xg[:, jt, dc * 128:(dc + 1) * 128], cx.ident_bf[:]),
                        reads=[b_xg[jt], cx.b_const], writes=[b_tp[ts_]])
                if dc % 2 == 0:
                    sc.op("act", lambda a, dc=dc, ts_=ts_: a.copy(out=xgT[:, dc, :], in_=tp[ts_][:]), reads=[b_tp[ts_]], writes=[b_xgT])
                else:
                    sc.op("dve", lambda v, dc=dc, ts_=ts_: v.tensor_copy(out=xgT[:, dc, :], in_=tp[ts_][:]), reads=[b_tp[ts_]], writes=[b_xgT])
            if e + 1 < NX:
                gather(e + 1)
            for fb in range(4):
                s_ = fb % 2
                for fc in range(4):
                    f = fb * 4 + fc
                    q = cnt % 2
                    cnt += 1
                    for dc in range(16):
                        sc.op("pe", lambda t, dc=dc, fc=fc, s_=s_, q=q: t.matmul(
                            ap_[q][:], lhsT=wg[s_][:, dc, fc * 128:(fc + 1) * 128], rhs=xgT[:, dc, :],
                            start=(dc == 0), stop=(dc == 15)), reads=[b_wg[s_], b_xgT], writes=[b_ap[q]])
                    for dc in range(16):
                        sc.op("pe", lambda t, dc=dc, fc=fc, s_=s_, q=q: t.matmul(
                            up_[q][:], lhsT=wu[s_][:, dc, fc * 128:(fc + 1) * 128], rhs=xgT[:, dc, :],
                            start=(dc == 0), stop=(dc == 15)), reads=[b_wu[s_], b_xgT], writes=[b_up[q]])
                    sc.op("act", lambda a, q=q: a.activation(out=sa[q][:], in_=ap_[q][:], func=AF.Silu), reads=[b_ap[q]], writes=[b_sa[q]])
                    sc.op("dve", lambda v, q=q, f=f: v.tensor_tensor(out=hT[:, f, :], in0=sa[q][:], in1=up_[q][:], op=ALU.mult),
                          reads=[b_sa[q], b_up[q]], writes=[b_hT])
                if fb + 2 < 4:
                    load_s1(e, fb + 2)
                if fb == 0:
                    load_s2(e, 0)
                if fb == 1:
                    load_s2(e, 1)
            for nb in range(4):
                s_ = nb % 2
                for jt in range(4):
                    q = cnt % 2
                    cnt += 1
                    for fc in range(16):
                        sc.op("pe", lambda t, fc=fc, jt=jt, s_=s_, q=q: t.matmul(
                            yp[q][:], lhsT=hT[:, fc, jt * 128:(jt + 1) * 128], rhs=wd[s_][:, fc, :],
                            start=(fc == 0), stop=(fc == 15)), reads=[b_hT, b_wd[s_]], writes=[b_yp[q]])
                    sc.op("dve", lambda v, jt=jt, nb=nb, q=q: v.scalar_tensor_tensor(
                        out=ysb[:, jt, nb * 512:(nb + 1) * 512], in0=yp[q][:], scalar=cx.gsel[:, e, jt:jt + 1],
                        in1=gt2[:, nb * 512:(nb + 1) * 512], op0=ALU.mult, op1=ALU.mult),
                        reads=[b_yp[q], cx.b_sel, b_gt2], writes=[b_ysb[jt]])
                if nb + 2 < 4:
                    load_s2(e, nb + 2)
                elif e + 1 < NX:
                    load_s1(e + 1, nb - 2)
            for jt in range(4):
                sc.dma("pool", "sca", lambda g, jt=jt: g.indirect_dma_start(
                    out=dr["out"], out_offset=bass.IndirectOffsetOnAxis(ap=cx.idx[:, e, jt:jt + 1], axis=0),
                    in_=ysb[:, jt, :], in_offset=None, compute_op=ALU.add),
                    reads=[b_ysb[jt], cx.b_sel], writes=[cx.b_out])
        sc.barrier()


def build(upto="G", dbg_names=(), nchunk=8, nheadA=8, nheadB=8, dbg_scratch=False, nexp=16):
    nc = bass.Bass("TRN2", target_bir_lowering=False)
    cx = Ctx()
    cx.nc = nc
    cx.uid = 0
    cx.nchunk = nchunk
    cx.nheadA = nheadA
    cx.nheadB = nheadB
    cx.nexp = nexp
    dr = {}

    def ext(name, shape, dt=F32):
        dr[name] = nc.dram_tensor(name, list(shape), dt, kind="ExternalInput").ap()

    ext("x", [S, D])
    ext("cT", [128, 16])
    ext("w_ada", [D, 6 * D])
    ext("b_ada", [1, 6 * D])
    ext("g1T", [128, 16])
    ext("g2T", [128, 16])
    ext("w_in", [D, 3072])
    ext("gains", [128, 4])
    ext("rbs", [1, 264])
    ext("w_out", [D, D])
    ext("w_router", [D, NE])
    ext("w_gate", [NE, D, D])
    ext("w_up", [NE, D, D])
    ext("w_down", [NE, D, D])
    ext("ident", [128, 128])
    ext("rotT", [128, 128])
    ext("cosT", [128, S])
    ext("sinT", [128, S])
    ext("oh", [128, 32, 384])
    ext("negm", [128, 384])
    ext("triL", [128, 128])
    ext("iota512", [128, 512])
    ext("tvals", [128, NT, 2])
    dr["out"] = nc.dram_tensor("out", [S, D], F32, kind="ExternalOutput").ap()
    kw = {"kind": "ExternalOutput"} if dbg_scratch else {}
    dr["vec_d"] = nc.dram_tensor("vec_d", [6, D], F32, **kw).ap()
    dr["qT_d"] = nc.dram_tensor("qT_d", [16, 128, S], BF16, **kw).ap()
    dr["kT_d"] = nc.dram_tensor("kT_d", [4, 128, S], BF16, **kw).ap()
    dr["v_d"] = nc.dram_tensor("v_d", [S, 512], BF16, **kw).ap()
    dr["mixT_d"] = nc.dram_tensor("mixT_d", [16, 128, S], BF16, **kw).ap()
    dr["h2_d"] = nc.dram_tensor("h2_d", [S, D], BF16, **kw).ap()
    cx.dr = dr
    cx.dbg = {}
    dbg_shapes = {"dbg_mod": ([128, 96], F32), "dbg_idx": ([128, NE, 4], I32), "dbg_g": ([128, NE, 4], F32),
                  "dbg_aff": ([128, NE, NT], F32), "dbg_lo": ([128, NE], F32)}
    for n_ in dbg_names:
        shp, dt = dbg_shapes[n_]
        cx.dbg[n_] = nc.dram_tensor(n_, shp, dt, kind="ExternalOutput").ap()
    cx.b_vec = Buf("vec_d")
    cx.b_qT = Buf("qT_d")
    cx.b_kT = Buf("kT_d")
    cx.b_v = Buf("v_d")
    cx.b_mix = Buf("mixT_d")
    cx.b_h2 = Buf("h2_d")
    cx.b_out = Buf("out")
    with contextlib.ExitStack() as es:
        sc = Sched(nc, es)
        cx.sc = sc
        phase_consts(cx, es)
        order = "ABCDEFG"
        n = order.index(upto)
        phase_A(cx)
        if n >= 1:
            phase_B(cx)
        es_m = contextlib.ExitStack()
        if n >= 2:
            mask_prepare(cx, es_m)
            phase_C(cx)
        if n >= 3:
            phase_D(cx)
        es_m.close()
        if n >= 4:
            phase_E(cx, es)
        if n >= 5:
            phase_F(cx, es)
        if n >= 6:
            phase_G(cx)
        sc.barrier()
    return nc, cx


def _t5_bucket(rel):
    nb = 16
    ret = np.where(rel > 0, nb, 0)
    n = np.abs(rel)
    max_exact = 8
    nf = np.maximum(n, 1).astype(np.float32)
    large = max_exact + (np.log(nf / np.float32(max_exact)) / np.float32(math.log(128 / max_exact))
                         * np.float32(nb - max_exact)).astype(np.int32)
    large = np.minimum(large, nb - 1)
    return ret + np.where(n < max_exact, n, large)


_CONST = {}


def _consts():
    if _CONST:
        return _CONST
    c = {}
    c["ident"] = np.eye(128, dtype=np.float32)
    rot = np.zeros((128, 128), np.float32)
    for m in range(128):
        if (m % 64) < 32:
            rot[m + 32, m] = -1.0
        else:
            rot[m - 32, m] = 1.0
    c["rotT"] = rot
    t = np.arange(S)
    row = (t // 64).astype(np.float32)
    col = (t % 64).astype(np.float32)
    inv = (1.0 / (np.float32(10000.0) ** (np.arange(0, 64, 2, dtype=np.float32) / np.float32(64)))).astype(np.float32)
    ang_r = (row[:, None] * inv[None, :]).astype(np.float32)
    ang_c = (col[:, None] * inv[None, :]).astype(np.float32)
    cosT = np.concatenate([np.cos(ang_r), np.cos(ang_r), np.cos(ang_c), np.cos(ang_c)], axis=1).T
    sinT = np.concatenate([np.sin(ang_r), np.sin(ang_r), np.sin(ang_c), np.sin(ang_c)], axis=1).T
    c["cosT"] = np.ascontiguousarray(cosT.astype(np.float32))
    c["sinT"] = np.ascontiguousarray(sinT.astype(np.float32))
    k = np.arange(128)[:, None, None]
    off = np.arange(3)[None, :, None] - 1
    q = np.arange(128)[None, None, :]
    rel = k + off * 128 - q
    valid = np.abs(rel) <= 128
    bk = _t5_bucket(rel)
    oh = np.zeros((128, 32, 3, 128), np.float32)
    for b in range(32):
        oh[:, b] = ((bk == b) & valid).astype(np.float32)
    c["oh"] = np.ascontiguousarray(oh.reshape(128, 32, 384))
    c["negm"] = np.ascontiguousarray(np.where(valid, 0.0, -30000.0).astype(np.float32).reshape(128, 384))
    tri = (np.arange(128)[:, None] < np.arange(128)[None, :]).astype(np.float32)
    c["triL"] = tri
    c["iota512"] = np.ascontiguousarray(np.broadcast_to(np.arange(512, dtype=np.float32), (128, 512)))
    tg = np.arange(NT)[None, :] * 128 + np.arange(128)[:, None]
    c["tvals"] = np.ascontiguousarray(np.stack([tg // 64, tg % 64], axis=-1).astype(np.float32))
    _CONST.update(c)
    return _CONST


def make_in_map(b, x, c, w_ada, b_ada, g_norm1, w_in, qn_a, kn_a, qn_b, kn_b, sink_b, rel_bias, w_out, g_norm2,
                w_router, w_gate, w_up, w_down):
    f = lambda a: np.ascontiguousarray(np.asarray(a, dtype=np.float32))
    m = dict(_consts())
    m["x"] = f(x[b])
    m["cT"] = f(np.asarray(c[b]).reshape(16, 128).T)
    m["w_ada"] = f(w_ada)
    m["b_ada"] = f(np.asarray(b_ada).reshape(1, -1))
    m["g1T"] = f(np.asarray(g_norm1).reshape(16, 128).T)
    m["g2T"] = f(np.asarray(g_norm2).reshape(16, 128).T)
    m["w_in"] = f(w_in)
    m["gains"] = f(np.stack([np.asarray(qn_a), np.asarray(kn_a), np.asarray(qn_b), np.asarray(kn_b)], axis=1))
    m["rbs"] = f(np.concatenate([np.asarray(rel_bias).reshape(-1), np.asarray(sink_b).reshape(-1)]).reshape(1, 264))
    m["w_out"] = f(w_out)
    m["w_router"] = f(w_router)
    m["w_gate"] = f(w_gate)
    m["w_up"] = f(w_up)
    m["w_down"] = f(w_down)
    return m


_NC = {}


def kernel(**inputs):
    if "nc" not in _NC:
        _NC["nc"] = build("G")[0]
    nc = _NC["nc"]
    in_maps = [make_in_map(i % 4, **inputs) for i in range(N_CORES)]
    res = run_bass_kernel_spmd(nc, in_maps, core_ids=list(range(N_CORES)))
    out = np.stack([np.asarray(res.results[b]["out"]) for b in range(4)], axis=0)
    return out.astype(np.float32)
```
